# Optimizing a Trainium2 kernel written in Bass

```python
import jax
import jax.numpy as jnp
from jax import lax
import numpy as np

D_MODEL = 1024
BATCH = 8
SEQ = 4096
DEPTH = 4

CHUNK = 64
N_EVEN = (DEPTH + 1) // 2
N_ODD = DEPTH // 2
DEEPNORM_ALPHA = (2 * DEPTH) ** 0.25
DEEPNORM_BETA = (8 * DEPTH) ** -0.25
LN_EPS = 1e-5
RMS_EPS = 1e-6

A_BLOCK = 128
A_GROUPS = 4
A_WIDTH = D_MODEL // 2
A_GROUP_DIM = A_WIDTH // A_GROUPS

B_HEADS = 4
B_HEAD_DIM = D_MODEL // 8
B_WIDTH = B_HEADS * B_HEAD_DIM
CONV_K = 4

EVEN_SPLITS = (A_WIDTH, A_WIDTH, B_WIDTH, B_WIDTH, B_WIDTH, B_WIDTH, B_HEADS, B_HEADS)
EVEN_IN = sum(EVEN_SPLITS)
EVEN_MIX = A_WIDTH + B_WIDTH

C_HEADS = 8
C_NOPE = 128
C_ROPE = 64
C_V = D_MODEL // C_HEADS
Q_LORA = 3 * D_MODEL // 8
KV_LORA = D_MODEL // 4
ODD_IN = Q_LORA + KV_LORA + C_ROPE
ROPE_THETA = 10000.0
Q_BLOCK = 128

N_EXPERTS = 32
TOP_K = 4
D_FF = D_MODEL
SWIGLU_LIMIT = 7.0
SWIGLU_ALPHA = 1.702
EXPERT_BLOCK = 512

kernel_name = 'hybrid_gmlp_gdn_mla_moe_deepnorm'


def layer_norm(x, g, b):
    xf = x.astype(jnp.float32)
    mu = jnp.mean(xf, axis=-1, keepdims=True)
    var = jnp.mean(jnp.square(xf - mu), axis=-1, keepdims=True)
    return ((xf - mu) * lax.rsqrt(var + LN_EPS) * g + b).astype(x.dtype)


def rms_norm(x, g):
    xf = x.astype(jnp.float32)
    return (xf * lax.rsqrt(jnp.mean(xf * xf, axis=-1, keepdims=True) + RMS_EPS) * g).astype(x.dtype)


def l2_normalize(x):
    return x * lax.rsqrt(jnp.sum(x * x, axis=-1, keepdims=True) + RMS_EPS)


def split_columns(h, sizes):
    return jnp.split(h, np.cumsum(sizes)[:-1].tolist(), axis=-1)


def spatial_gating(u, v, ln_g, ln_b, w_s, b_s):
    bsz, seq, _ = u.shape
    n_blk = seq // A_BLOCK
    u = jax.nn.gelu(u, approximate=False)
    v = jax.nn.gelu(v, approximate=False).reshape(bsz, seq, A_GROUPS, A_GROUP_DIM)
    v = layer_norm(v, ln_g.reshape(A_GROUPS, A_GROUP_DIM), ln_b.reshape(A_GROUPS, A_GROUP_DIM))
    pos_chunk = jnp.arange(A_BLOCK) // CHUNK
    allowed = pos_chunk[None, :] <= pos_chunk[:, None]
    w = jnp.where(allowed[None], w_s, 0)
    v = v.reshape(bsz, n_blk, A_BLOCK, A_GROUPS, A_GROUP_DIM)
    mixed = jnp.einsum('gij,bnjgc->bnigc', w, v) + b_s.T[:, :, None]
    return u * mixed.reshape(bsz, seq, A_WIDTH)


def causal_depthwise_conv(x, w):
    xp = jnp.pad(x, ((0, 0), (CONV_K - 1, 0), (0, 0)))
    return lax.conv_general_dilated(xp, w[:, None, :].astype(x.dtype), window_strides=(1,), padding='VALID',
                                    dimension_numbers=('NWC', 'WIO', 'NWC'), feature_group_count=x.shape[-1])


def chunked_gated_delta_rule(q, k, v, g, beta):
    bsz, nh, seq, dk = q.shape
    dv = v.shape[-1]
    n_chunks = seq // CHUNK

    def to_chunks(t):
        return t.reshape(bsz, nh, n_chunks, CHUNK, *t.shape[3:])

    q, k, v, g, beta = (to_chunks(t) for t in (q, k, v, g, beta))
    g = jnp.cumsum(g, axis=-1)
    idx = jnp.arange(CHUNK)
    incl = idx[:, None] >= idx[None, :]
    strict = idx[:, None] > idx[None, :]
    decay = jnp.exp(jnp.where(incl, g[..., :, None] - g[..., None, :], -jnp.inf))
    k_beta = k * beta[..., None]
    lower = jnp.where(strict, jnp.einsum('bhncd,bhnmd->bhncm', k_beta, k) * decay, 0.0)
    eye = jnp.eye(CHUNK, dtype=jnp.float32)
    t_mat = lax.linalg.triangular_solve(eye + lower, jnp.broadcast_to(eye, lower.shape),
                                        left_side=True, lower=True, unit_diagonal=True)
    u = t_mat @ (v * beta[..., None])
    w = t_mat @ (k_beta * jnp.exp(g)[..., None])
    intra = jnp.where(incl, jnp.einsum('bhncd,bhnmd->bhncm', q, k) * decay, 0.0)
    q_dec = q * jnp.exp(g)[..., None]
    g_last = g[..., -1]
    k_dec = k * jnp.exp(g_last[..., None] - g)[..., None]

    def step(state, inp):
        u_c, w_c, intra_c, q_c, k_c, gl_c = inp
        v_new = u_c - w_c @ state
        out = q_c @ state + intra_c @ v_new
        state = state * jnp.exp(gl_c)[..., None, None] + jnp.swapaxes(k_c, -1, -2) @ v_new
        return state, out

    xs = tuple(jnp.moveaxis(t, 2, 0) for t in (u, w, intra, q_dec, k_dec, g_last))
    state0 = jnp.zeros((bsz, nh, dk, dv), jnp.float32)
    _, out = lax.scan(step, state0, xs)
    return jnp.moveaxis(out, 0, 2).reshape(bsz, nh, seq, dv)


def gated_deltanet(q, k, v, a, b, z, conv_w, a_log, dt_bias, norm_w):
    bsz, seq, _ = q.shape
    qkv = jax.nn.silu(causal_depthwise_conv(jnp.concatenate([q, k, v], axis=-1), conv_w))
    q, k, v = jnp.split(qkv, 3, axis=-1)

    def heads(t):
        return t.reshape(bsz, seq, B_HEADS, B_HEAD_DIM).transpose(0, 2, 1, 3).astype(jnp.float32)

    q = l2_normalize(heads(q)) * (B_HEAD_DIM ** -0.5)
    k = l2_normalize(heads(k))
    v = heads(v)
    beta = jax.nn.sigmoid(b.astype(jnp.float32)).transpose(0, 2, 1)
    g = (-jnp.exp(a_log.astype(jnp.float32)) * jax.nn.softplus(a.astype(jnp.float32) + dt_bias)).transpose(0, 2, 1)
    o = chunked_gated_delta_rule(q, k, v, g, beta).transpose(0, 2, 1, 3)
    gate = jax.nn.silu(z.reshape(bsz, seq, B_HEADS, B_HEAD_DIM).astype(jnp.float32))
    o = rms_norm(o, norm_w) * gate
    return o.reshape(bsz, seq, B_WIDTH).astype(z.dtype)


def even_mixer(x, w_in, sgu_ln_g, sgu_ln_b, sgu_w, sgu_b, conv_w, a_log, dt_bias, gdn_norm, w_out):
    a_u, a_v, b_q, b_k, b_v, b_z, b_a, b_b = split_columns(x @ w_in, EVEN_SPLITS)
    y_a = spatial_gating(a_u, a_v, sgu_ln_g, sgu_ln_b, sgu_w, sgu_b)
    y_b = gated_deltanet(b_q, b_k, b_v, b_a, b_b, b_z, conv_w, a_log, dt_bias, gdn_norm)
    return jnp.concatenate([y_a, y_b], axis=-1) @ w_out


def rope_tables(seq):
    inv_freq = ROPE_THETA ** (-jnp.arange(0, C_ROPE, 2, dtype=jnp.float32) / C_ROPE)
    ang = jnp.arange(seq, dtype=jnp.float32)[:, None] * inv_freq[None, :]
    return jnp.cos(ang), jnp.sin(ang)


def apply_rope(x, cos, sin):
    x1, x2 = jnp.split(x, 2, axis=-1)
    c = cos[None, :, None, :]
    s = sin[None, :, None, :]
    return jnp.concatenate([x1 * c - x2 * s, x1 * s + x2 * c], axis=-1).astype(x.dtype)


def chunk_causal_attention(q, k, v, scale):
    seq = q.shape[2]
    outs = []
    for blk in range(seq // Q_BLOCK):
        q0 = blk * Q_BLOCK
        k_end = q0 + Q_BLOCK
        s = jnp.einsum('bhqd,bhkd->bhqk', q[:, :, q0:k_end], k[:, :, :k_end]).astype(jnp.float32) * scale
        q_chunk = (q0 + jnp.arange(Q_BLOCK)) // CHUNK
        k_chunk = jnp.arange(k_end) // CHUNK
        s = jnp.where(k_chunk[None, :] <= q_chunk[:, None], s, -jnp.inf)
        p = jax.nn.softmax(s, axis=-1).astype(v.dtype)
        outs.append(jnp.einsum('bhqk,bhkd->bhqd', p, v[:, :, :k_end]))
    return jnp.concatenate(outs, axis=2)


def odd_mixer(x, w_in, q_norm, w_uq, kv_norm, w_ukv, w_out, cos, sin):
    bsz, seq, _ = x.shape
    c_q, c_kv, k_pe = split_columns(x @ w_in, (Q_LORA, KV_LORA, C_ROPE))
    q = (rms_norm(c_q, q_norm) @ w_uq).reshape(bsz, seq, C_HEADS, C_NOPE + C_ROPE)
    q = jnp.concatenate([q[..., :C_NOPE], apply_rope(q[..., C_NOPE:], cos, sin)], axis=-1)
    kv = (rms_norm(c_kv, kv_norm) @ w_ukv).reshape(bsz, seq, C_HEADS, C_NOPE + C_V)
    k_pe = apply_rope(k_pe[:, :, None, :], cos, sin)
    k = jnp.concatenate([kv[..., :C_NOPE], jnp.broadcast_to(k_pe, (bsz, seq, C_HEADS, C_ROPE))], axis=-1)
    v = kv[..., C_NOPE:]
    o = chunk_causal_attention(q.transpose(0, 2, 1, 3), k.transpose(0, 2, 1, 3), v.transpose(0, 2, 1, 3),
                               (C_NOPE + C_ROPE) ** -0.5)
    return o.transpose(0, 2, 1, 3).reshape(bsz, seq, C_HEADS * C_V) @ w_out


def clamped_swiglu(h):
    glu = jnp.minimum(h[..., ::2], SWIGLU_LIMIT)
    lin = jnp.clip(h[..., 1::2], -SWIGLU_LIMIT, SWIGLU_LIMIT)
    return glu * jax.nn.sigmoid(SWIGLU_ALPHA * glu) * (lin + 1.0)


def moe_ffn(x, w_router, b_router, w_gate_up, b_gate_up, w_down, b_down):
    bsz, seq, d = x.shape
    xt = x.reshape(-1, d)
    n_tok = xt.shape[0]
    n_assign = n_tok * TOP_K
    logits = (xt @ w_router + b_router).astype(jnp.float32)
    top_logit, top_e = lax.top_k(logits, TOP_K)
    gate = jax.nn.softmax(top_logit, axis=-1)
    e_flat = top_e.reshape(-1)
    order = jnp.argsort(e_flat)
    e_sorted = e_flat[order]
    tok_sorted = (order // TOP_K).astype(jnp.int32)
    gate_sorted = gate.reshape(-1)[order]
    counts = jnp.bincount(e_flat, length=N_EXPERTS)
    padded = (counts + EXPERT_BLOCK - 1) // EXPERT_BLOCK * EXPERT_BLOCK
    start = jnp.cumsum(counts) - counts
    pad_end = jnp.cumsum(padded)
    pad_start = pad_end - padded
    dest = pad_start[e_sorted] + jnp.arange(n_assign) - start[e_sorted]
    n_blocks = -(-n_assign // EXPERT_BLOCK) + N_EXPERTS
    slots = n_blocks * EXPERT_BLOCK
    tok_buf = jnp.full((slots,), n_tok, jnp.int32).at[dest].set(tok_sorted)
    gate_buf = jnp.zeros((slots,), jnp.float32).at[dest].set(gate_sorted)
    block_expert = jnp.minimum(jnp.searchsorted(pad_end, jnp.arange(n_blocks) * EXPERT_BLOCK, side='right'),
                               N_EXPERTS - 1)
    x_pad = jnp.concatenate([xt, jnp.zeros((1, d), xt.dtype)], axis=0)

    def run_block(args):
        tok, gt, e = args
        h = x_pad[tok] @ w_gate_up[e] + b_gate_up[e]
        y = clamped_swiglu(h) @ w_down[e] + b_down[e]
        return y * gt[:, None].astype(y.dtype)

    y = lax.map(run_block, (tok_buf.reshape(n_blocks, EXPERT_BLOCK), gate_buf.reshape(n_blocks, EXPERT_BLOCK),
                            block_expert))
    out = jax.ops.segment_sum(y.reshape(slots, d), tok_buf, num_segments=n_tok + 1)[:n_tok]
    return out.reshape(bsz, seq, d)


def setup_inputs(seed: int = 0) -> dict:
    key = jax.random.key(seed)
    ks = jax.random.split(key, 32)

    def nrm(k, shape, scale):
        return jax.random.normal(k, shape, jnp.float32) * scale

    def gain(k, shape):
        return 1.0 + nrm(k, shape, 0.02)

    dt = jnp.exp(jax.random.uniform(ks[8], (N_EVEN, B_HEADS), jnp.float32, np.log(1e-3), np.log(1e-1)))
    return {
        'x': nrm(ks[0], (BATCH, SEQ, D_MODEL), 1.0),
        'even_w_in': nrm(ks[1], (N_EVEN, D_MODEL, EVEN_IN), D_MODEL ** -0.5),
        'even_sgu_ln_g': gain(ks[2], (N_EVEN, A_WIDTH)),
        'even_sgu_ln_b': nrm(ks[3], (N_EVEN, A_WIDTH), 0.02),
        'even_sgu_w': nrm(ks[4], (N_EVEN, A_GROUPS, A_BLOCK, A_BLOCK), A_BLOCK ** -0.5),
        'even_sgu_b': gain(ks[5], (N_EVEN, A_GROUPS, A_BLOCK)),
        'even_conv_w': nrm(ks[6], (N_EVEN, CONV_K, 3 * B_WIDTH), CONV_K ** -0.5),
        'even_a_log': jnp.log(jax.random.uniform(ks[7], (N_EVEN, B_HEADS), jnp.float32, 1.0, 16.0)),
        'even_dt_bias': dt + jnp.log(-jnp.expm1(-dt)),
        'even_gdn_norm': gain(ks[9], (N_EVEN, B_HEAD_DIM)),
        'even_w_out': nrm(ks[10], (N_EVEN, EVEN_MIX, D_MODEL), EVEN_MIX ** -0.5 * DEEPNORM_BETA),
        'odd_w_in': nrm(ks[11], (N_ODD, D_MODEL, ODD_IN), D_MODEL ** -0.5),
        'odd_q_norm': gain(ks[12], (N_ODD, Q_LORA)),
        'odd_w_uq': nrm(ks[13], (N_ODD, Q_LORA, C_HEADS * (C_NOPE + C_ROPE)), Q_LORA ** -0.5),
        'odd_kv_norm': gain(ks[14], (N_ODD, KV_LORA)),
        'odd_w_ukv': nrm(ks[15], (N_ODD, KV_LORA, C_HEADS * (C_NOPE + C_V)), KV_LORA ** -0.5),
        'odd_w_out': nrm(ks[16], (N_ODD, C_HEADS * C_V, D_MODEL), (C_HEADS * C_V) ** -0.5 * DEEPNORM_BETA),
        'ln_mix_g': gain(ks[17], (DEPTH, D_MODEL)),
        'ln_mix_b': nrm(ks[18], (DEPTH, D_MODEL), 0.02),
        'ln_ffn_g': gain(ks[19], (DEPTH, D_MODEL)),
        'ln_ffn_b': nrm(ks[20], (DEPTH, D_MODEL), 0.02),
        'moe_w_router': nrm(ks[21], (DEPTH, D_MODEL, N_EXPERTS), D_MODEL ** -0.5),
        'moe_b_router': nrm(ks[22], (DEPTH, N_EXPERTS), 0.01),
        'moe_w_gate_up': nrm(ks[23], (DEPTH, N_EXPERTS, D_MODEL, 2 * D_FF), D_MODEL ** -0.5),
        'moe_b_gate_up': nrm(ks[24], (DEPTH, N_EXPERTS, 2 * D_FF), 0.02),
        'moe_w_down': nrm(ks[25], (DEPTH, N_EXPERTS, D_FF, D_MODEL), D_FF ** -0.5 * DEEPNORM_BETA),
        'moe_b_down': nrm(ks[26], (DEPTH, N_EXPERTS, D_MODEL), 0.02),
    }


def reference(x, even_w_in, even_sgu_ln_g, even_sgu_ln_b, even_sgu_w, even_sgu_b, even_conv_w, even_a_log,
              even_dt_bias, even_gdn_norm, even_w_out, odd_w_in, odd_q_norm, odd_w_uq, odd_kv_norm, odd_w_ukv,
              odd_w_out, ln_mix_g, ln_mix_b, ln_ffn_g, ln_ffn_b, moe_w_router, moe_b_router, moe_w_gate_up,
              moe_b_gate_up, moe_w_down, moe_b_down):
    cos, sin = rope_tables(x.shape[1])
    for layer in range(DEPTH):
        i = layer // 2
        if layer % 2 == 0:
            mix = even_mixer(x, even_w_in[i], even_sgu_ln_g[i], even_sgu_ln_b[i], even_sgu_w[i], even_sgu_b[i],
                             even_conv_w[i], even_a_log[i], even_dt_bias[i], even_gdn_norm[i], even_w_out[i])
        else:
            mix = odd_mixer(x, odd_w_in[i], odd_q_norm[i], odd_w_uq[i], odd_kv_norm[i], odd_w_ukv[i],
                            odd_w_out[i], cos, sin)
        x = layer_norm(DEEPNORM_ALPHA * x + mix, ln_mix_g[layer], ln_mix_b[layer])
        ffn = moe_ffn(x, moe_w_router[layer], moe_b_router[layer], moe_w_gate_up[layer], moe_b_gate_up[layer],
                      moe_w_down[layer], moe_b_down[layer])
        x = layer_norm(DEEPNORM_ALPHA * x + ffn, ln_ffn_g[layer], ln_ffn_b[layer])
    return x
```

```python
import contextlib
import numpy as np
import concourse.bass as bass
import concourse.mybir as mybir
from concourse.bass_utils import run_bass_kernel_spmd

F32 = mybir.dt.float32
BF16 = mybir.dt.bfloat16
AF = mybir.ActivationFunctionType
ALU = mybir.AluOpType
AX = mybir.AxisListType

D = 1024
SEQ = 4096
NT = SEQ // 128
DEPTH = 4
NE = 32
ALPHA = (2 * DEPTH) ** 0.25
LN_EPS = 1e-5
TC = 1024
NCH = SEQ // TC
TPC = TC // 128
SILU_CAP = 11.913920224372246


class Buf:
    __slots__ = ("w", "r", "excl")

    def __init__(self, excl=False):
        self.w = None
        self.r = {}
        self.excl = excl


class Sched:
    N_DMA_SEMS = 16

    def __init__(self, nc, stack):
        self.nc = nc
        self.eng = {"pe": nc.tensor, "dve": nc.vector, "act": nc.scalar,
                    "pool": nc.gpsimd, "sp": nc.sync}
        self.sems = {}
        self.cnt = {}
        for e in self.eng:
            self.sems[e] = stack.enter_context(nc.semaphore("s_" + e))
            self.cnt[e] = 0
        for i in range(self.N_DMA_SEMS):
            k = ("d", i)
            self.sems[k] = stack.enter_context(nc.semaphore("s_d%d" % i))
            self.cnt[k] = 0
        self.seen = {e: {} for e in self.eng}
        self.dma_rr = 0
        self.n_ins = 0

    def _waits(self, e, reads, writes, extra=()):
        waits = {}

        def add(st):
            if st is None:
                return
            k, v = st
            if e == "pe" and k == "pe":
                return
            assert not (k == e and v > self.cnt[e]), "self-deadlock"
            if v > waits.get(k, 0):
                waits[k] = v
        for b in reads:
            add(b.w)
            if b.excl:
                for k, v in b.r.items():
                    if k != e:
                        add((k, v))
        for b in writes:
            add(b.w)
            for k, v in b.r.items():
                add((k, v))
        for st in extra:
            add(st)
        eng = self.eng[e]
        seen = self.seen[e]
        for k, v in waits.items():
            if seen.get(k, 0) >= v:
                continue
            seen[k] = v
            eng.wait_ge(self.sems[k], v)

    def _stamp(self, stamp, reads, writes):
        k, v = stamp
        for b in reads:
            if b.r.get(k, 0) < v:
                b.r[k] = v
        for b in writes:
            b.w = stamp
            b.r = {}

    def op(self, e, fn, reads=(), writes=(), inc=True):
        self._waits(e, reads, writes)
        ins = fn(self.eng[e])
        self.n_ins += 1
        if inc:
            self.cnt[e] += 1
            ins.then_inc(self.sems[e], 1)
            stamp = (e, self.cnt[e])
        else:
            stamp = (e, self.cnt[e] + 1)
        self._stamp(stamp, reads, writes)
        return ins

    def dma(self, out, in_, reads=(), writes=(), q="sp", **kw):
        i = self.dma_rr
        self.dma_rr = (self.dma_rr + 1) % self.N_DMA_SEMS
        k = ("d", i)
        extra = [(k, self.cnt[k])] if self.cnt[k] else []
        self._waits(q, reads, writes, extra)
        ins = self.eng[q].dma_start(out=out, in_=in_, **kw)
        self.n_ins += 1
        self.cnt[k] += 16
        ins.then_inc(self.sems[k], 16)
        self._stamp((k, self.cnt[k]), reads, writes)
        return ins

    def finish(self, bufs):
        self._waits("sp", bufs, ())

    def barrier(self):
        stamps = [(k, v) for k, v in self.cnt.items() if v > 0]
        for e in self.eng:
            self._waits(e, (), (), stamps)


class T:
    def __init__(self, t, nb=1, excl=False):
        self.t = t
        self.b = Buf(excl)
        self.bs = [Buf(excl) for _ in range(nb)]


class K:
    pass


def build(phases, n_layers=DEPTH, dbg=None, nc=None, ext_ins=None, ext_out=None):
    if nc is None:
        nc = bass.Bass("TRN2", target_bir_lowering=False)
    k = K()
    k.nc = nc
    k.dbg = dbg

    def din(name, shape, dt=F32):
        if ext_ins is not None:
            return ext_ins[name]
        return nc.dram_tensor(name, list(shape), dt, kind="ExternalInput").ap()

    I = {}

    def inp(name, shape, dt=F32):
        if name not in I:
            I[name] = din(name, shape, dt)
        return I[name]
    k.inp = inp
    inp("x", [SEQ, D])
    inp("ident", [128, 128])
    k.I = I
    k.y = ext_out if ext_out is not None else nc.dram_tensor("y", [SEQ, D], F32, kind="ExternalOutput").ap()
    k.xres = nc.dram_tensor("xres", [SEQ, D], F32, kind="Internal").ap()
    k.xT_d = nc.dram_tensor("xT_d", [8, 128, SEQ], BF16, kind="Internal").ap()
    k.xres_b = [Buf() for _ in range(NT)]
    k.xT_b = [Buf() for _ in range(NT)]
    k.y_b = [Buf() for _ in range(NT)]
    k.xcur = I["x"]
    k.xcur_is_in = True

    with contextlib.ExitStack() as st:
        S = Sched(nc, st)
        k.S = S
        k.st = st

        def sb(name, shape, dt=F32, nb=1):
            return T(st.enter_context(nc.sbuf_tensor("sb_" + name, list(shape), dt)), nb)
        k.sb = sb
        k.ps = [T(st.enter_context(nc.psum_tensor("ps%d" % i, [128, 1024], F32)), 2, excl=True)
                for i in range(4)]
        k.ident = sb("ident", [128, 128])
        S.dma(k.ident.t[:], I["ident"], writes=[k.ident.b])
        k.ones = sb("ones", [1, 128])
        S.op("dve", lambda e: e.memset(k.ones.t[:], 1.0), writes=[k.ones.b])
        k.xt = [sb("xt%d" % i, [128, D]) for i in range(2)]
        k.xTt = [sb("xTt%d" % i, [128, 8, 128], BF16) for i in range(2)]
        k.rr = 0

        for ph in phases:
            if ph[0] == "prep":
                prep_phase(k)
            elif ph[0] == "moe":
                moe_phase(k, ph[1], final=ph[2])
            elif ph[0] == "odd":
                odd_phase(k, ph[1])
            elif ph[0] == "even":
                even_phase(k, ph[1])
            elif ph[0] == "dump":
                for t in range(NT):
                    xt = k.xt[t % 2]
                    S.dma(xt.t[:], k.xcur[t * 128:(t + 1) * 128, :], reads=[k.xres_b[t]], writes=[xt.b])
                    S.dma(k.y[t * 128:(t + 1) * 128, :], xt.t[:], reads=[xt.b], writes=[k.y_b[t]])
        S.finish(k.y_b)
        S.barrier()
        global LAST_INPUTS
        LAST_INPUTS = set(I.keys())
        print("instructions:", S.n_ins, "counts:", {kk: v for kk, v in S.cnt.items() if not isinstance(kk, tuple)})
    return nc


def emit_xT(k, t, src, src_b):
    S = k.S
    j = k.rr
    k.rr ^= 1
    ps = k.ps[3]
    for c in range(8):
        S.op("pe", lambda e, c=c: e.transpose(out=ps.t[:, c * 128:(c + 1) * 128],
                                              in_=src[:, c * 128:(c + 1) * 128],
                                              identity=k.ident.t[:]),
             reads=[src_b, k.ident.b], writes=[ps.bs[c // 4]], inc=(c % 4 == 3))
    xTt = k.xTt[j]
    S.op("act", lambda e: e.copy(out=xTt.t[:].rearrange("p c t -> p (c t)"), in_=ps.t[:]),
         reads=ps.bs, writes=[xTt.b])
    S.dma(k.xT_d.rearrange("c p t -> p c t")[:, :, t * 128:(t + 1) * 128], xTt.t[:],
          reads=[xTt.b], writes=[k.xT_b[t]])


def prep_phase(k):
    S = k.S
    for t in range(NT):
        xt = k.xt[t % 2]
        S.dma(xt.t[:], k.xcur[t * 128:(t + 1) * 128, :], writes=[xt.b])
        emit_xT(k, t, xt.t, xt.b)


def moe_phase(k, l, final=False):
    S, nc, sb = k.S, k.nc, k.sb
    I = {}
    I["moe_w_router"] = k.inp("moe_w_router_%d" % l, [D, NE])
    I["moe_b_router"] = k.inp("moe_b_router_%d" % l, [1, NE])
    I["moe_w_gate_up"] = k.inp("moe_w_gate_up_%d" % l, [NE, D, 2 * D])
    I["moe_b_gate_up"] = k.inp("moe_b_gate_up_%d" % l, [NE, 2 * D])
    I["moe_w_down"] = k.inp("moe_w_down_%d" % l, [NE, D, D])
    I["moe_b_down"] = k.inp("moe_b_down_%d" % l, [NE, D])
    I["ln_ffn_g"] = k.inp("ln_ffn_g_%d" % l, [1, D])
    I["ln_ffn_b"] = k.inp("ln_ffn_b_%d" % l, [1, D])
    n_ch = (k.dbg or {}).get("n_ch", NCH)
    n_e = (k.dbg or {}).get("n_e", NE)
    with contextlib.ExitStack() as st:
        def sb(name, shape, dt=F32, nb=1):
            return T(st.enter_context(nc.sbuf_tensor("sbm%d_" % l + name, list(shape), dt)), nb)
        wr_f = sb("wr_f", [128, 8, NE])
        wr_b = sb("wr_b", [128, 8, NE], BF16)
        S.dma(wr_f.t[:], I["moe_w_router"].rearrange("(c p) e -> p c e", p=128), writes=[wr_f.b])
        S.op("dve", lambda e: e.tensor_copy(out=wr_b.t[:], in_=wr_f.t[:]), reads=[wr_f.b], writes=[wr_b.b])
        br = sb("br", [128, NE])
        S.dma(br.t[:], I["moe_b_router"].partition_broadcast(128), writes=[br.b])
        stg = [sb("stg%d" % i, [128, 2 * D]) for i in range(2)]
        bgu = T(stg[0].t[0:NE, :])
        bgu.b = stg[0].b
        S.dma(bgu.t[:], I["moe_b_gate_up"], writes=[bgu.b])
        bguT = sb("bguT", [128, 16, NE])
        ps = k.ps[3]
        for two in range(2):
            for m in range(8):
                i = two * 8 + m
                S.op("pe", lambda e, two=two, m=m, i=i: e.transpose(
                    out=ps.t[:, i * NE:(i + 1) * NE],
                    in_=bgu.t[:, 2 * m * 128 + two:2 * (m + 1) * 128:2],
                    identity=k.ident.t[0:NE, 0:NE]),
                    reads=[bgu.b, k.ident.b], writes=[ps.bs[0]], inc=(i == 15))
        S.op("dve", lambda e: e.tensor_copy(out=bguT.t[:].rearrange("p a b -> p (a b)"), in_=ps.t[:, 0:16 * NE]),
             reads=[ps.bs[0]], writes=[bguT.b])
        bgu2 = sb("bgu2", [128, 16, NE])
        S.op("dve", lambda e: e.tensor_scalar(out=bgu2.t[:, 0:8, :], in0=bguT.t[:, 0:8, :], scalar1=1.702, scalar2=None, op0=ALU.mult),
             reads=[bguT.b], writes=[bgu2.b])
        S.op("dve", lambda e: e.tensor_scalar(out=bgu2.t[:, 8:16, :], in0=bguT.t[:, 8:16, :], scalar1=1.0, scalar2=None, op0=ALU.add),
             reads=[bguT.b], writes=[bgu2.b])
        bd = sb("bd", [128, D])
        S.op("pool", lambda e: e.memset(bd.t[:], 0.0), writes=[bd.b])
        S.dma(bd.t[0:NE, :], I["moe_b_down"], writes=[bd.b])
        lng = sb("lng", [128, D])
        lnb = sb("lnb", [128, D])
        S.dma(lng.t[:], I["ln_ffn_g"].partition_broadcast(128), writes=[lng.b])
        S.dma(lnb.t[:], I["ln_ffn_b"].partition_broadcast(128), writes=[lnb.b])

        stop = (k.dbg or {}).get("stop", 99)
        if stop <= 1:
            S.barrier()
            return
        xTs = sb("xTs", [128, 8, TC], BF16)
        acc = [sb("acc%d" % i, [128, D]) for i in range(TPC)]
        gate = sb("gate", [128, TPC, NE], nb=TPC)
        wgu = [sb("wgu%d" % i, [128, 8, 2 * D], BF16, nb=8) for i in range(2)]
        wd = [sb("wd%d" % i, [128, 8, D], BF16, nb=4) for i in range(1)]
        actb = [sb("actb%d" % i, [128, 8, 512], BF16, nb=8) for i in range(2)]
        tsl = [sb("tsl%d" % i, [128, 512]) for i in range(2)]
        tlc2 = [sb("tlc2%d" % i, [128, 512]) for i in range(2)]
        tslc = [sb("tslc%d" % i, [128, 512]) for i in range(2)]
        pair_i = [0]
        small = sb("small", [128, 64])
        sm_b = [Buf() for _ in range(8)]
        gT = sb("gT", [128, 128])
        S.op("pool", lambda e: e.memset(gT.t[:], 0.0), writes=[gT.b])
        lg = sb("lg", [128, NE])
        lm = sb("lm", [128, NE])

        wgu_src = I["moe_w_gate_up"]
        wd_src = I["moe_w_down"]
        stg_rr = [0]

        def prep_steps(e, wg, wdd):
            steps = []
            for c in range(8):
                def f(c=c):
                    s_ = stg[stg_rr[0]]
                    stg_rr[0] ^= 1
                    S.dma(s_.t[:], wgu_src[e, c * 128:(c + 1) * 128, :], writes=[s_.b])
                    S.op("pool", lambda en: en.tensor_copy(out=wg.t[:, c, 0:D], in_=s_.t[:, 0:2 * D:2]),
                         reads=[s_.b], writes=[wg.bs[c]])
                    S.op("act", lambda en: en.copy(out=wg.t[:, c, D:2 * D], in_=s_.t[:, 1:2 * D:2]),
                         reads=[s_.b], writes=[wg.bs[c]])
                steps.append(f)
            for c2 in range(4):
                def f(c2=c2):
                    s_ = stg[stg_rr[0]]
                    stg_rr[0] ^= 1
                    S.dma(s_.t[:].rearrange("p (c n) -> p c n", c=2),
                          wd_src[e, c2 * 256:(c2 + 1) * 256, :].rearrange("(c p) n -> p c n", p=128),
                          writes=[s_.b])
                    S.op("act", lambda en: en.mul(out=wdd.t[:, 2 * c2:2 * c2 + 2, :].rearrange("p c n -> p (c n)"),
                                                  in_=s_.t[:], mul=1.0 / 1.702),
                         reads=[s_.b], writes=[wdd.bs[c2]])
                steps.append(f)
            return steps

        if not (k.dbg or {}).get("skip_prep"):
            for f in prep_steps(0, wgu[0], wd[0]):
                f()

        for ch in range(n_ch):
            t0 = ch * TPC
            S.dma(xTs.t[:], k.xT_d.rearrange("c p t -> p c t")[:, :, ch * TC:(ch + 1) * TC],
                  reads=k.xT_b[t0:t0 + TPC], writes=[xTs.b])
            for tt in range(TPC):
                pr = k.ps[3]
                for c in range(8):
                    S.op("pe", lambda e, c=c, tt=tt: e.matmul(pr.t[:, 0:NE], lhsT=xTs.t[:, c, tt * 128:(tt + 1) * 128],
                                                             rhs=wr_b.t[:, c, :], start=(c == 0), stop=(c == 7)),
                         reads=[xTs.b, wr_b.b], writes=[pr.bs[0]], inc=(c == 7))
                S.op("dve", lambda e: e.tensor_tensor(out=lg.t[:], in0=pr.t[:, 0:NE], in1=br.t[:], op=ALU.add),
                     reads=[pr.bs[0], br.b], writes=[lg.b])
                if stop <= 2.1:
                    continue
                S.op("dve", lambda e: e.max(out=small.t[:, 0:8], in_=lg.t[:]), reads=[lg.b], writes=[sm_b[0]])
                if stop <= 2.2:
                    continue
                S.op("dve", lambda e: e.tensor_scalar(out=lm.t[:], in0=lg.t[:], scalar1=small.t[:, 3:4], scalar2=None,
                                                      op0=ALU.is_ge), reads=[lg.b, sm_b[0]], writes=[lm.b])
                S.op("dve", lambda e: e.tensor_scalar(out=small.t[:, 8:9], in0=small.t[:, 0:1], scalar1=-1.0,
                                                      scalar2=None, op0=ALU.mult), reads=[sm_b[0]], writes=[sm_b[1]])
                S.op("act", lambda e: e.activation(out=lg.t[:], in_=lg.t[:], func=AF.Exp, bias=small.t[:, 8:9], scale=1.0),
                     reads=[lg.b, sm_b[1]], writes=[lg.b])
                S.op("dve", lambda e: e.scalar_tensor_tensor(out=lm.t[:], in0=lg.t[:], scalar=1.0, in1=lm.t[:],
                                                             op0=ALU.mult, op1=ALU.mult, accum_out=small.t[:, 9:10]),
                     reads=[lg.b, lm.b], writes=[lm.b, sm_b[2]])
                S.op("dve", lambda e: e.reciprocal(out=small.t[:, 10:11], in_=small.t[:, 9:10]), reads=[sm_b[2]], writes=[sm_b[3]])
                S.op("dve", lambda e, tt=tt: e.tensor_scalar(out=gate.t[:, tt, :], in0=lm.t[:], scalar1=small.t[:, 10:11],
                                                             scalar2=None, op0=ALU.mult),
                     reads=[lm.b, sm_b[3]], writes=[gate.bs[tt]])
                if stop <= 2.3:
                    continue
                pt = k.ps[3]
                S.op("pe", lambda e, tt=tt: e.transpose(out=pt.t[0:NE, 512:640], in_=gate.t[:, tt, :], identity=k.ident.t[:]),
                     reads=[gate.bs[tt], k.ident.b], writes=[pt.bs[1]])
                S.op("dve", lambda e: e.tensor_copy(out=gT.t[0:NE, :], in_=pt.t[0:NE, 512:640]), reads=[pt.bs[1]], writes=[gT.b])
                if stop <= 2.4:
                    continue
                pa = k.ps[2]
                for h in range(2):
                    S.op("pe", lambda e, h=h: e.matmul(pa.t[:, h * 512:(h + 1) * 512], lhsT=gT.t[:], rhs=bd.t[:, h * 512:(h + 1) * 512],
                                                       start=True, stop=True),
                         reads=[gT.b, bd.b], writes=[pa.bs[h]])
                S.op("act", lambda e, tt=tt: e.copy(out=acc[tt].t[:], in_=pa.t[:]), reads=pa.bs, writes=[acc[tt].b])

            if stop < 3:
                continue
            for e_ in range(n_e):
                wg = wgu[e_ % 2]
                wdd = wd[0]
                nxt = None
                if e_ + 1 < n_e:
                    nxt_gu = prep_steps(e_ + 1, wgu[(e_ + 1) % 2], wd[0])
                    gu_steps, d_steps = nxt_gu[:8], nxt_gu[8:]
                elif ch + 1 < n_ch:
                    nxt_gu = prep_steps(0, wgu[0], wd[0])
                    gu_steps, d_steps = nxt_gu[:8], nxt_gu[8:]
                else:
                    gu_steps, d_steps = [], []
                si = 0
                for sub in range(TC // 512):
                    ab = actb[sub % 2]
                    tok = slice(sub * 512, (sub + 1) * 512)
                    for m in range(8):
                        pg = k.ps[m % 2]
                        for half in range(2):
                            for c in range(8):
                                S.op("pe", lambda e, c=c, half=half, m=m: e.matmul(
                                    pg.t[:, half * 512:(half + 1) * 512],
                                    lhsT=wg.t[:, c, half * D + m * 128: half * D + (m + 1) * 128],
                                    rhs=xTs.t[:, c, tok], start=(c == 0), stop=(c == 7)),
                                    reads=[wg.bs[c], xTs.b], writes=[pg.bs[half]], inc=(c == 7))
                        bg = bgu2.t[:, m, e_:e_ + 1]
                        bl = bgu2.t[:, 8 + m, e_:e_ + 1]
                        pi_ = pair_i[0] % 2
                        pair_i[0] += 1
                        sl_, lc_, slc_ = tsl[pi_], tlc2[pi_], tslc[pi_]
                        S.op("act", lambda e: e.activation(out=sl_.t[:], in_=pg.t[:, 0:512], func=AF.Silu, bias=bg, scale=1.702),
                             reads=[pg.bs[0], bgu2.b], writes=[sl_.b])
                        S.op("dve", lambda e: e.tensor_scalar(out=lc_.t[:], in0=pg.t[:, 512:1024], scalar1=bl, scalar2=8.0,
                                                              op0=ALU.add, op1=ALU.min),
                             reads=[pg.bs[1], bgu2.b], writes=[lc_.b])
                        S.op("pool", lambda e: e.tensor_scalar(out=slc_.t[:], in0=sl_.t[:], scalar1=SILU_CAP, scalar2=None, op0=ALU.min),
                             reads=[sl_.b], writes=[slc_.b])
                        S.op("dve", lambda e, m=m: e.scalar_tensor_tensor(out=ab.t[:, m, :], in0=lc_.t[:], scalar=-6.0, in1=slc_.t[:],
                                                                          op0=ALU.max, op1=ALU.mult),
                             reads=[lc_.b, slc_.b], writes=[ab.bs[m]])
                        if si < len(gu_steps):
                            gu_steps[si]()
                            si += 1
                    for j in range(4):
                        tt = sub * 4 + j
                        pd = k.ps[2]
                        for h in range(2):
                            for c in range(8):
                                S.op("pe", lambda e, c=c, h=h, j=j: e.matmul(
                                    pd.t[:, h * 512:(h + 1) * 512],
                                    lhsT=ab.t[:, c, j * 128:(j + 1) * 128],
                                    rhs=wdd.t[:, c, h * 512:(h + 1) * 512], start=(c == 0), stop=(c == 7)),
                                    reads=[ab.bs[c], wdd.bs[c // 2]], writes=[pd.bs[h]], inc=(c == 7))
                            S.op("dve", lambda e, h=h, tt=tt: e.scalar_tensor_tensor(
                                out=acc[tt].t[:, h * 512:(h + 1) * 512], in0=pd.t[:, h * 512:(h + 1) * 512],
                                scalar=gate.t[:, tt, e_:e_ + 1], in1=acc[tt].t[:, h * 512:(h + 1) * 512],
                                op0=ALU.mult, op1=ALU.add),
                                reads=[pd.bs[h], gate.bs[tt], acc[tt].b], writes=[acc[tt].b])
                while si < len(gu_steps):
                    gu_steps[si]()
                    si += 1
                for f in d_steps:
                    f()

            if stop < 4:
                continue
            for tt in range(TPC):
                t = t0 + tt
                epilogue(k, t, acc[tt], lng, lnb, small, sm_b, final)
        S.barrier()
    k.xcur = k.xres
    k.xcur_is_in = False


def epilogue(k, t, acc, lng, lnb, small, sm_b, final, src=None, src_bufs=None):
    S = k.S
    xt = k.xt[t % 2]
    src_b = k.xres_b[t] if not k.xcur_is_in else Buf()
    if src is None:
        src, src_bufs = acc.t[:], [acc.b]
    S.dma(xt.t[:], k.xcur[t * 128:(t + 1) * 128, :], reads=[src_b], writes=[xt.b])
    S.op("dve", lambda e: e.scalar_tensor_tensor(out=acc.t[:], in0=xt.t[:], scalar=ALPHA, in1=src,
                                                 op0=ALU.mult, op1=ALU.add),
         reads=[xt.b] + list(src_bufs), writes=[acc.b])
    S.op("dve", lambda e: e.bn_stats(out=small.t[:, 16:22], in_=acc.t[:, 0:512]), reads=[acc.b], writes=[sm_b[4]])
    S.op("dve", lambda e: e.bn_stats(out=small.t[:, 22:28], in_=acc.t[:, 512:1024]), reads=[acc.b], writes=[sm_b[5]])
    S.op("dve", lambda e: e.bn_aggr(out=small.t[:, 28:30], in_=small.t[:, 16:28]), reads=[sm_b[4], sm_b[5]], writes=[sm_b[6]])
    S.op("dve", lambda e: e.tensor_scalar(out=small.t[:, 30:31], in0=small.t[:, 29:30], scalar1=LN_EPS, scalar2=None, op0=ALU.add),
         reads=[sm_b[6]], writes=[sm_b[7]])
    S.op("act", lambda e: e.activation(out=small.t[:, 31:32], in_=small.t[:, 30:31], func=AF.Sqrt),
         reads=[sm_b[7]], writes=[sm_b[7]])
    S.op("dve", lambda e: e.reciprocal(out=small.t[:, 32:33], in_=small.t[:, 31:32]), reads=[sm_b[7]], writes=[sm_b[7]])
    S.op("dve", lambda e: e.tensor_scalar(out=acc.t[:], in0=acc.t[:], scalar1=small.t[:, 28:29], scalar2=small.t[:, 32:33],
                                          op0=ALU.subtract, op1=ALU.mult),
         reads=[acc.b, sm_b[6], sm_b[7]], writes=[acc.b])
    S.op("pool", lambda e: e.tensor_tensor(out=acc.t[:], in0=acc.t[:], in1=lng.t[:], op=ALU.mult),
         reads=[acc.b, lng.b], writes=[acc.b])
    S.op("pool", lambda e: e.tensor_tensor(out=xt.t[:], in0=acc.t[:], in1=lnb.t[:], op=ALU.add),
         reads=[acc.b, lnb.b], writes=[xt.b])
    if final:
        S.dma(k.y[t * 128:(t + 1) * 128, :], xt.t[:], reads=[xt.b], writes=[k.y_b[t]])
    else:
        S.dma(k.xres[t * 128:(t + 1) * 128, :], xt.t[:], reads=[xt.b], writes=[k.xres_b[t]])
        emit_xT(k, t, xt.t, xt.b)


QL, KVL, ROPE = 384, 256, 64
QK_SCALE = 192.0 ** -0.5
RMS_EPS = 1e-6


def load_cast(k, S, stg, dst_fn, src_ap, width, eng="act"):
    S.dma(stg.t[:, 0:width], src_ap, writes=[stg.b])


def odd_phase(k, l):
    S, nc = k.S, k.nc
    i = l // 2
    w_in = k.inp("odd_w_in_%d" % i, [D, 704])
    q_norm = k.inp("odd_q_norm_%d" % i, [1, QL])
    w_uq = k.inp("odd_w_uq_%d" % i, [QL, 1536])
    kv_norm = k.inp("odd_kv_norm_%d" % i, [1, KVL])
    w_ukv = k.inp("odd_w_ukv_%d" % i, [KVL, 2048])
    w_out = k.inp("odd_w_out_%d" % i, [D, D])
    ln_g = k.inp("ln_mix_g_%d" % l, [1, D])
    ln_b = k.inp("ln_mix_b_%d" % l, [1, D])
    cos2T = k.inp("cos2T", [128, SEQ])
    sin2T = k.inp("sin2T", [128, SEQ])
    oT_d = nc.dram_tensor("oT_d_%d" % l, [8, 128, SEQ], BF16, kind="Internal").ap()
    oT_b = [Buf() for _ in range(8)]
    n_h = (k.dbg or {}).get("n_h", 8)
    with contextlib.ExitStack() as st:
        def sb(name, shape, dt=F32, nb=1):
            return T(st.enter_context(nc.sbuf_tensor("sbo%d_" % l + name, list(shape), dt)), nb)
        w_in_b = sb("w_in_b", [128, 8, 704], BF16)
        wkp = sb("wkp", [128, 8, 128], BF16)
        wkr = sb("wkr", [128, 8, 128], BF16)
        w_uq_b = sb("w_uq_b", [128, 3, 1536], BF16)
        wqr = sb("wqr", [128, 3, 8, 128], BF16)
        wqrot = sb("wqrot", [128, 3, 8, 128], BF16)
        w_ukv_b = sb("w_ukv_b", [128, 2, 2048], BF16)
        qn_bc = sb("qn_bc", [128, QL])
        kvn_bc = sb("kvn_bc", [128, KVL])
        lng = sb("lng", [128, D])
        lnb = sb("lnb", [128, D])
        ones_b = sb("ones_b", [128, 128], BF16)
        S.op("pool", lambda e: e.memset(ones_b.t[:], 1.0), writes=[ones_b.b])
        S.dma(qn_bc.t[:], q_norm.partition_broadcast(128), writes=[qn_bc.b])
        S.dma(kvn_bc.t[:], kv_norm.partition_broadcast(128), writes=[kvn_bc.b])
        S.dma(lng.t[:], ln_g.partition_broadcast(128), writes=[lng.b])
        S.dma(lnb.t[:], ln_b.partition_broadcast(128), writes=[lnb.b])
        cqnT = sb("cqnT", [128, 3, SEQ], BF16, nb=8)
        ckvnT = sb("ckvnT", [128, 2, SEQ], BF16, nb=8)
        kpeT = sb("kpeT", [128, SEQ], BF16, nb=8)
        xTc = [sb("xTc%d" % j, [128, 8, 512], BF16) for j in range(2)]
        cosc = [sb("cosc%d" % j, [128, 512]) for j in range(2)]
        sinc = [sb("sinc%d" % j, [128, 512]) for j in range(2)]
        tmp1 = sb("tmp1", [128, 512])
        tmp2 = sb("tmp2", [128, 512])
        junk = sb("junk", [128, 512])
        cn = sb("cn", [128, 640])
        small = sb("small", [128, 64])
        sm_b = [Buf() for _ in range(8)]
        ssb = [Buf() for _ in range(4)]
        rr = [0]
        stA = contextlib.ExitStack()
        stg = [T(stA.enter_context(nc.sbuf_tensor("sbo%d_stg%d" % (l, j), [128, 2048], F32))) for j in range(2)]

        def ld(dst_ap, src_ap, shape3, dstT):
            s_ = stg[rr[0]]
            rr[0] ^= 1
            c_, n_ = shape3
            S.dma(s_.t[:, 0:c_ * n_].rearrange("p (c n) -> p c n", c=c_), src_ap, writes=[s_.b])
            S.op("act", lambda e: e.copy(out=dst_ap, in_=s_.t[:, 0:c_ * n_].rearrange("p (c n) -> p c n", c=c_)),
                 reads=[s_.b], writes=[dstT.b])
        for c2 in range(4):
            ld(w_in_b.t[:, 2 * c2:2 * c2 + 2, :], w_in[c2 * 256:(c2 + 1) * 256, :].rearrange("(c p) n -> p c n", p=128), (2, 704), w_in_b)
        for j in range(3):
            ld(w_uq_b.t[:, j:j + 1, :], w_uq[j * 128:(j + 1) * 128, :].rearrange("(c p) n -> p c n", p=128), (1, 1536), w_uq_b)
        for j in range(2):
            ld(w_ukv_b.t[:, j:j + 1, :], w_ukv[j * 128:(j + 1) * 128, :].rearrange("(c p) n -> p c n", p=128), (1, 2048), w_ukv_b)
        S.barrier()
        stA.close()
        S.op("pool", lambda e: e.memset(wkp.t[:], 0.0), writes=[wkp.b])
        S.op("pool", lambda e: e.memset(wkr.t[:], 0.0), writes=[wkr.b])
        S.op("pool", lambda e: e.memset(wqr.t[:], 0.0), writes=[wqr.b])
        S.op("pool", lambda e: e.memset(wqrot.t[:], 0.0), writes=[wqrot.b])
        S.op("dve", lambda e: e.tensor_copy(out=wkp.t[:, :, 0:64], in_=w_in_b.t[:, :, 640:704]), reads=[w_in_b.b], writes=[wkp.b])
        S.op("dve", lambda e: e.tensor_scalar(out=wkr.t[:, :, 0:32], in0=w_in_b.t[:, :, 672:704], scalar1=-1.0, scalar2=None, op0=ALU.mult),
             reads=[w_in_b.b], writes=[wkr.b])
        S.op("dve", lambda e: e.tensor_copy(out=wkr.t[:, :, 32:64], in_=w_in_b.t[:, :, 640:672]), reads=[w_in_b.b], writes=[wkr.b])
        for j in range(3):
            v_ = w_uq_b.t[:, j, :].rearrange("p (h d) -> p h d", h=8)
            S.op("dve", lambda e: e.tensor_copy(out=wqr.t[:, j, :, 0:64], in_=v_[:, :, 128:192]), reads=[w_uq_b.b], writes=[wqr.b])
            S.op("dve", lambda e: e.tensor_scalar(out=wqrot.t[:, j, :, 0:32], in0=v_[:, :, 160:192], scalar1=-1.0, scalar2=None, op0=ALU.mult),
                 reads=[w_uq_b.b], writes=[wqrot.b])
            S.op("dve", lambda e: e.tensor_copy(out=wqrot.t[:, j, :, 32:64], in_=v_[:, :, 128:160]), reads=[w_uq_b.b], writes=[wqrot.b])


        def rope_combine(pa, pa_b, pb, pb_b, cc, sc, dst_ap, dst_b):
            S.op("dve", lambda e: e.tensor_tensor(out=tmp1.t[:], in0=pa, in1=cc.t[:], op=ALU.mult),
                 reads=[pa_b, cc.b], writes=[tmp1.b])
            S.op("dve", lambda e: e.tensor_tensor(out=tmp2.t[:], in0=pb, in1=sc.t[:], op=ALU.mult),
                 reads=[pb_b, sc.b], writes=[tmp2.b])
            S.op("pool", lambda e: e.tensor_tensor(out=dst_ap, in0=tmp1.t[:], in1=tmp2.t[:], op=ALU.add),
                 reads=[tmp1.b, tmp2.b], writes=[dst_b])

        ostop = (k.dbg or {}).get("ostop", 99)
        for ch in range(8 if ostop >= 1 else 0):
            xc = xTc[ch % 2]
            cc, sc = cosc[ch % 2], sinc[ch % 2]
            tok = slice(ch * 512, (ch + 1) * 512)
            S.dma(xc.t[:], k.xT_d.rearrange("c p t -> p c t")[:, :, tok], reads=k.xT_b[ch * 4:ch * 4 + 4], writes=[xc.b])
            S.dma(cc.t[:], cos2T[:, tok], writes=[cc.b])
            S.dma(sc.t[:], sin2T[:, tok], writes=[sc.b])
            for j4 in range(4):
                t = ch * 4 + j4
                tl_ = slice(j4 * 128, (j4 + 1) * 128)
                pp = k.ps[j4 % 2]
                for (a, b_, hb) in ((0, 512, 0), (512, 704, 1)):
                    for c in range(8):
                        S.op("pe", lambda e: e.matmul(pp.t[:, a:b_], lhsT=xc.t[:, c, tl_], rhs=w_in_b.t[:, c, a:b_],
                                                      start=(c == 0), stop=(c == 7)),
                             reads=[xc.b, w_in_b.b], writes=[pp.bs[hb]], inc=(c == 7))
                S.op("act", lambda e: e.activation(out=junk.t[:, 0:QL], in_=pp.t[:, 0:QL], func=AF.Square, accum_out=small.t[:, 0:1]),
                     reads=[pp.bs[0]], writes=[junk.b, ssb[0]])
                S.op("act", lambda e: e.activation(out=junk.t[:, 0:KVL], in_=pp.t[:, QL:QL + KVL], func=AF.Square, accum_out=small.t[:, 1:2]),
                     reads=pp.bs, writes=[junk.b, ssb[1]])
                S.op("dve", lambda e: e.tensor_scalar(out=small.t[:, 2:3], in0=small.t[:, 0:1], scalar1=1.0 / QL, scalar2=RMS_EPS,
                                                      op0=ALU.mult, op1=ALU.add), reads=[ssb[0]], writes=[ssb[2]])
                S.op("dve", lambda e: e.tensor_scalar(out=small.t[:, 3:4], in0=small.t[:, 1:2], scalar1=1.0 / KVL, scalar2=RMS_EPS,
                                                      op0=ALU.mult, op1=ALU.add), reads=[ssb[1]], writes=[ssb[2]])
                S.op("act", lambda e: e.activation(out=small.t[:, 4:6], in_=small.t[:, 2:4], func=AF.Sqrt), reads=[ssb[2]], writes=[ssb[3]])
                S.op("dve", lambda e: e.reciprocal(out=small.t[:, 6:8], in_=small.t[:, 4:6]), reads=[ssb[3]], writes=[ssb[3]])
                S.op("dve", lambda e: e.scalar_tensor_tensor(out=cn.t[:, 0:QL], in0=pp.t[:, 0:QL], scalar=small.t[:, 6:7], in1=qn_bc.t[:],
                                                             op0=ALU.mult, op1=ALU.mult),
                     reads=[pp.bs[0], ssb[3], qn_bc.b], writes=[cn.b])
                S.op("dve", lambda e: e.scalar_tensor_tensor(out=cn.t[:, QL:640], in0=pp.t[:, QL:640], scalar=small.t[:, 7:8], in1=kvn_bc.t[:],
                                                             op0=ALU.mult, op1=ALU.mult),
                     reads=pp.bs + [ssb[3], kvn_bc.b], writes=[cn.b])
                pt = k.ps[2]
                for j in range(5):
                    S.op("pe", lambda e: e.transpose(out=pt.t[:, j * 128:(j + 1) * 128], in_=cn.t[:, j * 128:(j + 1) * 128], identity=k.ident.t[:]),
                         reads=[cn.b, k.ident.b], writes=[pt.bs[j // 4]], inc=(j in (3, 4)))
                S.op("act", lambda e: e.copy(out=cqnT.t[:, :, t * 128:(t + 1) * 128], in_=pt.t[:, 0:384].rearrange("p (j t) -> p j t", j=3)),
                     reads=[pt.bs[0]], writes=[cqnT.bs[ch]])
                S.op("act", lambda e: e.copy(out=ckvnT.t[:, :, t * 128:(t + 1) * 128], in_=pt.t[:, 384:640].rearrange("p (j t) -> p j t", j=2)),
                     reads=pt.bs, writes=[ckvnT.bs[ch]])
            pk = k.ps[3]
            for (wt, hb) in ((wkp, 0), (wkr, 1)):
                for c in range(8):
                    S.op("pe", lambda e: e.matmul(pk.t[:, hb * 512:(hb + 1) * 512], lhsT=wt.t[:, c, :], rhs=xc.t[:, c, :],
                                                  start=(c == 0), stop=(c == 7)),
                         reads=[wt.b, xc.b], writes=[pk.bs[hb]], inc=(c == 7))
            rope_combine(pk.t[:, 0:512], pk.bs[0], pk.t[:, 512:1024], pk.bs[1], cc, sc, kpeT.t[:, tok], kpeT.bs[ch])

        stB = contextlib.ExitStack()

        def sb(name, shape, dt=F32, nb=1):
            return T(stB.enter_context(nc.sbuf_tensor("sbo%d_" % l + name, list(shape), dt)), nb)
        qn = sb("qn", [128, SEQ], BF16, nb=8)
        qr = sb("qr", [128, SEQ], BF16, nb=8)
        kn = sb("kn", [128, SEQ], BF16, nb=8)
        vv = sb("vv", [128, NT, 128], BF16, nb=8)
        oTh = sb("oTh", [128, SEQ], BF16)
        sq = sb("sq", [128, 512], BF16)
        PT = [sb("PT%d" % j, [128, 512], BF16) for j in range(2)]
        rs = sb("rs", [128, 128])
        mq = sb("mq", [128, 16])
        negB = sb("negB", [128, 8])
        for h in range(n_h if ostop >= 2 else 0):
            for ch in range(8):
                tok = slice(ch * 512, (ch + 1) * 512)
                cc, sc = cosc[ch % 2], sinc[ch % 2]
                S.dma(cc.t[:], cos2T[:, tok], writes=[cc.b])
                S.dma(sc.t[:], sin2T[:, tok], writes=[sc.b])
                p0 = k.ps[0]
                for j in range(3):
                    S.op("pe", lambda e: e.matmul(p0.t[:, 0:512], lhsT=w_uq_b.t[:, j, h * 192:h * 192 + 128], rhs=cqnT.t[:, j, tok],
                                                  start=(j == 0), stop=(j == 2)),
                         reads=[w_uq_b.b, cqnT.bs[ch]], writes=[p0.bs[0]], inc=(j == 2))
                S.op("act", lambda e: e.copy(out=qn.t[:, tok], in_=p0.t[:, 0:512]), reads=[p0.bs[0]], writes=[qn.bs[ch]])
                for j in range(2):
                    S.op("pe", lambda e: e.matmul(p0.t[:, 512:1024], lhsT=w_ukv_b.t[:, j, h * 256:h * 256 + 128], rhs=ckvnT.t[:, j, tok],
                                                  start=(j == 0), stop=(j == 1)),
                         reads=[w_ukv_b.b, ckvnT.bs[ch]], writes=[p0.bs[1]], inc=(j == 1))
                S.op("act", lambda e: e.copy(out=kn.t[:, tok], in_=p0.t[:, 512:1024]), reads=[p0.bs[1]], writes=[kn.bs[ch]])
                p1 = k.ps[1]
                for (wt, hb) in ((wqr, 0), (wqrot, 1)):
                    for j in range(3):
                        S.op("pe", lambda e: e.matmul(p1.t[:, hb * 512:(hb + 1) * 512], lhsT=wt.t[:, j, h, :], rhs=cqnT.t[:, j, tok],
                                                      start=(j == 0), stop=(j == 2)),
                             reads=[wt.b, cqnT.bs[ch]], writes=[p1.bs[hb]], inc=(j == 2))
                rope_combine(p1.t[:, 0:512], p1.bs[0], p1.t[:, 512:1024], p1.bs[1], cc, sc, qr.t[:, tok], qr.bs[ch])
                p2 = k.ps[2]
                for j4 in range(4):
                    t = ch * 4 + j4
                    for j in range(2):
                        S.op("pe", lambda e: e.matmul(p2.t[:, j4 * 128:(j4 + 1) * 128], lhsT=ckvnT.t[:, j, t * 128:(t + 1) * 128],
                                                      rhs=w_ukv_b.t[:, j, h * 256 + 128:h * 256 + 256], start=(j == 0), stop=(j == 1)),
                             reads=[w_ukv_b.b, ckvnT.bs[ch]], writes=[p2.bs[0]], inc=(j == 1 and j4 == 3))
                S.op("act", lambda e: e.copy(out=vv.t[:, ch * 4:ch * 4 + 4, :], in_=p2.t[:, 0:512].rearrange("p (a b) -> p a b", a=4)),
                     reads=[p2.bs[0]], writes=[vv.bs[ch]])
                p3 = k.ps[3]
                for half, (srcs) in enumerate((((qn, qn.bs[ch]), (qr, qr.bs[ch])), ((kn, kn.bs[ch]), (kpeT, kpeT.bs[ch])))):
                    for si, (tt_, tb_) in enumerate(srcs):
                        S.op("act", lambda e: e.activation(out=sq.t[:], in_=tt_.t[:, tok], func=AF.Square), reads=[tb_], writes=[sq.b])
                        S.op("pe", lambda e: e.matmul(p3.t[:, half * 512:(half + 1) * 512], lhsT=ones_b.t[:], rhs=sq.t[:],
                                                      start=(si == 0), stop=(si == 1)),
                             reads=[ones_b.b, sq.b], writes=[p3.bs[half]])
                    S.op("dve", lambda e: e.tensor_reduce(out=mq.t[:, half * 8 + ch:half * 8 + ch + 1], in_=p3.t[:, half * 512:(half + 1) * 512],
                                                          axis=AX.X, op=ALU.max),
                         reads=[p3.bs[half]], writes=[mq.b])
            S.op("dve", lambda e: e.tensor_reduce(out=small.t[:, 8:9], in_=mq.t[:, 0:8], axis=AX.X, op=ALU.max), reads=[mq.b], writes=[sm_b[0]])
            S.op("dve", lambda e: e.tensor_reduce(out=small.t[:, 9:10], in_=mq.t[:, 8:16], axis=AX.X, op=ALU.max), reads=[mq.b], writes=[sm_b[0]])
            S.op("dve", lambda e: e.tensor_tensor(out=small.t[:, 10:11], in0=small.t[:, 8:9], in1=small.t[:, 9:10], op=ALU.mult),
                 reads=[sm_b[0]], writes=[sm_b[1]])
            S.op("act", lambda e: e.activation(out=small.t[:, 11:12], in_=small.t[:, 10:11], func=AF.Sqrt), reads=[sm_b[1]], writes=[sm_b[1]])
            S.op("dve", lambda e: e.tensor_scalar(out=negB.t[:, h:h + 1], in0=small.t[:, 11:12], scalar1=-QK_SCALE, scalar2=None, op0=ALU.mult),
                 reads=[sm_b[1]], writes=[negB.b])
            gi = 0
            for qt in range(NT if ostop >= 3 else 0):
                qs = slice(qt * 128, (qt + 1) * 128)
                po = k.ps[2 + (qt % 2)]
                nblk = qt + 1
                for g0 in range(0, nblk, 4):
                    nb_ = min(4, nblk - g0)
                    psS = k.ps[(gi // 2) % 2]
                    hb = gi % 2
                    pt_ = PT[gi % 2]
                    gi += 1
                    for bi in range(nb_):
                        kb = g0 + bi
                        ks = slice(kb * 128, (kb + 1) * 128)
                        o_ = psS.t[:, hb * 512 + bi * 128:hb * 512 + (bi + 1) * 128]
                        S.op("pe", lambda e: e.matmul(o_, lhsT=kn.t[:, ks], rhs=qn.t[:, qs], start=True, stop=False),
                             reads=[kn.bs[kb // 4], qn.bs[qt // 4]], writes=[psS.bs[hb]], inc=False)
                        S.op("pe", lambda e: e.matmul(o_, lhsT=kpeT.t[:, ks], rhs=qr.t[:, qs], start=False, stop=True),
                             reads=[kpeT.bs[kb // 4], qr.bs[qt // 4]], writes=[psS.bs[hb]], inc=(bi == nb_ - 1))
                    S.op("act", lambda e: e.activation(out=pt_.t[:, 0:nb_ * 128], in_=psS.t[:, hb * 512:hb * 512 + nb_ * 128], func=AF.Exp,
                                                       bias=negB.t[:, h:h + 1], scale=QK_SCALE),
                         reads=[psS.bs[hb], negB.b], writes=[pt_.b])
                    if g0 + nb_ == nblk:
                        bi = nb_ - 1
                        S.op("pool", lambda e: e.memset(pt_.t[64:128, bi * 128:bi * 128 + 64], 0.0), writes=[pt_.b])
                    for bi in range(nb_):
                        kb = g0 + bi
                        S.op("pe", lambda e: e.matmul(po.t[:, 0:128], lhsT=vv.t[:, kb, :], rhs=pt_.t[:, bi * 128:(bi + 1) * 128],
                                                      start=(kb == 0), stop=(kb == nblk - 1)),
                             reads=[vv.bs[kb // 4], pt_.b], writes=[po.bs[0]], inc=False)
                        S.op("pe", lambda e: e.matmul(po.t[:, 128:256], lhsT=ones_b.t[:], rhs=pt_.t[:, bi * 128:(bi + 1) * 128],
                                                      start=False, stop=(kb == nblk - 1)),
                             reads=[ones_b.b, pt_.b], writes=[po.bs[0]], inc=(bi == nb_ - 1))
                S.op("dve", lambda e: e.reciprocal(out=rs.t[:], in_=po.t[:, 128:256]), reads=[po.bs[0]], writes=[rs.b])
                S.op("dve", lambda e: e.tensor_tensor(out=oTh.t[:, qs], in0=po.t[:, 0:128], in1=rs.t[:], op=ALU.mult),
                     reads=[po.bs[0], rs.b], writes=[oTh.b])
            S.dma(oT_d[h], oTh.t[:], reads=[oTh.b], writes=[oT_b[h]])

        S.barrier()
        stB.close()

        def sb(name, shape, dt=F32, nb=1):
            return T(st.enter_context(nc.sbuf_tensor("sbo%d_" % l + name, list(shape), dt)), nb)
        stg = [sb("stgC", [128, 2048])]
        rr[0] = 0
        w_out_b = sb("w_out_b", [128, 8, D], BF16)
        for c2 in range(4):
            ld(w_out_b.t[:, 2 * c2:2 * c2 + 2, :], w_out[c2 * 256:(c2 + 1) * 256, :].rearrange("(c p) n -> p c n", p=128), (2, D), w_out_b)
            rr[0] = 0
        work = [sb("work%d" % j, [128, D]) for j in range(1)]
        for ch in range(8 if ostop >= 4 else 0):
            xc = xTc[ch % 2]
            tok = slice(ch * 512, (ch + 1) * 512)
            S.dma(xc.t[:], oT_d.rearrange("c p t -> p c t")[:, :, tok], reads=oT_b, writes=[xc.b])
            for j4 in range(4):
                t = ch * 4 + j4
                pp = k.ps[j4 % 2]
                for hb in range(2):
                    for c in range(8):
                        S.op("pe", lambda e: e.matmul(pp.t[:, hb * 512:(hb + 1) * 512], lhsT=xc.t[:, c, j4 * 128:(j4 + 1) * 128],
                                                      rhs=w_out_b.t[:, c, hb * 512:(hb + 1) * 512], start=(c == 0), stop=(c == 7)),
                             reads=[xc.b, w_out_b.b], writes=[pp.bs[hb]], inc=(c == 7))
                epilogue(k, t, work[0], lng, lnb, small, sm_b, False, src=pp.t[:], src_bufs=pp.bs)
        S.barrier()
    k.xcur = k.xres
    k.xcur_is_in = False


def even_phase(k, l):
    S, nc = k.S, k.nc
    i = l // 2
    w_in = k.inp("even_w_in_%d" % i, [D, 3080])
    w_out = k.inp("even_w_out_%d" % i, [D, D])
    sgT = k.inp("even_sgu_wT_%d" % i, [128, 4, 128])
    bsT_d = k.inp("even_sgu_bT_%d" % i, [128, 4])
    sg_d = k.inp("even_sgu_ln_g_%d" % i, [1, 512])
    sb_d = k.inp("even_sgu_ln_b_%d" % i, [1, 512])
    cw_d = k.inp("even_conv_wT_%d" % i, [128, 12, 4])
    alog_d = k.inp("even_a_log_%d" % i, [1, 4])
    dtb_d = k.inp("even_dt_bias_%d" % i, [1, 4])
    gnw_d = k.inp("even_gdn_norm_%d" % i, [1, 128])
    ln_g = k.inp("ln_mix_g_%d" % l, [1, D])
    ln_b = k.inp("ln_mix_b_%d" % l, [1, D])
    tri_d = k.inp("tri", [128, 128])
    maskL_d = k.inp("maskL", [128, 128])
    n_chunks = (k.dbg or {}).get("e_ch", 8)
    with contextlib.ExitStack() as st:
        def sb(name, shape, dt=F32, nb=1):
            return T(st.enter_context(nc.sbuf_tensor("sbe%d_" % l + name, list(shape), dt)), nb)
        w_in_b = sb("w_in_b", [128, 8, 3080], BF16)
        w_out_b = sb("w_out_b", [128, 8, D], BF16)
        wsT = sb("wsT", [128, 4, 128], BF16)
        bsT = sb("bsT", [128, 4])
        sg_bc = sb("sg_bc", [128, 512])
        sb_bc = sb("sb_bc", [128, 512])
        cw = sb("cw", [128, 12, 4])
        negA = sb("negA", [128, 4])
        dtb = sb("dtb", [128, 4])
        gnw = sb("gnw", [128, 128])
        lng = sb("lng", [128, D])
        lnb = sb("lnb", [128, D])
        tri = sb("tri", [128, 128])
        maskL = sb("maskL", [128, 128])
        ones_f = sb("ones_f", [128, 128])
        S.op("pool", lambda e: e.memset(ones_f.t[:], 1.0), writes=[ones_f.b])
        for (dst, src) in ((bsT, bsT_d), (cw, cw_d), (tri, tri_d), (maskL, maskL_d)):
            S.dma(dst.t[:], src, writes=[dst.b])
        for (dst, src) in ((sg_bc, sg_d), (sb_bc, sb_d), (negA, alog_d), (dtb, dtb_d), (gnw, gnw_d), (lng, ln_g), (lnb, ln_b)):
            S.dma(dst.t[:], src.partition_broadcast(128), writes=[dst.b])
        S.op("act", lambda e: e.activation(out=negA.t[:], in_=negA.t[:], func=AF.Exp), reads=[negA.b], writes=[negA.b])
        S.op("dve", lambda e: e.tensor_scalar(out=negA.t[:], in0=negA.t[:], scalar1=-1.0, scalar2=None, op0=ALU.mult),
             reads=[negA.b], writes=[negA.b])
        stA = contextlib.ExitStack()
        stg = [T(stA.enter_context(nc.sbuf_tensor("sbe%d_stg%d" % (l, j), [128, 3080], F32))) for j in range(2)]
        for c in range(8):
            s_ = stg[c % 2]
            S.dma(s_.t[:], w_in[c * 128:(c + 1) * 128, :], writes=[s_.b])
            S.op("act" if c % 2 else "pool", lambda e: (e.copy if c % 2 else e.tensor_copy)(out=w_in_b.t[:, c, :], in_=s_.t[:]),
                 reads=[s_.b], writes=[w_in_b.b])
        for c2 in range(4):
            s_ = stg[c2 % 2]
            S.dma(s_.t[:, 0:2048].rearrange("p (c n) -> p c n", c=2), w_out[c2 * 256:(c2 + 1) * 256, :].rearrange("(c p) n -> p c n", p=128),
                  writes=[s_.b])
            S.op("act", lambda e: e.copy(out=w_out_b.t[:, 2 * c2:2 * c2 + 2, :], in_=s_.t[:, 0:2048].rearrange("p (c n) -> p c n", c=2)),
                 reads=[s_.b], writes=[w_out_b.b])
        s_ = stg[0]
        S.dma(s_.t[:, 0:512].rearrange("p (g i) -> p g i", g=4), sgT, writes=[s_.b])
        S.op("dve", lambda e: e.tensor_copy(out=wsT.t[:], in_=s_.t[:, 0:512].rearrange("p (g i) -> p g i", g=4)), reads=[s_.b], writes=[wsT.b])
        S.op("dve", lambda e: e.memset(wsT.t[64:128, :, 0:64], 0.0), writes=[wsT.b])
        S.barrier()
        stA.close()

        xTc = [sb("xTc%d" % j, [128, 8, 512], BF16) for j in range(2)]
        raw = sb("raw", [128, 12, 515], nb=12)
        qkv = sb("qkv", [128, 12, 512], nb=12)
        S.op("pool", lambda e: e.memset(raw.t[:, :, 0:3], 0.0), writes=raw.bs)
        cvt = sb("cvt", [128, 512])
        sqt = sb("sqt", [128, 512])
        rst = sb("rst", [128, 512])
        u_sb = sb("u_sb", [128, 512])
        vg = sb("vg", [128, 512])
        vnb = sb("vnb", [128, 512], BF16)
        sz = sb("sz", [128, 512])
        mixt = sb("mixt", [128, D], nb=8)
        work = sb("work", [128, D])
        small = sb("small", [128, 64])
        sm_b = [Buf() for _ in range(8)]
        sm2 = sb("sm2", [128, 96])
        s2 = [Buf() for _ in range(24)]
        St = [sb("St%d" % h, [128, 128]) for h in range(4)]
        for h in range(4):
            S.op("pool", lambda e: e.memset(St[h].t[:], 0.0), writes=[St[h].b])
        tn = {}
        for nm in ("kbg", "kdec", "vb", "gB", "t1", "t2", "Dn", "DT", "egr", "qdT", "M0", "M1", "N0", "N1", "P0", "P1", "intraT", "u", "wT",
                   "vnew", "junk", "yb"):
            tn[nm] = sb("g_" + nm, [128, 128])
        slots = [(k.ps[2].t[:, 0:128], k.ps[2].bs[0]), (k.ps[2].t[:, 512:640], k.ps[2].bs[1]),
                 (k.ps[3].t[:, 0:128], k.ps[3].bs[0]), (k.ps[3].t[:, 512:640], k.ps[3].bs[1])]
        sl_i = [0]

        def slot():
            r = slots[sl_i[0] % len(slots)]
            sl_i[0] += 1
            return r
        ident = k.ident

        def mm(out_ap, out_b, lhsT, lb, rhs, rb, start=True, stop=True):
            S.op("pe", lambda e: e.matmul(out_ap, lhsT=lhsT, rhs=rhs, start=start, stop=stop), reads=[lb, rb], writes=[out_b])

        estop = (k.dbg or {}).get("estop", 99)
        for ch in range(n_chunks if estop >= 1 else 0):
            xc = xTc[ch % 2]
            tok = slice(ch * 512, (ch + 1) * 512)
            S.dma(xc.t[:], k.xT_d.rearrange("c p t -> p c t")[:, :, tok], reads=k.xT_b[ch * 4:ch * 4 + 4], writes=[xc.b])
            for cc in range(12):
                pp = k.ps[1]
                hb = cc % 2
                for c in range(8):
                    S.op("pe", lambda e: e.matmul(pp.t[:, hb * 512:(hb + 1) * 512], lhsT=w_in_b.t[:, c, 1024 + cc * 128:1024 + (cc + 1) * 128],
                                                  rhs=xc.t[:, c, :], start=(c == 0), stop=(c == 7)),
                         reads=[w_in_b.b, xc.b], writes=[pp.bs[hb]], inc=(c == 7))
                if ch > 0:
                    S.op("dve", lambda e: e.tensor_copy(out=raw.t[:, cc, 0:3], in_=raw.t[:, cc, 512:515]), reads=[raw.bs[cc]], writes=[raw.bs[cc]])
                S.op("act", lambda e: e.copy(out=raw.t[:, cc, 3:515], in_=pp.t[:, hb * 512:(hb + 1) * 512]), reads=[pp.bs[hb]], writes=[raw.bs[cc]])
                S.op("act", lambda e: e.activation(out=cvt.t[:], in_=raw.t[:, cc, 3:515], func=AF.Copy, scale=cw.t[:, cc, 3:4]),
                     reads=[raw.bs[cc], cw.b], writes=[cvt.b])
                for s_ in (1, 2, 3):
                    S.op("dve", lambda e: e.scalar_tensor_tensor(out=cvt.t[:], in0=raw.t[:, cc, 3 - s_:515 - s_], scalar=cw.t[:, cc, 3 - s_:4 - s_],
                                                                 in1=cvt.t[:], op0=ALU.mult, op1=ALU.add),
                         reads=[raw.bs[cc], cw.b, cvt.b], writes=[cvt.b])
                S.op("act", lambda e: e.activation(out=qkv.t[:, cc, :], in_=cvt.t[:], func=AF.Silu), reads=[cvt.b], writes=[qkv.bs[cc]])
                if cc < 8:
                    S.op("act", lambda e: e.activation(out=sqt.t[:], in_=qkv.t[:, cc, :], func=AF.Square), reads=[qkv.bs[cc]], writes=[sqt.b])
                    pn = k.ps[0]
                    S.op("pe", lambda e: e.matmul(pn.t[:, hb * 512:(hb + 1) * 512], lhsT=ones_f.t[:], rhs=sqt.t[:], start=True, stop=True),
                         reads=[ones_f.b, sqt.b], writes=[pn.bs[hb]])
                    S.op("dve", lambda e: e.tensor_scalar(out=rst.t[:], in0=pn.t[:, hb * 512:(hb + 1) * 512], scalar1=RMS_EPS, scalar2=None, op0=ALU.add),
                         reads=[pn.bs[hb]], writes=[rst.b])
                    S.op("act", lambda e: e.activation(out=rst.t[:], in_=rst.t[:], func=AF.Sqrt), reads=[rst.b], writes=[rst.b])
                    S.op("dve", lambda e: e.reciprocal(out=rst.t[:], in_=rst.t[:]), reads=[rst.b], writes=[rst.b])
                    sc_ = (128.0 ** -0.5) if cc < 4 else 1.0
                    S.op("dve", lambda e: e.scalar_tensor_tensor(out=qkv.t[:, cc, :], in0=qkv.t[:, cc, :], scalar=sc_, in1=rst.t[:],
                                                                 op0=ALU.mult, op1=ALU.mult),
                         reads=[qkv.bs[cc], rst.b], writes=[qkv.bs[cc]])
            for j4 in range(4 if estop >= 2 else 0):
                t = ch * 4 + j4
                tl_ = slice(j4 * 128, (j4 + 1) * 128)
                pa = k.ps[0]
                for hb in range(2):
                    for c in range(8):
                        S.op("pe", lambda e: e.matmul(pa.t[:, hb * 512:(hb + 1) * 512], lhsT=xc.t[:, c, tl_], rhs=w_in_b.t[:, c, hb * 512:(hb + 1) * 512],
                                                      start=(c == 0), stop=(c == 7)),
                             reads=[xc.b, w_in_b.b], writes=[pa.bs[hb]], inc=(c == 7))
                S.op("act", lambda e: e.activation(out=u_sb.t[:], in_=pa.t[:, 0:512], func=AF.Gelu), reads=[pa.bs[0]], writes=[u_sb.b])
                S.op("act", lambda e: e.activation(out=vg.t[:], in_=pa.t[:, 512:1024], func=AF.Gelu), reads=[pa.bs[1]], writes=[vg.b])
                for g in range(4):
                    S.op("dve", lambda e: e.bn_stats(out=sm2.t[:, g * 6:(g + 1) * 6], in_=vg.t[:, g * 128:(g + 1) * 128]), reads=[vg.b], writes=[s2[g]])
                    S.op("dve", lambda e: e.bn_aggr(out=sm2.t[:, 24 + 2 * g:26 + 2 * g], in_=sm2.t[:, g * 6:(g + 1) * 6]), reads=[s2[g]], writes=[s2[4 + g]])
                    S.op("dve", lambda e: e.tensor_scalar(out=sm2.t[:, 32 + g:33 + g], in0=sm2.t[:, 25 + 2 * g:26 + 2 * g], scalar1=LN_EPS, scalar2=None,
                                                          op0=ALU.add), reads=[s2[4 + g]], writes=[s2[8]])
                S.op("act", lambda e: e.activation(out=sm2.t[:, 36:40], in_=sm2.t[:, 32:36], func=AF.Sqrt), reads=[s2[8]], writes=[s2[9]])
                S.op("dve", lambda e: e.reciprocal(out=sm2.t[:, 40:44], in_=sm2.t[:, 36:40]), reads=[s2[9]], writes=[s2[9]])
                for g in range(4):
                    S.op("dve", lambda e: e.tensor_scalar(out=vg.t[:, g * 128:(g + 1) * 128], in0=vg.t[:, g * 128:(g + 1) * 128],
                                                          scalar1=sm2.t[:, 24 + 2 * g:25 + 2 * g], scalar2=sm2.t[:, 40 + g:41 + g],
                                                          op0=ALU.subtract, op1=ALU.mult),
                         reads=[vg.b, s2[4 + g], s2[9]], writes=[vg.b])
                S.op("pool", lambda e: e.tensor_tensor(out=vg.t[:], in0=vg.t[:], in1=sg_bc.t[:], op=ALU.mult), reads=[vg.b, sg_bc.b], writes=[vg.b])
                S.op("pool", lambda e: e.tensor_tensor(out=vnb.t[:], in0=vg.t[:], in1=sb_bc.t[:], op=ALU.add), reads=[vg.b, sb_bc.b], writes=[vnb.b])
                pm = k.ps[1]
                for g in range(4):
                    S.op("pe", lambda e: e.matmul(pm.t[:, g * 128:(g + 1) * 128], lhsT=wsT.t[:, g, :], rhs=vnb.t[:, g * 128:(g + 1) * 128],
                                                  start=True, stop=True),
                         reads=[wsT.b, vnb.b], writes=[pm.bs[0]], inc=(g == 3))
                mb = mixt.bs[0]
                for g in range(4):
                    S.op("dve", lambda e: e.scalar_tensor_tensor(out=mixt.t[:, g * 128:(g + 1) * 128], in0=pm.t[:, g * 128:(g + 1) * 128],
                                                                 scalar=bsT.t[:, g:g + 1], in1=u_sb.t[:, g * 128:(g + 1) * 128],
                                                                 op0=ALU.add, op1=ALU.mult),
                         reads=[pm.bs[0], bsT.b, u_sb.b], writes=[mb])
                if estop < 3:
                    continue
                for c in range(8):
                    S.op("pe", lambda e: e.matmul(pm.t[:, 512:1024], lhsT=xc.t[:, c, tl_], rhs=w_in_b.t[:, c, 2560:3072], start=(c == 0), stop=(c == 7)),
                         reads=[xc.b, w_in_b.b], writes=[pm.bs[1]], inc=(c == 7))
                S.op("act", lambda e: e.activation(out=sz.t[:], in_=pm.t[:, 512:1024], func=AF.Silu), reads=[pm.bs[1]], writes=[sz.b])
                pab, pab_b = slot()
                for c in range(8):
                    S.op("pe", lambda e: e.matmul(pab[:, 0:8], lhsT=xc.t[:, c, tl_], rhs=w_in_b.t[:, c, 3072:3080], start=(c == 0), stop=(c == 7)),
                         reads=[xc.b, w_in_b.b], writes=[pab_b], inc=(c == 7))
                S.op("act", lambda e: e.activation(out=sm2.t[:, 48:52], in_=pab[:, 4:8], func=AF.Sigmoid), reads=[pab_b], writes=[s2[10]])
                S.op("dve", lambda e: e.tensor_scalar(out=sm2.t[:, 80:84], in0=sm2.t[:, 48:52], scalar1=-1.0, scalar2=None, op0=ALU.mult),
                     reads=[s2[10]], writes=[s2[18]])
                S.op("dve", lambda e: e.tensor_tensor(out=sm2.t[:, 76:80], in0=pab[:, 0:4], in1=dtb.t[:], op=ALU.add), reads=[pab_b, dtb.b], writes=[s2[11]])
                S.op("act", lambda e: e.activation(out=sm2.t[:, 76:80], in_=sm2.t[:, 76:80], func=AF.Exp), reads=[s2[11]], writes=[s2[11]])
                S.op("dve", lambda e: e.tensor_scalar(out=sm2.t[:, 76:80], in0=sm2.t[:, 76:80], scalar1=1.0, scalar2=None, op0=ALU.add),
                     reads=[s2[11]], writes=[s2[11]])
                S.op("act", lambda e: e.activation(out=sm2.t[:, 76:80], in_=sm2.t[:, 76:80], func=AF.Ln), reads=[s2[11]], writes=[s2[11]])
                S.op("dve", lambda e: e.tensor_tensor(out=sm2.t[:, 52:56], in0=sm2.t[:, 76:80], in1=negA.t[:], op=ALU.mult), reads=[s2[11], negA.b], writes=[s2[12]])
                pg, pg_b = slot()
                mm(pg[:, 0:4], pg_b, tri.t[:], tri.b, sm2.t[:, 52:56], s2[12])
                mm(pg[:, 4:8], pg_b, ones_f.t[:], ones_f.b, sm2.t[:, 52:56], s2[12])
                S.op("dve", lambda e: e.tensor_copy(out=sm2.t[:, 56:64], in_=pg[:, 0:8]), reads=[pg_b], writes=[s2[13]])
                S.op("act", lambda e: e.activation(out=sm2.t[:, 64:68], in_=sm2.t[:, 60:64], func=AF.Exp), reads=[s2[13]], writes=[s2[14]])
                S.op("act", lambda e: e.activation(out=sm2.t[:, 68:72], in_=sm2.t[:, 56:60], func=AF.Exp), reads=[s2[13]], writes=[s2[15]])
                S.op("dve", lambda e: e.tensor_tensor(out=sm2.t[:, 68:72], in0=sm2.t[:, 68:72], in1=sm2.t[:, 48:52], op=ALU.mult), reads=[s2[15], s2[10]], writes=[s2[15]])
                S.op("dve", lambda e: e.tensor_tensor(out=sm2.t[:, 72:76], in0=sm2.t[:, 60:64], in1=sm2.t[:, 56:60], op=ALU.subtract), reads=[s2[13]], writes=[s2[16]])
                S.op("act", lambda e: e.activation(out=sm2.t[:, 72:76], in_=sm2.t[:, 72:76], func=AF.Exp), reads=[s2[16]], writes=[s2[16]])

                if estop < 4:
                    continue
                for h in range(4):
                    qT = qkv.t[:, h, tl_]
                    kT = qkv.t[:, 4 + h, tl_]
                    vT = qkv.t[:, 8 + h, tl_]
                    qb, kb_, vb_ = qkv.bs[h], qkv.bs[4 + h], qkv.bs[8 + h]
                    col = lambda a: sm2.t[:, a + h:a + h + 1]
                    pk, pk_b = slot()
                    mm(pk, pk_b, kT, kb_, ident.t[:], ident.b)
                    pv, pv_b = slot()
                    mm(pv, pv_b, vT, vb_, ident.t[:], ident.b)
                    if estop <= 4.05:
                        continue
                    S.op("dve", lambda e: e.tensor_scalar(out=tn["kbg"].t[:], in0=pk, scalar1=col(68), scalar2=None, op0=ALU.mult),
                         reads=[pk_b, s2[15]], writes=[tn["kbg"].b])
                    S.op("act", lambda e: e.activation(out=tn["kdec"].t[:], in_=pk, func=AF.Copy, scale=col(72)),
                         reads=[pk_b, s2[16]], writes=[tn["kdec"].b])
                    S.op("dve", lambda e: e.tensor_scalar(out=tn["vb"].t[:], in0=pv, scalar1=col(48), scalar2=None, op0=ALU.mult),
                         reads=[pv_b, s2[10]], writes=[tn["vb"].b])
                    if estop <= 4.1:
                        continue
                    S.op("pool", lambda e: e.tensor_scalar(out=tn["gB"].t[:], in0=ones_f.t[:], scalar1=col(52), scalar2=None, op0=ALU.mult),
                         reads=[ones_f.b, s2[12]], writes=[tn["gB"].b])
                    pr, pr_b = slot()
                    mm(pr, pr_b, tn["gB"].t[:], tn["gB"].b, tri.t[:], tri.b)
                    S.op("dve", lambda e: e.tensor_scalar(out=tn["t1"].t[:], in0=pr, scalar1=col(56), scalar2=0.0, op0=ALU.subtract, op1=ALU.max),
                         reads=[pr_b, s2[13]], writes=[tn["t1"].b])
                    S.op("act", lambda e: e.activation(out=tn["t1"].t[:], in_=tn["t1"].t[:], func=AF.Exp, scale=-1.0), reads=[tn["t1"].b], writes=[tn["t1"].b])
                    S.op("pool", lambda e: e.tensor_tensor(out=tn["Dn"].t[:], in0=tn["t1"].t[:], in1=maskL.t[:], op=ALU.mult),
                         reads=[tn["t1"].b, maskL.b], writes=[tn["Dn"].b])
                    S.op("dve", lambda e: e.tensor_scalar(out=tn["t2"].t[:], in0=pr, scalar1=col(56), scalar2=0.0, op0=ALU.subtract, op1=ALU.min),
                         reads=[pr_b, s2[13]], writes=[tn["t2"].b])
                    S.op("act", lambda e: e.activation(out=tn["t2"].t[:], in_=tn["t2"].t[:], func=AF.Exp), reads=[tn["t2"].b], writes=[tn["t2"].b])
                    S.op("pool", lambda e: e.tensor_tensor(out=tn["DT"].t[:], in0=tn["t2"].t[:], in1=tri.t[:], op=ALU.mult),
                         reads=[tn["t2"].b, tri.b], writes=[tn["DT"].b])
                    S.op("act", lambda e: e.activation(out=tn["egr"].t[:], in_=pr, func=AF.Exp), reads=[pr_b], writes=[tn["egr"].b])
                    S.op("pool", lambda e: e.tensor_tensor(out=tn["qdT"].t[:], in0=qT, in1=tn["egr"].t[:], op=ALU.mult),
                         reads=[qb, tn["egr"].b], writes=[tn["qdT"].b])
                    if estop <= 4.2:
                        continue
                    pkk, pkk_b = slot()
                    mm(pkk, pkk_b, kT, kb_, kT, kb_)
                    S.op("dve", lambda e: e.scalar_tensor_tensor(out=tn["M0"].t[:], in0=pkk, scalar=col(80), in1=tn["Dn"].t[:], op0=ALU.mult, op1=ALU.mult),
                         reads=[pkk_b, s2[18], tn["Dn"].b], writes=[tn["M0"].b])
                    pn_, pn_b = slot()
                    mm(pn_, pn_b, tn["M0"].t[:], tn["M0"].b, ident.t[:], ident.b)
                    S.op("act", lambda e: e.copy(out=tn["N0"].t[:], in_=pn_), reads=[pn_b], writes=[tn["N0"].b])
                    S.op("dve", lambda e: e.tensor_tensor(out=tn["P0"].t[:], in0=pn_, in1=ident.t[:], op=ALU.add), reads=[pn_b, ident.b], writes=[tn["P0"].b])
                    pqk, pqk_b = slot()
                    mm(pqk, pqk_b, kT, kb_, qT, qb)
                    S.op("dve", lambda e: e.tensor_tensor(out=tn["intraT"].t[:], in0=pqk, in1=tn["DT"].t[:], op=ALU.mult),
                         reads=[pqk_b, tn["DT"].b], writes=[tn["intraT"].b])
                    if estop <= 4.3:
                        continue
                    cm, cn_, cp = "M0", "N0", "P0"
                    for s_ in range(6):
                        nm_, nn_, np_ = ("M1", "N1", "P1") if cm == "M0" else ("M0", "N0", "P0")
                        p1, p1_b = slot()
                        mm(p1, p1_b, tn[cn_].t[:], tn[cn_].b, tn[cm].t[:], tn[cm].b)
                        S.op("act", lambda e: e.copy(out=tn[nm_].t[:], in_=p1), reads=[p1_b], writes=[tn[nm_].b])
                        if s_ < 5:
                            p2, p2_b = slot()
                            mm(p2, p2_b, tn[cm].t[:], tn[cm].b, tn[cn_].t[:], tn[cn_].b)
                            S.op("dve", lambda e: e.tensor_copy(out=tn[nn_].t[:], in_=p2), reads=[p2_b], writes=[tn[nn_].b])
                        p3, p3_b = slot()
                        mm(p3, p3_b, tn[nm_].t[:], tn[nm_].b, tn[cp].t[:], tn[cp].b)
                        S.op("dve", lambda e: e.tensor_tensor(out=tn[np_].t[:], in0=p3, in1=tn[cp].t[:], op=ALU.add),
                             reads=[p3_b, tn[cp].b], writes=[tn[np_].b])
                        cm, cn_, cp = nm_, nn_, np_
                    if estop <= 4.4:
                        continue
                    TT = tn[cp]
                    pu_, pu_b = slot()
                    mm(pu_, pu_b, TT.t[:], TT.b, tn["vb"].t[:], tn["vb"].b)
                    S.op("act", lambda e: e.copy(out=tn["u"].t[:], in_=pu_), reads=[pu_b], writes=[tn["u"].b])
                    pw, pw_b = slot()
                    mm(pw, pw_b, tn["kbg"].t[:], tn["kbg"].b, TT.t[:], TT.b)
                    S.op("act", lambda e: e.copy(out=tn["wT"].t[:], in_=pw), reads=[pw_b], writes=[tn["wT"].b])
                    if estop <= 4.5:
                        continue
                    Sh = St[h]
                    pvn, pvn_b = slot()
                    mm(pvn, pvn_b, tn["wT"].t[:], tn["wT"].b, Sh.t[:], Sh.b)
                    S.op("dve", lambda e: e.tensor_tensor(out=tn["vnew"].t[:], in0=tn["u"].t[:], in1=pvn, op=ALU.subtract),
                         reads=[tn["u"].b, pvn_b], writes=[tn["vnew"].b])
                    po_, po_b = slot()
                    mm(po_, po_b, tn["qdT"].t[:], tn["qdT"].b, Sh.t[:], Sh.b, start=True, stop=False)
                    mm(po_, po_b, tn["intraT"].t[:], tn["intraT"].b, tn["vnew"].t[:], tn["vnew"].b, start=False, stop=True)
                    pS, pS_b = slot()
                    mm(pS, pS_b, tn["kdec"].t[:], tn["kdec"].b, tn["vnew"].t[:], tn["vnew"].b)
                    S.op("dve", lambda e: e.scalar_tensor_tensor(out=Sh.t[:], in0=Sh.t[:], scalar=col(64), in1=pS, op0=ALU.mult, op1=ALU.add),
                         reads=[Sh.b, s2[14], pS_b], writes=[Sh.b])
                    if estop <= 4.6:
                        continue
                    S.op("act", lambda e: e.activation(out=tn["junk"].t[:], in_=po_, func=AF.Square, accum_out=sm2.t[:, 84 + h:85 + h]),
                         reads=[po_b], writes=[tn["junk"].b, s2[19]])
                    S.op("dve", lambda e: e.tensor_scalar(out=sm2.t[:, 88 + h:89 + h], in0=sm2.t[:, 84 + h:85 + h], scalar1=1.0 / 128, scalar2=RMS_EPS,
                                                          op0=ALU.mult, op1=ALU.add), reads=[s2[19]], writes=[s2[20]])
                    S.op("act", lambda e: e.activation(out=sm2.t[:, 88 + h:89 + h], in_=sm2.t[:, 88 + h:89 + h], func=AF.Sqrt), reads=[s2[20]], writes=[s2[20]])
                    S.op("dve", lambda e: e.reciprocal(out=sm2.t[:, 92 + h:93 + h], in_=sm2.t[:, 88 + h:89 + h]), reads=[s2[20]], writes=[s2[21]])
                    S.op("dve", lambda e: e.scalar_tensor_tensor(out=tn["yb"].t[:], in0=po_, scalar=sm2.t[:, 92 + h:93 + h], in1=gnw.t[:],
                                                                 op0=ALU.mult, op1=ALU.mult),
                         reads=[po_b, s2[21], gnw.b], writes=[tn["yb"].b])
                    S.op("pool", lambda e: e.tensor_tensor(out=mixt.t[:, 512 + h * 128:512 + (h + 1) * 128], in0=tn["yb"].t[:],
                                                           in1=sz.t[:, h * 128:(h + 1) * 128], op=ALU.mult),
                         reads=[tn["yb"].b, sz.b], writes=[mb])
                if estop < 5:
                    continue
                pT = k.ps[3]
                xTt = k.xTt[k.rr]
                k.rr ^= 1
                for c in range(8):
                    S.op("pe", lambda e: e.transpose(out=pT.t[:, c * 128:(c + 1) * 128], in_=mixt.t[:, c * 128:(c + 1) * 128], identity=ident.t[:]),
                         reads=[mb, ident.b], writes=[pT.bs[c // 4]], inc=(c % 4 == 3))
                S.op("act", lambda e: e.copy(out=xTt.t[:].rearrange("p c t -> p (c t)"), in_=pT.t[:]), reads=pT.bs, writes=[xTt.b])
                po2 = k.ps[0]
                for hb in range(2):
                    for c in range(8):
                        S.op("pe", lambda e: e.matmul(po2.t[:, hb * 512:(hb + 1) * 512], lhsT=xTt.t[:, c, :], rhs=w_out_b.t[:, c, hb * 512:(hb + 1) * 512],
                                                      start=(c == 0), stop=(c == 7)),
                             reads=[xTt.b, w_out_b.b], writes=[po2.bs[hb]], inc=(c == 7))
                epilogue_defer.append((t, po2))
                epilogue_now(k, t, work, lng, lnb, small, sm_b, po2)
        S.barrier()
    k.xcur = k.xres
    k.xcur_is_in = False


epilogue_defer = []


def epilogue_now(k, t, work, lng, lnb, small, sm_b, po2):
    epilogue(k, t, work, lng, lnb, small, sm_b, False, src=po2.t[:], src_bufs=po2.bs)


CONSTS = None
LAST_INPUTS = set()


def layer_inputs(inputs, b):
    m = {"x": np.ascontiguousarray(inputs["x"][b]), "ident": np.eye(128, dtype=np.float32)}
    inv_freq = (np.float32(10000.0) ** (-np.arange(0, 64, 2, dtype=np.float32) / np.float32(64))).astype(np.float32)
    ang = (np.arange(SEQ, dtype=np.float32)[:, None] * inv_freq[None, :]).astype(np.float32)
    cos2T = np.zeros((128, SEQ), np.float32)
    sin2T = np.zeros((128, SEQ), np.float32)
    cos2T[0:32] = np.cos(ang).T
    cos2T[32:64] = np.cos(ang).T
    sin2T[0:32] = np.sin(ang).T
    sin2T[32:64] = np.sin(ang).T
    m["cos2T"] = cos2T
    m["sin2T"] = sin2T
    m["tri"] = np.triu(np.ones((128, 128), np.float32))
    m["maskL"] = np.tril(np.ones((128, 128), np.float32), -1)
    for i in range(2):
        m["even_w_in_%d" % i] = inputs["even_w_in"][i]
        m["even_w_out_%d" % i] = inputs["even_w_out"][i]
        m["even_sgu_wT_%d" % i] = np.ascontiguousarray(np.transpose(inputs["even_sgu_w"][i], (2, 0, 1)))
        m["even_sgu_bT_%d" % i] = np.ascontiguousarray(inputs["even_sgu_b"][i].T)
        m["even_conv_wT_%d" % i] = np.ascontiguousarray(inputs["even_conv_w"][i].T.reshape(12, 128, 4).transpose(1, 0, 2))
        for nm in ("even_sgu_ln_g", "even_sgu_ln_b", "even_a_log", "even_dt_bias", "even_gdn_norm"):
            m["%s_%d" % (nm, i)] = inputs[nm][i][None, :]
    for i in range(2):
        for nm in ("odd_w_in", "odd_w_uq", "odd_w_ukv", "odd_w_out"):
            m["%s_%d" % (nm, i)] = inputs[nm][i]
        for nm in ("odd_q_norm", "odd_kv_norm"):
            m["%s_%d" % (nm, i)] = inputs[nm][i][None, :]
    for l in range(DEPTH):
        for nm in ("moe_w_router", "moe_w_gate_up", "moe_b_gate_up", "moe_w_down", "moe_b_down"):
            m["%s_%d" % (nm, l)] = inputs[nm][l]
        for nm in ("moe_b_router", "ln_ffn_g", "ln_ffn_b", "ln_mix_g", "ln_mix_b"):
            m["%s_%d" % (nm, l)] = inputs[nm][l][None, :]
    return m


def make_consts():
    return {"ident": np.eye(128, dtype=np.float32)}


def kernel(**inputs):
    phases = [("prep",)]
    for l in range(DEPTH):
        phases.append(("even", l) if l % 2 == 0 else ("odd", l))
        phases.append(("moe", l, l == DEPTH - 1))
    nc = build(phases)
    names = set(LAST_INPUTS)
    inputs = {k_: np.asarray(v) for k_, v in inputs.items()}
    n_cores = inputs["x"].shape[0]
    in_maps = []
    for b_ in range(n_cores):
        m = layer_inputs(inputs, b_)
        in_maps.append({k_: np.ascontiguousarray(v, dtype=np.float32) for k_, v in m.items() if k_ in names})
    res = run_bass_kernel_spmd(nc, in_maps, core_ids=list(range(n_cores)))
    return np.stack([np.asarray(r["y"], dtype=np.float32) for r in res.results], axis=0)
```

```python
import contextlib
import numpy as np
import concourse.bass as bass
import concourse.mybir as mybir
from concourse.bass_utils import run_bass_kernel_spmd

F32 = mybir.dt.float32
BF16 = mybir.dt.bfloat16
AF = mybir.ActivationFunctionType
ALU = mybir.AluOpType
AX = mybir.AxisListType

D = 1024
SEQ = 4096
NT = SEQ // 128
DEPTH = 4
NE = 32
ALPHA = (2 * DEPTH) ** 0.25
LN_EPS = 1e-5
TC = 1024
NCH = SEQ // TC
TPC = TC // 128
N_WD = 1
SILU_CAP = 11.913920224372246


class Buf:
    __slots__ = ("w", "r", "excl")

    def __init__(self, excl=False):
        self.w = None
        self.r = {}
        self.excl = excl


class Sched:
    N_DMA_SEMS = 16

    def __init__(self, nc, stack):
        self.nc = nc
        self.eng = {"pe": nc.tensor, "dve": nc.vector, "act": nc.scalar,
                    "pool": nc.gpsimd, "sp": nc.sync}
        self.sems = {}
        self.cnt = {}
        for e in self.eng:
            self.sems[e] = stack.enter_context(nc.semaphore("s_" + e))
            self.cnt[e] = 0
        for i in range(self.N_DMA_SEMS):
            k = ("d", i)
            self.sems[k] = stack.enter_context(nc.semaphore("s_d%d" % i))
            self.cnt[k] = 0
        self.seen = {e: {} for e in self.eng}
        self.dma_rr = 0
        self.n_ins = 0

    def _waits(self, e, reads, writes, extra=()):
        waits = {}

        def add(st):
            if st is None:
                return
            k, v = st
            if e == "pe" and k == "pe":
                return
            assert not (k == e and v > self.cnt[e]), "self-deadlock"
            if v > waits.get(k, 0):
                waits[k] = v
        for b in reads:
            add(b.w)
            if b.excl:
                for k, v in b.r.items():
                    if k != e:
                        add((k, v))
        for b in writes:
            add(b.w)
            for k, v in b.r.items():
                add((k, v))
        for st in extra:
            add(st)
        eng = self.eng[e]
        seen = self.seen[e]
        for k, v in waits.items():
            if seen.get(k, 0) >= v:
                continue
            seen[k] = v
            eng.wait_ge(self.sems[k], v)

    def _stamp(self, stamp, reads, writes):
        k, v = stamp
        for b in reads:
            if b.r.get(k, 0) < v:
                b.r[k] = v
        for b in writes:
            b.w = stamp
            b.r = {}

    def op(self, e, fn, reads=(), writes=(), inc=True):
        self._waits(e, reads, writes)
        ins = fn(self.eng[e])
        self.n_ins += 1
        if inc:
            self.cnt[e] += 1
            ins.then_inc(self.sems[e], 1)
            stamp = (e, self.cnt[e])
        else:
            stamp = (e, self.cnt[e] + 1)
        self._stamp(stamp, reads, writes)
        return ins

    def dma(self, out, in_, reads=(), writes=(), q="sp", **kw):
        i = self.dma_rr
        self.dma_rr = (self.dma_rr + 1) % self.N_DMA_SEMS
        k = ("d", i)
        extra = [(k, self.cnt[k])] if self.cnt[k] else []
        self._waits(q, reads, writes, extra)
        ins = self.eng[q].dma_start(out=out, in_=in_, **kw)
        self.n_ins += 1
        self.cnt[k] += 16
        ins.then_inc(self.sems[k], 16)
        self._stamp((k, self.cnt[k]), reads, writes)
        return ins

    def finish(self, bufs):
        self._waits("sp", bufs, ())

    def barrier(self):
        stamps = [(k, v) for k, v in self.cnt.items() if v > 0]
        for e in self.eng:
            self._waits(e, (), (), stamps)


class T:
    def __init__(self, t, nb=1, excl=False):
        self.t = t
        self.b = Buf(excl)
        self.bs = [Buf(excl) for _ in range(nb)]


class K:
    pass


def build(phases, n_layers=DEPTH, dbg=None, nc=None, ext_ins=None, ext_out=None):
    if nc is None:
        nc = bass.Bass("TRN2", target_bir_lowering=False)
    k = K()
    k.nc = nc
    k.dbg = dbg

    def din(name, shape, dt=F32):
        if ext_ins is not None:
            return ext_ins[name]
        return nc.dram_tensor(name, list(shape), dt, kind="ExternalInput").ap()

    I = {}

    def inp(name, shape, dt=F32):
        if name not in I:
            I[name] = din(name, shape, dt)
        return I[name]
    k.inp = inp
    inp("x", [SEQ, D])
    inp("ident", [128, 128])
    k.I = I
    k.y = ext_out if ext_out is not None else nc.dram_tensor("y", [SEQ, D], F32, kind="ExternalOutput").ap()
    k.xres = nc.dram_tensor("xres", [SEQ, D], F32, kind="Internal").ap()
    k.xT_d = nc.dram_tensor("xT_d", [8, 128, SEQ], BF16, kind="Internal").ap()
    k.xres_b = [Buf() for _ in range(NT)]
    k.xT_b = [Buf() for _ in range(NT)]
    k.y_b = [Buf() for _ in range(NT)]
    k.xcur = I["x"]
    k.xcur_is_in = True

    with contextlib.ExitStack() as st:
        S = Sched(nc, st)
        k.S = S
        k.st = st

        def sb(name, shape, dt=F32, nb=1):
            return T(st.enter_context(nc.sbuf_tensor("sb_" + name, list(shape), dt)), nb)
        k.sb = sb
        k.ps = [T(st.enter_context(nc.psum_tensor("ps%d" % i, [128, 1024], F32)), 2, excl=True)
                for i in range(4)]
        k.ident = sb("ident", [128, 128])
        S.dma(k.ident.t[:], I["ident"], writes=[k.ident.b])
        k.ones = sb("ones", [1, 128])
        S.op("dve", lambda e: e.memset(k.ones.t[:], 1.0), writes=[k.ones.b])
        k.xt = [sb("xt%d" % i, [128, D]) for i in range(2)]
        k.xTt = [sb("xTt%d" % i, [128, 8, 128], BF16) for i in range(2)]
        k.rr = 0

        for ph in phases:
            if ph[0] == "prep":
                prep_phase(k)
            elif ph[0] == "moe":
                moe_phase(k, ph[1], final=ph[2])
            elif ph[0] == "odd":
                odd_phase(k, ph[1])
            elif ph[0] == "even":
                even_phase(k, ph[1])
            elif ph[0] == "dump":
                for t in range(NT):
                    xt = k.xt[t % 2]
                    S.dma(xt.t[:], k.xcur[t * 128:(t + 1) * 128, :], reads=[k.xres_b[t]], writes=[xt.b])
                    S.dma(k.y[t * 128:(t + 1) * 128, :], xt.t[:], reads=[xt.b], writes=[k.y_b[t]])
        S.finish(k.y_b)
        S.barrier()
        global LAST_INPUTS
        LAST_INPUTS = set(I.keys())
        print("instructions:", S.n_ins, "counts:", {kk: v for kk, v in S.cnt.items() if not isinstance(kk, tuple)})
    return nc


def emit_xT(k, t, src, src_b):
    S = k.S
    j = k.rr
    k.rr ^= 1
    ps = k.ps[3]
    for c in range(8):
        S.op("pe", lambda e, c=c: e.transpose(out=ps.t[:, c * 128:(c + 1) * 128],
                                              in_=src[:, c * 128:(c + 1) * 128],
                                              identity=k.ident.t[:]),
             reads=[src_b, k.ident.b], writes=[ps.bs[c // 4]], inc=(c % 4 == 3))
    xTt = k.xTt[j]
    S.op("act", lambda e: e.copy(out=xTt.t[:].rearrange("p c t -> p (c t)"), in_=ps.t[:]),
         reads=ps.bs, writes=[xTt.b])
    S.dma(k.xT_d.rearrange("c p t -> p c t")[:, :, t * 128:(t + 1) * 128], xTt.t[:],
          reads=[xTt.b], writes=[k.xT_b[t]])


def prep_phase(k):
    S = k.S
    for t in range(NT):
        xt = k.xt[t % 2]
        S.dma(xt.t[:], k.xcur[t * 128:(t + 1) * 128, :], writes=[xt.b])
        emit_xT(k, t, xt.t, xt.b)


def moe_phase(k, l, final=False):
    S, nc, sb = k.S, k.nc, k.sb
    I = {}
    I["moe_w_router"] = k.inp("moe_w_router_%d" % l, [D, NE])
    I["moe_b_router"] = k.inp("moe_b_router_%d" % l, [1, NE])
    I["moe_w_gate_up"] = k.inp("moe_w_gate_up_%d" % l, [NE, D, 2 * D])
    I["moe_b_gate_up"] = k.inp("moe_b_gate_up_%d" % l, [NE, 2 * D])
    I["moe_w_down"] = k.inp("moe_w_down_%d" % l, [NE, D, D])
    I["moe_b_down"] = k.inp("moe_b_down_%d" % l, [NE, D])
    I["ln_ffn_g"] = k.inp("ln_ffn_g_%d" % l, [1, D])
    I["ln_ffn_b"] = k.inp("ln_ffn_b_%d" % l, [1, D])
    n_ch = (k.dbg or {}).get("n_ch", NCH)
    n_e = (k.dbg or {}).get("n_e", NE)
    with contextlib.ExitStack() as st:
        def sb(name, shape, dt=F32, nb=1):
            return T(st.enter_context(nc.sbuf_tensor("sbm%d_" % l + name, list(shape), dt)), nb)
        wr_f = sb("wr_f", [128, 8, NE])
        wr_b = sb("wr_b", [128, 8, NE], BF16)
        S.dma(wr_f.t[:], I["moe_w_router"].rearrange("(c p) e -> p c e", p=128), writes=[wr_f.b])
        S.op("dve", lambda e: e.tensor_copy(out=wr_b.t[:], in_=wr_f.t[:]), reads=[wr_f.b], writes=[wr_b.b])
        br = sb("br", [128, NE])
        S.dma(br.t[:], I["moe_b_router"].partition_broadcast(128), writes=[br.b])
        stg = [sb("stg%d" % i, [128, 2 * D]) for i in range(2)]
        bgu = T(stg[0].t[0:NE, :])
        bgu.b = stg[0].b
        S.dma(bgu.t[:], I["moe_b_gate_up"], writes=[bgu.b])
        bguT = sb("bguT", [128, 16, NE])
        ps = k.ps[3]
        for two in range(2):
            for m in range(8):
                i = two * 8 + m
                S.op("pe", lambda e, two=two, m=m, i=i: e.transpose(
                    out=ps.t[:, i * NE:(i + 1) * NE],
                    in_=bgu.t[:, 2 * m * 128 + two:2 * (m + 1) * 128:2],
                    identity=k.ident.t[0:NE, 0:NE]),
                    reads=[bgu.b, k.ident.b], writes=[ps.bs[0]], inc=(i == 15))
        S.op("dve", lambda e: e.tensor_copy(out=bguT.t[:].rearrange("p a b -> p (a b)"), in_=ps.t[:, 0:16 * NE]),
             reads=[ps.bs[0]], writes=[bguT.b])
        bgu2 = sb("bgu2", [128, 16, NE])
        S.op("dve", lambda e: e.tensor_scalar(out=bgu2.t[:, 0:8, :], in0=bguT.t[:, 0:8, :], scalar1=1.702, scalar2=None, op0=ALU.mult),
             reads=[bguT.b], writes=[bgu2.b])
        S.op("dve", lambda e: e.tensor_scalar(out=bgu2.t[:, 8:16, :], in0=bguT.t[:, 8:16, :], scalar1=1.0, scalar2=None, op0=ALU.add),
             reads=[bguT.b], writes=[bgu2.b])
        bd = sb("bd", [128, D])
        S.op("pool", lambda e: e.memset(bd.t[:], 0.0), writes=[bd.b])
        S.dma(bd.t[0:NE, :], I["moe_b_down"], writes=[bd.b])
        lng = sb("lng", [128, D])
        lnb = sb("lnb", [128, D])
        S.dma(lng.t[:], I["ln_ffn_g"].partition_broadcast(128), writes=[lng.b])
        S.dma(lnb.t[:], I["ln_ffn_b"].partition_broadcast(128), writes=[lnb.b])

        stop = (k.dbg or {}).get("stop", 99)
        if stop <= 1:
            S.barrier()
            return
        xTs = sb("xTs", [128, 8, TC], BF16)
        acc = [sb("acc%d" % i, [128, D]) for i in range(TPC)]
        gate = sb("gate", [128, TPC, NE], nb=TPC)
        wgu = [sb("wgu%d" % i, [128, 8, 2 * D], BF16, nb=8) for i in range(2)]
        wd = [sb("wd%d" % i, [128, 8, D], BF16, nb=4) for i in range(N_WD)]
        actb = [sb("actb%d" % i, [128, 8, 512], BF16, nb=8) for i in range(2)]
        tsl = [sb("tsl%d" % i, [128, 512]) for i in range(2)]
        tlc2 = [sb("tlc2%d" % i, [128, 512]) for i in range(2)]
        tslc = [sb("tslc%d" % i, [128, 512]) for i in range(2)]
        pair_i = [0]
        small = sb("small", [128, 64])
        sm_b = [Buf() for _ in range(8)]
        gT = sb("gT", [128, 128])
        S.op("pool", lambda e: e.memset(gT.t[:], 0.0), writes=[gT.b])
        lg = sb("lg", [128, NE])
        lm = sb("lm", [128, NE])

        wgu_src = I["moe_w_gate_up"]
        wd_src = I["moe_w_down"]
        stg_rr = [0]

        def prep_steps(e, wg, wdd):
            steps = []
            for c in range(8):
                def f(c=c):
                    s_ = stg[stg_rr[0]]
                    stg_rr[0] ^= 1
                    S.dma(s_.t[:], wgu_src[e, c * 128:(c + 1) * 128, :], writes=[s_.b])
                    S.op("dve", lambda en: en.tensor_copy(out=wg.t[:, c, 0:D], in_=s_.t[:, 0:2 * D:2]),
                         reads=[s_.b], writes=[wg.bs[c]])
                    S.op("act", lambda en: en.copy(out=wg.t[:, c, D:2 * D], in_=s_.t[:, 1:2 * D:2]),
                         reads=[s_.b], writes=[wg.bs[c]])
                steps.append(f)
            for c2 in range(4):
                def f(c2=c2):
                    s_ = stg[stg_rr[0]]
                    stg_rr[0] ^= 1
                    S.dma(s_.t[:].rearrange("p (c n) -> p c n", c=2),
                          wd_src[e, c2 * 256:(c2 + 1) * 256, :].rearrange("(c p) n -> p c n", p=128),
                          writes=[s_.b])
                    S.op("act", lambda en: en.mul(out=wdd.t[:, 2 * c2:2 * c2 + 2, :].rearrange("p c n -> p (c n)"),
                                                  in_=s_.t[:], mul=1.0 / 1.702),
                         reads=[s_.b], writes=[wdd.bs[c2]])
                steps.append(f)
            return steps

        if not (k.dbg or {}).get("skip_prep"):
            for f in prep_steps(0, wgu[0], wd[0]):
                f()

        for ch in range(n_ch):
            t0 = ch * TPC
            S.dma(xTs.t[:], k.xT_d.rearrange("c p t -> p c t")[:, :, ch * TC:(ch + 1) * TC],
                  reads=k.xT_b[t0:t0 + TPC], writes=[xTs.b])
            for tt in range(TPC):
                pr = k.ps[3]
                for c in range(8):
                    S.op("pe", lambda e, c=c, tt=tt: e.matmul(pr.t[:, 0:NE], lhsT=xTs.t[:, c, tt * 128:(tt + 1) * 128],
                                                             rhs=wr_b.t[:, c, :], start=(c == 0), stop=(c == 7)),
                         reads=[xTs.b, wr_b.b], writes=[pr.bs[0]], inc=(c == 7))
                S.op("dve", lambda e: e.tensor_tensor(out=lg.t[:], in0=pr.t[:, 0:NE], in1=br.t[:], op=ALU.add),
                     reads=[pr.bs[0], br.b], writes=[lg.b])
                if stop <= 2.1:
                    continue
                S.op("dve", lambda e: e.max(out=small.t[:, 0:8], in_=lg.t[:]), reads=[lg.b], writes=[sm_b[0]])
                if stop <= 2.2:
                    continue
                S.op("dve", lambda e: e.tensor_scalar(out=lm.t[:], in0=lg.t[:], scalar1=small.t[:, 3:4], scalar2=None,
                                                      op0=ALU.is_ge), reads=[lg.b, sm_b[0]], writes=[lm.b])
                S.op("dve", lambda e: e.tensor_scalar(out=small.t[:, 8:9], in0=small.t[:, 0:1], scalar1=-1.0,
                                                      scalar2=None, op0=ALU.mult), reads=[sm_b[0]], writes=[sm_b[1]])
                S.op("act", lambda e: e.activation(out=lg.t[:], in_=lg.t[:], func=AF.Exp, bias=small.t[:, 8:9], scale=1.0),
                     reads=[lg.b, sm_b[1]], writes=[lg.b])
                S.op("dve", lambda e: e.scalar_tensor_tensor(out=lm.t[:], in0=lg.t[:], scalar=1.0, in1=lm.t[:],
                                                             op0=ALU.mult, op1=ALU.mult, accum_out=small.t[:, 9:10]),
                     reads=[lg.b, lm.b], writes=[lm.b, sm_b[2]])
                S.op("dve", lambda e: e.reciprocal(out=small.t[:, 10:11], in_=small.t[:, 9:10]), reads=[sm_b[2]], writes=[sm_b[3]])
                S.op("dve", lambda e, tt=tt: e.tensor_scalar(out=gate.t[:, tt, :], in0=lm.t[:], scalar1=small.t[:, 10:11],
                                                             scalar2=None, op0=ALU.mult),
                     reads=[lm.b, sm_b[3]], writes=[gate.bs[tt]])
                if stop <= 2.3:
                    continue
                pt = k.ps[3]
                S.op("pe", lambda e, tt=tt: e.transpose(out=pt.t[0:NE, 512:640], in_=gate.t[:, tt, :], identity=k.ident.t[:]),
                     reads=[gate.bs[tt], k.ident.b], writes=[pt.bs[1]])
                S.op("dve", lambda e: e.tensor_copy(out=gT.t[0:NE, :], in_=pt.t[0:NE, 512:640]), reads=[pt.bs[1]], writes=[gT.b])
                if stop <= 2.4:
                    continue
                pa = k.ps[2]
                for h in range(2):
                    S.op("pe", lambda e, h=h: e.matmul(pa.t[:, h * 512:(h + 1) * 512], lhsT=gT.t[:], rhs=bd.t[:, h * 512:(h + 1) * 512],
                                                       start=True, stop=True),
                         reads=[gT.b, bd.b], writes=[pa.bs[h]])
                S.op("act", lambda e, tt=tt: e.copy(out=acc[tt].t[:], in_=pa.t[:]), reads=pa.bs, writes=[acc[tt].b])

            if stop < 3:
                continue
            def emit_gu(e_, sub, steps):
                wg = wgu[e_ % 2]
                ab = actb[sub % 2]
                tok = slice(sub * 512, (sub + 1) * 512)
                si = 0
                for m in range(8):
                    pg = k.ps[m % 2]
                    for half in range(2):
                        for c in range(8):
                            S.op("pe", lambda e: e.matmul(
                                pg.t[:, half * 512:(half + 1) * 512],
                                lhsT=wg.t[:, c, half * D + m * 128: half * D + (m + 1) * 128],
                                rhs=xTs.t[:, c, tok], start=(c == 0), stop=(c == 7)),
                                reads=[wg.bs[c], xTs.b], writes=[pg.bs[half]], inc=(c == 7))
                    bg = bgu2.t[:, m, e_:e_ + 1]
                    bl = bgu2.t[:, 8 + m, e_:e_ + 1]
                    pi_ = pair_i[0] % 2
                    pair_i[0] += 1
                    sl_, lc_, slc_ = tsl[pi_], tlc2[pi_], tslc[pi_]
                    S.op("act", lambda e: e.activation(out=sl_.t[:], in_=pg.t[:, 0:512], func=AF.Silu, bias=bg, scale=1.702),
                         reads=[pg.bs[0], bgu2.b], writes=[sl_.b])
                    S.op("dve", lambda e: e.tensor_scalar(out=lc_.t[:], in0=pg.t[:, 512:1024], scalar1=bl, scalar2=8.0,
                                                          op0=ALU.add, op1=ALU.min),
                         reads=[pg.bs[1], bgu2.b], writes=[lc_.b])
                    S.op("pool", lambda e: e.tensor_scalar(out=slc_.t[:], in0=sl_.t[:], scalar1=SILU_CAP, scalar2=-3.0e38,
                                                           op0=ALU.min, op1=ALU.max),
                         reads=[sl_.b], writes=[slc_.b])
                    S.op("dve", lambda e: e.scalar_tensor_tensor(out=ab.t[:, m, :], in0=lc_.t[:], scalar=-6.0, in1=slc_.t[:],
                                                                 op0=ALU.max, op1=ALU.mult),
                         reads=[lc_.b, slc_.b], writes=[ab.bs[m]])
                    if si < len(steps):
                        steps[si]()
                        si += 1
                while si < len(steps):
                    steps[si]()
                    si += 1

            def emit_down(e_, sub):
                ab = actb[sub % 2]
                wdd = wd[e_ % len(wd)]
                for j in range(4):
                    tt = sub * 4 + j
                    pd = k.ps[2]
                    for h in range(2):
                        for c in range(8):
                            S.op("pe", lambda e: e.matmul(
                                pd.t[:, h * 512:(h + 1) * 512],
                                lhsT=ab.t[:, c, j * 128:(j + 1) * 128],
                                rhs=wdd.t[:, c, h * 512:(h + 1) * 512], start=(c == 0), stop=(c == 7)),
                                reads=[ab.bs[c], wdd.bs[c // 2]], writes=[pd.bs[h]], inc=(c == 7))
                        S.op("dve", lambda e: e.scalar_tensor_tensor(
                            out=acc[tt].t[:, h * 512:(h + 1) * 512], in0=pd.t[:, h * 512:(h + 1) * 512],
                            scalar=gate.t[:, tt, e_:e_ + 1], in1=acc[tt].t[:, h * 512:(h + 1) * 512],
                            op0=ALU.mult, op1=ALU.add),
                            reads=[pd.bs[h], gate.bs[tt], acc[tt].b], writes=[acc[tt].b])

            blocks = [(e_, sub) for e_ in range(n_e) for sub in range(TC // 512)]
            d_for = {}
            last_sub = TC // 512 - 1
            for bi, (e_, sub) in enumerate(blocks):
                steps = []
                if sub == 0:
                    if e_ + 1 < n_e:
                        nx = prep_steps(e_ + 1, wgu[(e_ + 1) % 2], wd[0])
                    elif ch + 1 < n_ch:
                        nx = prep_steps(0, wgu[0], wd[0])
                    else:
                        nx = []
                    steps, d_for[e_ + 1] = nx[:8], nx[8:]
                emit_gu(e_, sub, steps)
                if bi >= 1:
                    pe_, psub = blocks[bi - 1]
                    emit_down(pe_, psub)
                    if psub == last_sub:
                        for f in d_for.pop(pe_ + 1, []):
                            f()
            pe_, psub = blocks[-1]
            emit_down(pe_, psub)
            for f in d_for.pop(pe_ + 1, []):
                f()
            if stop < 4:
                continue
            for tt in range(TPC):
                t = t0 + tt
                epilogue(k, t, acc[tt], lng, lnb, small, sm_b, final)
        S.barrier()
    k.xcur = k.xres
    k.xcur_is_in = False


def epilogue(k, t, acc, lng, lnb, small, sm_b, final, src=None, src_bufs=None):
    S = k.S
    xt = k.xt[t % 2]
    src_b = k.xres_b[t] if not k.xcur_is_in else Buf()
    if src is None:
        src, src_bufs = acc.t[:], [acc.b]
    S.dma(xt.t[:], k.xcur[t * 128:(t + 1) * 128, :], reads=[src_b], writes=[xt.b])
    S.op("dve", lambda e: e.scalar_tensor_tensor(out=acc.t[:], in0=xt.t[:], scalar=ALPHA, in1=src,
                                                 op0=ALU.mult, op1=ALU.add),
         reads=[xt.b] + list(src_bufs), writes=[acc.b])
    S.op("dve", lambda e: e.bn_stats(out=small.t[:, 16:22], in_=acc.t[:, 0:512]), reads=[acc.b], writes=[sm_b[4]])
    S.op("dve", lambda e: e.bn_stats(out=small.t[:, 22:28], in_=acc.t[:, 512:1024]), reads=[acc.b], writes=[sm_b[5]])
    S.op("dve", lambda e: e.bn_aggr(out=small.t[:, 28:30], in_=small.t[:, 16:28]), reads=[sm_b[4], sm_b[5]], writes=[sm_b[6]])
    S.op("dve", lambda e: e.tensor_scalar(out=small.t[:, 30:31], in0=small.t[:, 29:30], scalar1=LN_EPS, scalar2=None, op0=ALU.add),
         reads=[sm_b[6]], writes=[sm_b[7]])
    S.op("act", lambda e: e.activation(out=small.t[:, 31:32], in_=small.t[:, 30:31], func=AF.Sqrt),
         reads=[sm_b[7]], writes=[sm_b[7]])
    S.op("dve", lambda e: e.reciprocal(out=small.t[:, 32:33], in_=small.t[:, 31:32]), reads=[sm_b[7]], writes=[sm_b[7]])
    S.op("dve", lambda e: e.tensor_scalar(out=acc.t[:], in0=acc.t[:], scalar1=small.t[:, 28:29], scalar2=small.t[:, 32:33],
                                          op0=ALU.subtract, op1=ALU.mult),
         reads=[acc.b, sm_b[6], sm_b[7]], writes=[acc.b])
    S.op("pool", lambda e: e.tensor_tensor(out=acc.t[:], in0=acc.t[:], in1=lng.t[:], op=ALU.mult),
         reads=[acc.b, lng.b], writes=[acc.b])
    S.op("pool", lambda e: e.tensor_tensor(out=xt.t[:], in0=acc.t[:], in1=lnb.t[:], op=ALU.add),
         reads=[acc.b, lnb.b], writes=[xt.b])
    if final:
        S.dma(k.y[t * 128:(t + 1) * 128, :], xt.t[:], reads=[xt.b], writes=[k.y_b[t]])
    else:
        S.dma(k.xres[t * 128:(t + 1) * 128, :], xt.t[:], reads=[xt.b], writes=[k.xres_b[t]])
        emit_xT(k, t, xt.t, xt.b)


QL, KVL, ROPE = 384, 256, 64
QK_SCALE = 192.0 ** -0.5
RMS_EPS = 1e-6


def load_cast(k, S, stg, dst_fn, src_ap, width, eng="act"):
    S.dma(stg.t[:, 0:width], src_ap, writes=[stg.b])


def odd_phase(k, l):
    S, nc = k.S, k.nc
    i = l // 2
    w_in = k.inp("odd_w_in_%d" % i, [D, 704])
    q_norm = k.inp("odd_q_norm_%d" % i, [1, QL])
    w_uq = k.inp("odd_w_uq_%d" % i, [QL, 1536])
    kv_norm = k.inp("odd_kv_norm_%d" % i, [1, KVL])
    w_ukv = k.inp("odd_w_ukv_%d" % i, [KVL, 2048])
    w_out = k.inp("odd_w_out_%d" % i, [D, D])
    ln_g = k.inp("ln_mix_g_%d" % l, [1, D])
    ln_b = k.inp("ln_mix_b_%d" % l, [1, D])
    cos2T = k.inp("cos2T", [128, SEQ])
    sin2T = k.inp("sin2T", [128, SEQ])
    oT_d = nc.dram_tensor("oT_d_%d" % l, [8, 128, SEQ], BF16, kind="Internal").ap()
    oT_b = [Buf() for _ in range(8)]
    n_h = (k.dbg or {}).get("n_h", 8)
    with contextlib.ExitStack() as st:
        def sb(name, shape, dt=F32, nb=1):
            return T(st.enter_context(nc.sbuf_tensor("sbo%d_" % l + name, list(shape), dt)), nb)
        w_in_b = sb("w_in_b", [128, 8, 704], BF16)
        wkp = sb("wkp", [128, 8, 128], BF16)
        wkr = sb("wkr", [128, 8, 128], BF16)
        w_uq_b = sb("w_uq_b", [128, 3, 1536], BF16)
        wqr = sb("wqr", [128, 3, 8, 128], BF16)
        wqrot = sb("wqrot", [128, 3, 8, 128], BF16)
        w_ukv_b = sb("w_ukv_b", [128, 2, 2048], BF16)
        qn_bc = sb("qn_bc", [128, QL])
        kvn_bc = sb("kvn_bc", [128, KVL])
        lng = sb("lng", [128, D])
        lnb = sb("lnb", [128, D])
        ones_b = sb("ones_b", [128, 128], BF16)
        S.op("pool", lambda e: e.memset(ones_b.t[:], 1.0), writes=[ones_b.b])
        S.dma(qn_bc.t[:], q_norm.partition_broadcast(128), writes=[qn_bc.b])
        S.dma(kvn_bc.t[:], kv_norm.partition_broadcast(128), writes=[kvn_bc.b])
        S.dma(lng.t[:], ln_g.partition_broadcast(128), writes=[lng.b])
        S.dma(lnb.t[:], ln_b.partition_broadcast(128), writes=[lnb.b])
        cqnT = sb("cqnT", [128, 3, SEQ], BF16, nb=8)
        ckvnT = sb("ckvnT", [128, 2, SEQ], BF16, nb=8)
        kpeT = sb("kpeT", [128, SEQ], BF16, nb=8)
        xTc = [sb("xTc%d" % j, [128, 8, 512], BF16) for j in range(2)]
        cosc = [sb("cosc%d" % j, [128, 512]) for j in range(2)]
        sinc = [sb("sinc%d" % j, [128, 512]) for j in range(2)]
        tmp1 = sb("tmp1", [128, 512])
        tmp2 = sb("tmp2", [128, 512])
        junk = sb("junk", [128, 512])
        cn = sb("cn", [128, 640])
        small = sb("small", [128, 64])
        sm_b = [Buf() for _ in range(8)]
        ssb = [Buf() for _ in range(4)]
        rr = [0]
        stA = contextlib.ExitStack()
        stg = [T(stA.enter_context(nc.sbuf_tensor("sbo%d_stg%d" % (l, j), [128, 2048], F32))) for j in range(2)]

        def ld(dst_ap, src_ap, shape3, dstT):
            s_ = stg[rr[0]]
            rr[0] ^= 1
            c_, n_ = shape3
            S.dma(s_.t[:, 0:c_ * n_].rearrange("p (c n) -> p c n", c=c_), src_ap, writes=[s_.b])
            S.op("act", lambda e: e.copy(out=dst_ap, in_=s_.t[:, 0:c_ * n_].rearrange("p (c n) -> p c n", c=c_)),
                 reads=[s_.b], writes=[dstT.b])
        for c2 in range(4):
            ld(w_in_b.t[:, 2 * c2:2 * c2 + 2, :], w_in[c2 * 256:(c2 + 1) * 256, :].rearrange("(c p) n -> p c n", p=128), (2, 704), w_in_b)
        for j in range(3):
            ld(w_uq_b.t[:, j:j + 1, :], w_uq[j * 128:(j + 1) * 128, :].rearrange("(c p) n -> p c n", p=128), (1, 1536), w_uq_b)
        for j in range(2):
            ld(w_ukv_b.t[:, j:j + 1, :], w_ukv[j * 128:(j + 1) * 128, :].rearrange("(c p) n -> p c n", p=128), (1, 2048), w_ukv_b)
        S.barrier()
        stA.close()
        S.op("pool", lambda e: e.memset(wkp.t[:], 0.0), writes=[wkp.b])
        S.op("pool", lambda e: e.memset(wkr.t[:], 0.0), writes=[wkr.b])
        S.op("pool", lambda e: e.memset(wqr.t[:], 0.0), writes=[wqr.b])
        S.op("pool", lambda e: e.memset(wqrot.t[:], 0.0), writes=[wqrot.b])
        S.op("dve", lambda e: e.tensor_copy(out=wkp.t[:, :, 0:64], in_=w_in_b.t[:, :, 640:704]), reads=[w_in_b.b], writes=[wkp.b])
        S.op("dve", lambda e: e.tensor_scalar(out=wkr.t[:, :, 0:32], in0=w_in_b.t[:, :, 672:704], scalar1=-1.0, scalar2=None, op0=ALU.mult),
             reads=[w_in_b.b], writes=[wkr.b])
        S.op("dve", lambda e: e.tensor_copy(out=wkr.t[:, :, 32:64], in_=w_in_b.t[:, :, 640:672]), reads=[w_in_b.b], writes=[wkr.b])
        for j in range(3):
            v_ = w_uq_b.t[:, j, :].rearrange("p (h d) -> p h d", h=8)
            S.op("dve", lambda e: e.tensor_copy(out=wqr.t[:, j, :, 0:64], in_=v_[:, :, 128:192]), reads=[w_uq_b.b], writes=[wqr.b])
            S.op("dve", lambda e: e.tensor_scalar(out=wqrot.t[:, j, :, 0:32], in0=v_[:, :, 160:192], scalar1=-1.0, scalar2=None, op0=ALU.mult),
                 reads=[w_uq_b.b], writes=[wqrot.b])
            S.op("dve", lambda e: e.tensor_copy(out=wqrot.t[:, j, :, 32:64], in_=v_[:, :, 128:160]), reads=[w_uq_b.b], writes=[wqrot.b])


        def rope_combine(pa, pa_b, pb, pb_b, cc, sc, dst_ap, dst_b):
            S.op("dve", lambda e: e.tensor_tensor(out=tmp1.t[:], in0=pa, in1=cc.t[:], op=ALU.mult),
                 reads=[pa_b, cc.b], writes=[tmp1.b])
            S.op("dve", lambda e: e.tensor_tensor(out=tmp2.t[:], in0=pb, in1=sc.t[:], op=ALU.mult),
                 reads=[pb_b, sc.b], writes=[tmp2.b])
            S.op("pool", lambda e: e.tensor_tensor(out=dst_ap, in0=tmp1.t[:], in1=tmp2.t[:], op=ALU.add),
                 reads=[tmp1.b, tmp2.b], writes=[dst_b])

        ostop = (k.dbg or {}).get("ostop", 99)
        for ch in range(8 if ostop >= 1 else 0):
            xc = xTc[ch % 2]
            cc, sc = cosc[ch % 2], sinc[ch % 2]
            tok = slice(ch * 512, (ch + 1) * 512)
            S.dma(xc.t[:], k.xT_d.rearrange("c p t -> p c t")[:, :, tok], reads=k.xT_b[ch * 4:ch * 4 + 4], writes=[xc.b])
            S.dma(cc.t[:], cos2T[:, tok], writes=[cc.b])
            S.dma(sc.t[:], sin2T[:, tok], writes=[sc.b])
            for j4 in range(4):
                t = ch * 4 + j4
                tl_ = slice(j4 * 128, (j4 + 1) * 128)
                pp = k.ps[j4 % 2]
                for (a, b_, hb) in ((0, 512, 0), (512, 704, 1)):
                    for c in range(8):
                        S.op("pe", lambda e: e.matmul(pp.t[:, a:b_], lhsT=xc.t[:, c, tl_], rhs=w_in_b.t[:, c, a:b_],
                                                      start=(c == 0), stop=(c == 7)),
                             reads=[xc.b, w_in_b.b], writes=[pp.bs[hb]], inc=(c == 7))
                S.op("act", lambda e: e.activation(out=junk.t[:, 0:QL], in_=pp.t[:, 0:QL], func=AF.Square, accum_out=small.t[:, 0:1]),
                     reads=[pp.bs[0]], writes=[junk.b, ssb[0]])
                S.op("act", lambda e: e.activation(out=junk.t[:, 0:KVL], in_=pp.t[:, QL:QL + KVL], func=AF.Square, accum_out=small.t[:, 1:2]),
                     reads=pp.bs, writes=[junk.b, ssb[1]])
                S.op("dve", lambda e: e.tensor_scalar(out=small.t[:, 2:3], in0=small.t[:, 0:1], scalar1=1.0 / QL, scalar2=RMS_EPS,
                                                      op0=ALU.mult, op1=ALU.add), reads=[ssb[0]], writes=[ssb[2]])
                S.op("dve", lambda e: e.tensor_scalar(out=small.t[:, 3:4], in0=small.t[:, 1:2], scalar1=1.0 / KVL, scalar2=RMS_EPS,
                                                      op0=ALU.mult, op1=ALU.add), reads=[ssb[1]], writes=[ssb[2]])
                S.op("act", lambda e: e.activation(out=small.t[:, 4:6], in_=small.t[:, 2:4], func=AF.Sqrt), reads=[ssb[2]], writes=[ssb[3]])
                S.op("dve", lambda e: e.reciprocal(out=small.t[:, 6:8], in_=small.t[:, 4:6]), reads=[ssb[3]], writes=[ssb[3]])
                S.op("dve", lambda e: e.scalar_tensor_tensor(out=cn.t[:, 0:QL], in0=pp.t[:, 0:QL], scalar=small.t[:, 6:7], in1=qn_bc.t[:],
                                                             op0=ALU.mult, op1=ALU.mult),
                     reads=[pp.bs[0], ssb[3], qn_bc.b], writes=[cn.b])
                S.op("dve", lambda e: e.scalar_tensor_tensor(out=cn.t[:, QL:640], in0=pp.t[:, QL:640], scalar=small.t[:, 7:8], in1=kvn_bc.t[:],
                                                             op0=ALU.mult, op1=ALU.mult),
                     reads=pp.bs + [ssb[3], kvn_bc.b], writes=[cn.b])
                pt = k.ps[2]
                for j in range(5):
                    S.op("pe", lambda e: e.transpose(out=pt.t[:, j * 128:(j + 1) * 128], in_=cn.t[:, j * 128:(j + 1) * 128], identity=k.ident.t[:]),
                         reads=[cn.b, k.ident.b], writes=[pt.bs[j // 4]], inc=(j in (3, 4)))
                S.op("act", lambda e: e.copy(out=cqnT.t[:, :, t * 128:(t + 1) * 128], in_=pt.t[:, 0:384].rearrange("p (j t) -> p j t", j=3)),
                     reads=[pt.bs[0]], writes=[cqnT.bs[ch]])
                S.op("act", lambda e: e.copy(out=ckvnT.t[:, :, t * 128:(t + 1) * 128], in_=pt.t[:, 384:640].rearrange("p (j t) -> p j t", j=2)),
                     reads=pt.bs, writes=[ckvnT.bs[ch]])
            pk = k.ps[3]
            for (wt, hb) in ((wkp, 0), (wkr, 1)):
                for c in range(8):
                    S.op("pe", lambda e: e.matmul(pk.t[:, hb * 512:(hb + 1) * 512], lhsT=wt.t[:, c, :], rhs=xc.t[:, c, :],
                                                  start=(c == 0), stop=(c == 7)),
                         reads=[wt.b, xc.b], writes=[pk.bs[hb]], inc=(c == 7))
            rope_combine(pk.t[:, 0:512], pk.bs[0], pk.t[:, 512:1024], pk.bs[1], cc, sc, kpeT.t[:, tok], kpeT.bs[ch])

        stB = contextlib.ExitStack()

        def sb(name, shape, dt=F32, nb=1):
            return T(stB.enter_context(nc.sbuf_tensor("sbo%d_" % l + name, list(shape), dt)), nb)
        qn = sb("qn", [128, SEQ], BF16, nb=8)
        qr = sb("qr", [128, SEQ], BF16, nb=8)
        kn = sb("kn", [128, SEQ], BF16, nb=8)
        vv = sb("vv", [128, NT, 128], BF16, nb=8)
        oTh = sb("oTh", [128, SEQ], BF16)
        sq = sb("sq", [128, 512], BF16)
        PT = [sb("PT%d" % j, [128, 512], BF16) for j in range(2)]
        rs = sb("rs", [128, 128])
        mq = sb("mq", [128, 16])
        negB = sb("negB", [128, 8])
        for h in range(n_h if ostop >= 2 else 0):
            for ch in range(8):
                tok = slice(ch * 512, (ch + 1) * 512)
                cc, sc = cosc[ch % 2], sinc[ch % 2]
                S.dma(cc.t[:], cos2T[:, tok], writes=[cc.b])
                S.dma(sc.t[:], sin2T[:, tok], writes=[sc.b])
                p0 = k.ps[0]
                for j in range(3):
                    S.op("pe", lambda e: e.matmul(p0.t[:, 0:512], lhsT=w_uq_b.t[:, j, h * 192:h * 192 + 128], rhs=cqnT.t[:, j, tok],
                                                  start=(j == 0), stop=(j == 2)),
                         reads=[w_uq_b.b, cqnT.bs[ch]], writes=[p0.bs[0]], inc=(j == 2))
                S.op("act", lambda e: e.copy(out=qn.t[:, tok], in_=p0.t[:, 0:512]), reads=[p0.bs[0]], writes=[qn.bs[ch]])
                for j in range(2):
                    S.op("pe", lambda e: e.matmul(p0.t[:, 512:1024], lhsT=w_ukv_b.t[:, j, h * 256:h * 256 + 128], rhs=ckvnT.t[:, j, tok],
                                                  start=(j == 0), stop=(j == 1)),
                         reads=[w_ukv_b.b, ckvnT.bs[ch]], writes=[p0.bs[1]], inc=(j == 1))
                S.op("act", lambda e: e.copy(out=kn.t[:, tok], in_=p0.t[:, 512:1024]), reads=[p0.bs[1]], writes=[kn.bs[ch]])
                p1 = k.ps[1]
                for (wt, hb) in ((wqr, 0), (wqrot, 1)):
                    for j in range(3):
                        S.op("pe", lambda e: e.matmul(p1.t[:, hb * 512:(hb + 1) * 512], lhsT=wt.t[:, j, h, :], rhs=cqnT.t[:, j, tok],
                                                      start=(j == 0), stop=(j == 2)),
                             reads=[wt.b, cqnT.bs[ch]], writes=[p1.bs[hb]], inc=(j == 2))
                rope_combine(p1.t[:, 0:512], p1.bs[0], p1.t[:, 512:1024], p1.bs[1], cc, sc, qr.t[:, tok], qr.bs[ch])
                p2 = k.ps[2]
                for j4 in range(4):
                    t = ch * 4 + j4
                    for j in range(2):
                        S.op("pe", lambda e: e.matmul(p2.t[:, j4 * 128:(j4 + 1) * 128], lhsT=ckvnT.t[:, j, t * 128:(t + 1) * 128],
                                                      rhs=w_ukv_b.t[:, j, h * 256 + 128:h * 256 + 256], start=(j == 0), stop=(j == 1)),
                             reads=[w_ukv_b.b, ckvnT.bs[ch]], writes=[p2.bs[0]], inc=(j == 1 and j4 == 3))
                S.op("act", lambda e: e.copy(out=vv.t[:, ch * 4:ch * 4 + 4, :], in_=p2.t[:, 0:512].rearrange("p (a b) -> p a b", a=4)),
                     reads=[p2.bs[0]], writes=[vv.bs[ch]])
                p3 = k.ps[3]
                for half, (srcs) in enumerate((((qn, qn.bs[ch]), (qr, qr.bs[ch])), ((kn, kn.bs[ch]), (kpeT, kpeT.bs[ch])))):
                    for si, (tt_, tb_) in enumerate(srcs):
                        S.op("act", lambda e: e.activation(out=sq.t[:], in_=tt_.t[:, tok], func=AF.Square), reads=[tb_], writes=[sq.b])
                        S.op("pe", lambda e: e.matmul(p3.t[:, half * 512:(half + 1) * 512], lhsT=ones_b.t[:], rhs=sq.t[:],
                                                      start=(si == 0), stop=(si == 1)),
                             reads=[ones_b.b, sq.b], writes=[p3.bs[half]])
                    S.op("dve", lambda e: e.tensor_reduce(out=mq.t[:, half * 8 + ch:half * 8 + ch + 1], in_=p3.t[:, half * 512:(half + 1) * 512],
                                                          axis=AX.X, op=ALU.max),
                         reads=[p3.bs[half]], writes=[mq.b])
            S.op("dve", lambda e: e.tensor_reduce(out=small.t[:, 8:9], in_=mq.t[:, 0:8], axis=AX.X, op=ALU.max), reads=[mq.b], writes=[sm_b[0]])
            S.op("dve", lambda e: e.tensor_reduce(out=small.t[:, 9:10], in_=mq.t[:, 8:16], axis=AX.X, op=ALU.max), reads=[mq.b], writes=[sm_b[0]])
            S.op("dve", lambda e: e.tensor_tensor(out=small.t[:, 10:11], in0=small.t[:, 8:9], in1=small.t[:, 9:10], op=ALU.mult),
                 reads=[sm_b[0]], writes=[sm_b[1]])
            S.op("act", lambda e: e.activation(out=small.t[:, 11:12], in_=small.t[:, 10:11], func=AF.Sqrt), reads=[sm_b[1]], writes=[sm_b[1]])
            S.op("dve", lambda e: e.tensor_scalar(out=negB.t[:, h:h + 1], in0=small.t[:, 11:12], scalar1=-QK_SCALE, scalar2=None, op0=ALU.mult),
                 reads=[sm_b[1]], writes=[negB.b])
            gi = 0
            for qt in range(NT if ostop >= 3 else 0):
                qs = slice(qt * 128, (qt + 1) * 128)
                po = k.ps[2 + (qt % 2)]
                nblk = qt + 1
                for g0 in range(0, nblk, 4):
                    nb_ = min(4, nblk - g0)
                    psS = k.ps[(gi // 2) % 2]
                    hb = gi % 2
                    pt_ = PT[gi % 2]
                    gi += 1
                    for bi in range(nb_):
                        kb = g0 + bi
                        ks = slice(kb * 128, (kb + 1) * 128)
                        o_ = psS.t[:, hb * 512 + bi * 128:hb * 512 + (bi + 1) * 128]
                        S.op("pe", lambda e: e.matmul(o_, lhsT=kn.t[:, ks], rhs=qn.t[:, qs], start=True, stop=False),
                             reads=[kn.bs[kb // 4], qn.bs[qt // 4]], writes=[psS.bs[hb]], inc=False)
                        S.op("pe", lambda e: e.matmul(o_, lhsT=kpeT.t[:, ks], rhs=qr.t[:, qs], start=False, stop=True),
                             reads=[kpeT.bs[kb // 4], qr.bs[qt // 4]], writes=[psS.bs[hb]], inc=(bi == nb_ - 1))
                    S.op("act", lambda e: e.activation(out=pt_.t[:, 0:nb_ * 128], in_=psS.t[:, hb * 512:hb * 512 + nb_ * 128], func=AF.Exp,
                                                       bias=negB.t[:, h:h + 1], scale=QK_SCALE),
                         reads=[psS.bs[hb], negB.b], writes=[pt_.b])
                    if g0 + nb_ == nblk:
                        bi = nb_ - 1
                        S.op("pool", lambda e: e.memset(pt_.t[64:128, bi * 128:bi * 128 + 64], 0.0), writes=[pt_.b])
                    for bi in range(nb_):
                        kb = g0 + bi
                        S.op("pe", lambda e: e.matmul(po.t[:, 0:128], lhsT=vv.t[:, kb, :], rhs=pt_.t[:, bi * 128:(bi + 1) * 128],
                                                      start=(kb == 0), stop=(kb == nblk - 1)),
                             reads=[vv.bs[kb // 4], pt_.b], writes=[po.bs[0]], inc=False)
                        S.op("pe", lambda e: e.matmul(po.t[:, 128:256], lhsT=ones_b.t[:], rhs=pt_.t[:, bi * 128:(bi + 1) * 128],
                                                      start=False, stop=(kb == nblk - 1)),
                             reads=[ones_b.b, pt_.b], writes=[po.bs[0]], inc=(bi == nb_ - 1))
                S.op("dve", lambda e: e.reciprocal(out=rs.t[:], in_=po.t[:, 128:256]), reads=[po.bs[0]], writes=[rs.b])
                S.op("dve", lambda e: e.tensor_tensor(out=oTh.t[:, qs], in0=po.t[:, 0:128], in1=rs.t[:], op=ALU.mult),
                     reads=[po.bs[0], rs.b], writes=[oTh.b])
            S.dma(oT_d[h], oTh.t[:], reads=[oTh.b], writes=[oT_b[h]])

        S.barrier()
        stB.close()

        def sb(name, shape, dt=F32, nb=1):
            return T(st.enter_context(nc.sbuf_tensor("sbo%d_" % l + name, list(shape), dt)), nb)
        stg = [sb("stgC", [128, 2048])]
        rr[0] = 0
        w_out_b = sb("w_out_b", [128, 8, D], BF16)
        for c2 in range(4):
            ld(w_out_b.t[:, 2 * c2:2 * c2 + 2, :], w_out[c2 * 256:(c2 + 1) * 256, :].rearrange("(c p) n -> p c n", p=128), (2, D), w_out_b)
            rr[0] = 0
        work = [sb("work%d" % j, [128, D]) for j in range(1)]
        for ch in range(8 if ostop >= 4 else 0):
            xc = xTc[ch % 2]
            tok = slice(ch * 512, (ch + 1) * 512)
            S.dma(xc.t[:], oT_d.rearrange("c p t -> p c t")[:, :, tok], reads=oT_b, writes=[xc.b])
            for j4 in range(4):
                t = ch * 4 + j4
                pp = k.ps[j4 % 2]
                for hb in range(2):
                    for c in range(8):
                        S.op("pe", lambda e: e.matmul(pp.t[:, hb * 512:(hb + 1) * 512], lhsT=xc.t[:, c, j4 * 128:(j4 + 1) * 128],
                                                      rhs=w_out_b.t[:, c, hb * 512:(hb + 1) * 512], start=(c == 0), stop=(c == 7)),
                             reads=[xc.b, w_out_b.b], writes=[pp.bs[hb]], inc=(c == 7))
                epilogue(k, t, work[0], lng, lnb, small, sm_b, False, src=pp.t[:], src_bufs=pp.bs)
        S.barrier()
    k.xcur = k.xres
    k.xcur_is_in = False


def even_phase(k, l):
    S, nc = k.S, k.nc
    i = l // 2
    w_in = k.inp("even_w_in_%d" % i, [D, 3080])
    w_out = k.inp("even_w_out_%d" % i, [D, D])
    sgT = k.inp("even_sgu_wT_%d" % i, [128, 4, 128])
    bsT_d = k.inp("even_sgu_bT_%d" % i, [128, 4])
    sg_d = k.inp("even_sgu_ln_g_%d" % i, [1, 512])
    sb_d = k.inp("even_sgu_ln_b_%d" % i, [1, 512])
    cw_d = k.inp("even_conv_wT_%d" % i, [128, 12, 4])
    alog_d = k.inp("even_a_log_%d" % i, [1, 4])
    dtb_d = k.inp("even_dt_bias_%d" % i, [1, 4])
    gnw_d = k.inp("even_gdn_norm_%d" % i, [1, 128])
    ln_g = k.inp("ln_mix_g_%d" % l, [1, D])
    ln_b = k.inp("ln_mix_b_%d" % l, [1, D])
    tri_d = k.inp("tri", [128, 128])
    maskL_d = k.inp("maskL", [128, 128])
    n_chunks = (k.dbg or {}).get("e_ch", 8)
    with contextlib.ExitStack() as st:
        def sb(name, shape, dt=F32, nb=1):
            return T(st.enter_context(nc.sbuf_tensor("sbe%d_" % l + name, list(shape), dt)), nb)
        w_in_b = sb("w_in_b", [128, 8, 3080], BF16)
        w_out_b = sb("w_out_b", [128, 8, D], BF16)
        wsT = sb("wsT", [128, 4, 128], BF16)
        bsT = sb("bsT", [128, 4])
        sg_bc = sb("sg_bc", [128, 512])
        sb_bc = sb("sb_bc", [128, 512])
        cw = sb("cw", [128, 12, 4])
        negA = sb("negA", [128, 4])
        dtb = sb("dtb", [128, 4])
        gnw = sb("gnw", [128, 128])
        lng = sb("lng", [128, D])
        lnb = sb("lnb", [128, D])
        tri = sb("tri", [128, 128])
        maskL = sb("maskL", [128, 128])
        ones_f = sb("ones_f", [128, 128])
        S.op("pool", lambda e: e.memset(ones_f.t[:], 1.0), writes=[ones_f.b])
        for (dst, src) in ((bsT, bsT_d), (cw, cw_d), (tri, tri_d), (maskL, maskL_d)):
            S.dma(dst.t[:], src, writes=[dst.b])
        for (dst, src) in ((sg_bc, sg_d), (sb_bc, sb_d), (negA, alog_d), (dtb, dtb_d), (gnw, gnw_d), (lng, ln_g), (lnb, ln_b)):
            S.dma(dst.t[:], src.partition_broadcast(128), writes=[dst.b])
        S.op("act", lambda e: e.activation(out=negA.t[:], in_=negA.t[:], func=AF.Exp), reads=[negA.b], writes=[negA.b])
        S.op("dve", lambda e: e.tensor_scalar(out=negA.t[:], in0=negA.t[:], scalar1=-1.0, scalar2=None, op0=ALU.mult),
             reads=[negA.b], writes=[negA.b])
        stA = contextlib.ExitStack()
        stg = [T(stA.enter_context(nc.sbuf_tensor("sbe%d_stg%d" % (l, j), [128, 3080], F32))) for j in range(2)]
        for c in range(8):
            s_ = stg[c % 2]
            S.dma(s_.t[:], w_in[c * 128:(c + 1) * 128, :], writes=[s_.b])
            S.op("act" if c % 2 else "pool", lambda e: (e.copy if c % 2 else e.tensor_copy)(out=w_in_b.t[:, c, :], in_=s_.t[:]),
                 reads=[s_.b], writes=[w_in_b.b])
        for c2 in range(4):
            s_ = stg[c2 % 2]
            S.dma(s_.t[:, 0:2048].rearrange("p (c n) -> p c n", c=2), w_out[c2 * 256:(c2 + 1) * 256, :].rearrange("(c p) n -> p c n", p=128),
                  writes=[s_.b])
            S.op("act", lambda e: e.copy(out=w_out_b.t[:, 2 * c2:2 * c2 + 2, :], in_=s_.t[:, 0:2048].rearrange("p (c n) -> p c n", c=2)),
                 reads=[s_.b], writes=[w_out_b.b])
        s_ = stg[0]
        S.dma(s_.t[:, 0:512].rearrange("p (g i) -> p g i", g=4), sgT, writes=[s_.b])
        S.op("dve", lambda e: e.tensor_copy(out=wsT.t[:], in_=s_.t[:, 0:512].rearrange("p (g i) -> p g i", g=4)), reads=[s_.b], writes=[wsT.b])
        S.op("dve", lambda e: e.memset(wsT.t[64:128, :, 0:64], 0.0), writes=[wsT.b])
        S.barrier()
        stA.close()

        xTc = [sb("xTc%d" % j, [128, 8, 512], BF16) for j in range(2)]
        raw = sb("raw", [128, 12, 515], nb=12)
        qkv = sb("qkv", [128, 12, 512], nb=12)
        S.op("pool", lambda e: e.memset(raw.t[:, :, 0:3], 0.0), writes=raw.bs)
        cvt = sb("cvt", [128, 512])
        sqt = sb("sqt", [128, 512])
        rst = sb("rst", [128, 512])
        u_sb = sb("u_sb", [128, 512])
        vg = sb("vg", [128, 512])
        vnb = sb("vnb", [128, 512], BF16)
        sz = sb("sz", [128, 512])
        mixt = sb("mixt", [128, D], nb=8)
        work = sb("work", [128, D])
        small = sb("small", [128, 64])
        sm_b = [Buf() for _ in range(8)]
        sm2 = sb("sm2", [128, 96])
        s2 = [Buf() for _ in range(24)]
        St = [sb("St%d" % h, [128, 128]) for h in range(4)]
        for h in range(4):
            S.op("pool", lambda e: e.memset(St[h].t[:], 0.0), writes=[St[h].b])
        tn = {}
        for nm in ("kbg", "kdec", "vb", "gB", "t1", "t2", "Dn", "DT", "egr", "qdT", "M0", "M1", "N0", "N1", "P0", "P1", "intraT", "u", "wT",
                   "vnew", "junk", "yb"):
            tn[nm] = sb("g_" + nm, [128, 128])
        slots = [(k.ps[2].t[:, 0:128], k.ps[2].bs[0]), (k.ps[2].t[:, 512:640], k.ps[2].bs[1]),
                 (k.ps[3].t[:, 0:128], k.ps[3].bs[0]), (k.ps[3].t[:, 512:640], k.ps[3].bs[1])]
        sl_i = [0]

        def slot():
            r = slots[sl_i[0] % len(slots)]
            sl_i[0] += 1
            return r
        ident = k.ident

        def mm(out_ap, out_b, lhsT, lb, rhs, rb, start=True, stop=True):
            S.op("pe", lambda e: e.matmul(out_ap, lhsT=lhsT, rhs=rhs, start=start, stop=stop), reads=[lb, rb], writes=[out_b])

        estop = (k.dbg or {}).get("estop", 99)
        for ch in range(n_chunks if estop >= 1 else 0):
            xc = xTc[ch % 2]
            tok = slice(ch * 512, (ch + 1) * 512)
            S.dma(xc.t[:], k.xT_d.rearrange("c p t -> p c t")[:, :, tok], reads=k.xT_b[ch * 4:ch * 4 + 4], writes=[xc.b])
            for cc in range(12):
                pp = k.ps[1]
                hb = cc % 2
                for c in range(8):
                    S.op("pe", lambda e: e.matmul(pp.t[:, hb * 512:(hb + 1) * 512], lhsT=w_in_b.t[:, c, 1024 + cc * 128:1024 + (cc + 1) * 128],
                                                  rhs=xc.t[:, c, :], start=(c == 0), stop=(c == 7)),
                         reads=[w_in_b.b, xc.b], writes=[pp.bs[hb]], inc=(c == 7))
                if ch > 0:
                    S.op("dve", lambda e: e.tensor_copy(out=raw.t[:, cc, 0:3], in_=raw.t[:, cc, 512:515]), reads=[raw.bs[cc]], writes=[raw.bs[cc]])
                S.op("act", lambda e: e.copy(out=raw.t[:, cc, 3:515], in_=pp.t[:, hb * 512:(hb + 1) * 512]), reads=[pp.bs[hb]], writes=[raw.bs[cc]])
                S.op("act", lambda e: e.activation(out=cvt.t[:], in_=raw.t[:, cc, 3:515], func=AF.Copy, scale=cw.t[:, cc, 3:4]),
                     reads=[raw.bs[cc], cw.b], writes=[cvt.b])
                for s_ in (1, 2, 3):
                    S.op("dve", lambda e: e.scalar_tensor_tensor(out=cvt.t[:], in0=raw.t[:, cc, 3 - s_:515 - s_], scalar=cw.t[:, cc, 3 - s_:4 - s_],
                                                                 in1=cvt.t[:], op0=ALU.mult, op1=ALU.add),
                         reads=[raw.bs[cc], cw.b, cvt.b], writes=[cvt.b])
                S.op("act", lambda e: e.activation(out=qkv.t[:, cc, :], in_=cvt.t[:], func=AF.Silu), reads=[cvt.b], writes=[qkv.bs[cc]])
                if cc < 8:
                    S.op("act", lambda e: e.activation(out=sqt.t[:], in_=qkv.t[:, cc, :], func=AF.Square), reads=[qkv.bs[cc]], writes=[sqt.b])
                    pn = k.ps[0]
                    S.op("pe", lambda e: e.matmul(pn.t[:, hb * 512:(hb + 1) * 512], lhsT=ones_f.t[:], rhs=sqt.t[:], start=True, stop=True),
                         reads=[ones_f.b, sqt.b], writes=[pn.bs[hb]])
                    S.op("dve", lambda e: e.tensor_scalar(out=rst.t[:], in0=pn.t[:, hb * 512:(hb + 1) * 512], scalar1=RMS_EPS, scalar2=None, op0=ALU.add),
                         reads=[pn.bs[hb]], writes=[rst.b])
                    S.op("act", lambda e: e.activation(out=rst.t[:], in_=rst.t[:], func=AF.Sqrt), reads=[rst.b], writes=[rst.b])
                    S.op("dve", lambda e: e.reciprocal(out=rst.t[:], in_=rst.t[:]), reads=[rst.b], writes=[rst.b])
                    sc_ = (128.0 ** -0.5) if cc < 4 else 1.0
                    S.op("dve", lambda e: e.scalar_tensor_tensor(out=qkv.t[:, cc, :], in0=qkv.t[:, cc, :], scalar=sc_, in1=rst.t[:],
                                                                 op0=ALU.mult, op1=ALU.mult),
                         reads=[qkv.bs[cc], rst.b], writes=[qkv.bs[cc]])
            for j4 in range(4 if estop >= 2 else 0):
                t = ch * 4 + j4
                tl_ = slice(j4 * 128, (j4 + 1) * 128)
                pa = k.ps[0]
                for hb in range(2):
                    for c in range(8):
                        S.op("pe", lambda e: e.matmul(pa.t[:, hb * 512:(hb + 1) * 512], lhsT=xc.t[:, c, tl_], rhs=w_in_b.t[:, c, hb * 512:(hb + 1) * 512],
                                                      start=(c == 0), stop=(c == 7)),
                             reads=[xc.b, w_in_b.b], writes=[pa.bs[hb]], inc=(c == 7))
                S.op("act", lambda e: e.activation(out=u_sb.t[:], in_=pa.t[:, 0:512], func=AF.Gelu), reads=[pa.bs[0]], writes=[u_sb.b])
                S.op("act", lambda e: e.activation(out=vg.t[:], in_=pa.t[:, 512:1024], func=AF.Gelu), reads=[pa.bs[1]], writes=[vg.b])
                for g in range(4):
                    S.op("dve", lambda e: e.bn_stats(out=sm2.t[:, g * 6:(g + 1) * 6], in_=vg.t[:, g * 128:(g + 1) * 128]), reads=[vg.b], writes=[s2[g]])
                    S.op("dve", lambda e: e.bn_aggr(out=sm2.t[:, 24 + 2 * g:26 + 2 * g], in_=sm2.t[:, g * 6:(g + 1) * 6]), reads=[s2[g]], writes=[s2[4 + g]])
                    S.op("dve", lambda e: e.tensor_scalar(out=sm2.t[:, 32 + g:33 + g], in0=sm2.t[:, 25 + 2 * g:26 + 2 * g], scalar1=LN_EPS, scalar2=None,
                                                          op0=ALU.add), reads=[s2[4 + g]], writes=[s2[8]])
                S.op("act", lambda e: e.activation(out=sm2.t[:, 36:40], in_=sm2.t[:, 32:36], func=AF.Sqrt), reads=[s2[8]], writes=[s2[9]])
                S.op("dve", lambda e: e.reciprocal(out=sm2.t[:, 40:44], in_=sm2.t[:, 36:40]), reads=[s2[9]], writes=[s2[9]])
                for g in range(4):
                    S.op("dve", lambda e: e.tensor_scalar(out=vg.t[:, g * 128:(g + 1) * 128], in0=vg.t[:, g * 128:(g + 1) * 128],
                                                          scalar1=sm2.t[:, 24 + 2 * g:25 + 2 * g], scalar2=sm2.t[:, 40 + g:41 + g],
                                                          op0=ALU.subtract, op1=ALU.mult),
                         reads=[vg.b, s2[4 + g], s2[9]], writes=[vg.b])
                S.op("pool", lambda e: e.tensor_tensor(out=vg.t[:], in0=vg.t[:], in1=sg_bc.t[:], op=ALU.mult), reads=[vg.b, sg_bc.b], writes=[vg.b])
                S.op("pool", lambda e: e.tensor_tensor(out=vnb.t[:], in0=vg.t[:], in1=sb_bc.t[:], op=ALU.add), reads=[vg.b, sb_bc.b], writes=[vnb.b])
                pm = k.ps[1]
                for g in range(4):
                    S.op("pe", lambda e: e.matmul(pm.t[:, g * 128:(g + 1) * 128], lhsT=wsT.t[:, g, :], rhs=vnb.t[:, g * 128:(g + 1) * 128],
                                                  start=True, stop=True),
                         reads=[wsT.b, vnb.b], writes=[pm.bs[0]], inc=(g == 3))
                mb = mixt.bs[0]
                for g in range(4):
                    S.op("dve", lambda e: e.scalar_tensor_tensor(out=mixt.t[:, g * 128:(g + 1) * 128], in0=pm.t[:, g * 128:(g + 1) * 128],
                                                                 scalar=bsT.t[:, g:g + 1], in1=u_sb.t[:, g * 128:(g + 1) * 128],
                                                                 op0=ALU.add, op1=ALU.mult),
                         reads=[pm.bs[0], bsT.b, u_sb.b], writes=[mb])
                if estop < 3:
                    continue
                for c in range(8):
                    S.op("pe", lambda e: e.matmul(pm.t[:, 512:1024], lhsT=xc.t[:, c, tl_], rhs=w_in_b.t[:, c, 2560:3072], start=(c == 0), stop=(c == 7)),
                         reads=[xc.b, w_in_b.b], writes=[pm.bs[1]], inc=(c == 7))
                S.op("act", lambda e: e.activation(out=sz.t[:], in_=pm.t[:, 512:1024], func=AF.Silu), reads=[pm.bs[1]], writes=[sz.b])
                pab, pab_b = slot()
                for c in range(8):
                    S.op("pe", lambda e: e.matmul(pab[:, 0:8], lhsT=xc.t[:, c, tl_], rhs=w_in_b.t[:, c, 3072:3080], start=(c == 0), stop=(c == 7)),
                         reads=[xc.b, w_in_b.b], writes=[pab_b], inc=(c == 7))
                S.op("act", lambda e: e.activation(out=sm2.t[:, 48:52], in_=pab[:, 4:8], func=AF.Sigmoid), reads=[pab_b], writes=[s2[10]])
                S.op("dve", lambda e: e.tensor_scalar(out=sm2.t[:, 80:84], in0=sm2.t[:, 48:52], scalar1=-1.0, scalar2=None, op0=ALU.mult),
                     reads=[s2[10]], writes=[s2[18]])
                S.op("dve", lambda e: e.tensor_tensor(out=sm2.t[:, 76:80], in0=pab[:, 0:4], in1=dtb.t[:], op=ALU.add), reads=[pab_b, dtb.b], writes=[s2[11]])
                S.op("act", lambda e: e.activation(out=sm2.t[:, 76:80], in_=sm2.t[:, 76:80], func=AF.Exp), reads=[s2[11]], writes=[s2[11]])
                S.op("dve", lambda e: e.tensor_scalar(out=sm2.t[:, 76:80], in0=sm2.t[:, 76:80], scalar1=1.0, scalar2=None, op0=ALU.add),
                     reads=[s2[11]], writes=[s2[11]])
                S.op("act", lambda e: e.activation(out=sm2.t[:, 76:80], in_=sm2.t[:, 76:80], func=AF.Ln), reads=[s2[11]], writes=[s2[11]])
                S.op("dve", lambda e: e.tensor_tensor(out=sm2.t[:, 52:56], in0=sm2.t[:, 76:80], in1=negA.t[:], op=ALU.mult), reads=[s2[11], negA.b], writes=[s2[12]])
                pg, pg_b = slot()
                mm(pg[:, 0:4], pg_b, tri.t[:], tri.b, sm2.t[:, 52:56], s2[12])
                mm(pg[:, 4:8], pg_b, ones_f.t[:], ones_f.b, sm2.t[:, 52:56], s2[12])
                S.op("dve", lambda e: e.tensor_copy(out=sm2.t[:, 56:64], in_=pg[:, 0:8]), reads=[pg_b], writes=[s2[13]])
                S.op("act", lambda e: e.activation(out=sm2.t[:, 64:68], in_=sm2.t[:, 60:64], func=AF.Exp), reads=[s2[13]], writes=[s2[14]])
                S.op("act", lambda e: e.activation(out=sm2.t[:, 68:72], in_=sm2.t[:, 56:60], func=AF.Exp), reads=[s2[13]], writes=[s2[15]])
                S.op("dve", lambda e: e.tensor_tensor(out=sm2.t[:, 68:72], in0=sm2.t[:, 68:72], in1=sm2.t[:, 48:52], op=ALU.mult), reads=[s2[15], s2[10]], writes=[s2[15]])
                S.op("dve", lambda e: e.tensor_tensor(out=sm2.t[:, 72:76], in0=sm2.t[:, 60:64], in1=sm2.t[:, 56:60], op=ALU.subtract), reads=[s2[13]], writes=[s2[16]])
                S.op("act", lambda e: e.activation(out=sm2.t[:, 72:76], in_=sm2.t[:, 72:76], func=AF.Exp), reads=[s2[16]], writes=[s2[16]])

                if estop < 4:
                    continue
                for h in range(4):
                    qT = qkv.t[:, h, tl_]
                    kT = qkv.t[:, 4 + h, tl_]
                    vT = qkv.t[:, 8 + h, tl_]
                    qb, kb_, vb_ = qkv.bs[h], qkv.bs[4 + h], qkv.bs[8 + h]
                    col = lambda a: sm2.t[:, a + h:a + h + 1]
                    pk, pk_b = slot()
                    mm(pk, pk_b, kT, kb_, ident.t[:], ident.b)
                    pv, pv_b = slot()
                    mm(pv, pv_b, vT, vb_, ident.t[:], ident.b)
                    if estop <= 4.05:
                        continue
                    S.op("dve", lambda e: e.tensor_scalar(out=tn["kbg"].t[:], in0=pk, scalar1=col(68), scalar2=None, op0=ALU.mult),
                         reads=[pk_b, s2[15]], writes=[tn["kbg"].b])
                    S.op("act", lambda e: e.activation(out=tn["kdec"].t[:], in_=pk, func=AF.Copy, scale=col(72)),
                         reads=[pk_b, s2[16]], writes=[tn["kdec"].b])
                    S.op("dve", lambda e: e.tensor_scalar(out=tn["vb"].t[:], in0=pv, scalar1=col(48), scalar2=None, op0=ALU.mult),
                         reads=[pv_b, s2[10]], writes=[tn["vb"].b])
                    if estop <= 4.1:
                        continue
                    S.op("pool", lambda e: e.tensor_scalar(out=tn["gB"].t[:], in0=ones_f.t[:], scalar1=col(52), scalar2=None, op0=ALU.mult),
                         reads=[ones_f.b, s2[12]], writes=[tn["gB"].b])
                    pr, pr_b = slot()
                    mm(pr, pr_b, tn["gB"].t[:], tn["gB"].b, tri.t[:], tri.b)
                    S.op("dve", lambda e: e.tensor_scalar(out=tn["t1"].t[:], in0=pr, scalar1=col(56), scalar2=0.0, op0=ALU.subtract, op1=ALU.max),
                         reads=[pr_b, s2[13]], writes=[tn["t1"].b])
                    S.op("act", lambda e: e.activation(out=tn["t1"].t[:], in_=tn["t1"].t[:], func=AF.Exp, scale=-1.0), reads=[tn["t1"].b], writes=[tn["t1"].b])
                    S.op("pool", lambda e: e.tensor_tensor(out=tn["Dn"].t[:], in0=tn["t1"].t[:], in1=maskL.t[:], op=ALU.mult),
                         reads=[tn["t1"].b, maskL.b], writes=[tn["Dn"].b])
                    S.op("dve", lambda e: e.tensor_scalar(out=tn["t2"].t[:], in0=pr, scalar1=col(56), scalar2=0.0, op0=ALU.subtract, op1=ALU.min),
                         reads=[pr_b, s2[13]], writes=[tn["t2"].b])
                    S.op("act", lambda e: e.activation(out=tn["t2"].t[:], in_=tn["t2"].t[:], func=AF.Exp), reads=[tn["t2"].b], writes=[tn["t2"].b])
                    S.op("pool", lambda e: e.tensor_tensor(out=tn["DT"].t[:], in0=tn["t2"].t[:], in1=tri.t[:], op=ALU.mult),
                         reads=[tn["t2"].b, tri.b], writes=[tn["DT"].b])
                    S.op("act", lambda e: e.activation(out=tn["egr"].t[:], in_=pr, func=AF.Exp), reads=[pr_b], writes=[tn["egr"].b])
                    S.op("pool", lambda e: e.tensor_tensor(out=tn["qdT"].t[:], in0=qT, in1=tn["egr"].t[:], op=ALU.mult),
                         reads=[qb, tn["egr"].b], writes=[tn["qdT"].b])
                    if estop <= 4.2:
                        continue
                    pkk, pkk_b = slot()
                    mm(pkk, pkk_b, kT, kb_, kT, kb_)
                    S.op("dve", lambda e: e.scalar_tensor_tensor(out=tn["M0"].t[:], in0=pkk, scalar=col(80), in1=tn["Dn"].t[:], op0=ALU.mult, op1=ALU.mult),
                         reads=[pkk_b, s2[18], tn["Dn"].b], writes=[tn["M0"].b])
                    pn_, pn_b = slot()
                    mm(pn_, pn_b, tn["M0"].t[:], tn["M0"].b, ident.t[:], ident.b)
                    S.op("act", lambda e: e.copy(out=tn["N0"].t[:], in_=pn_), reads=[pn_b], writes=[tn["N0"].b])
                    S.op("dve", lambda e: e.tensor_tensor(out=tn["P0"].t[:], in0=pn_, in1=ident.t[:], op=ALU.add), reads=[pn_b, ident.b], writes=[tn["P0"].b])
                    pqk, pqk_b = slot()
                    mm(pqk, pqk_b, kT, kb_, qT, qb)
                    S.op("dve", lambda e: e.tensor_tensor(out=tn["intraT"].t[:], in0=pqk, in1=tn["DT"].t[:], op=ALU.mult),
                         reads=[pqk_b, tn["DT"].b], writes=[tn["intraT"].b])
                    if estop <= 4.3:
                        continue
                    cm, cn_, cp = "M0", "N0", "P0"
                    for s_ in range(6):
                        nm_, nn_, np_ = ("M1", "N1", "P1") if cm == "M0" else ("M0", "N0", "P0")
                        p1, p1_b = slot()
                        mm(p1, p1_b, tn[cn_].t[:], tn[cn_].b, tn[cm].t[:], tn[cm].b)
                        S.op("act", lambda e: e.copy(out=tn[nm_].t[:], in_=p1), reads=[p1_b], writes=[tn[nm_].b])
                        if s_ < 5:
                            p2, p2_b = slot()
                            mm(p2, p2_b, tn[cm].t[:], tn[cm].b, tn[cn_].t[:], tn[cn_].b)
                            S.op("dve", lambda e: e.tensor_copy(out=tn[nn_].t[:], in_=p2), reads=[p2_b], writes=[tn[nn_].b])
                        p3, p3_b = slot()
                        mm(p3, p3_b, tn[nm_].t[:], tn[nm_].b, tn[cp].t[:], tn[cp].b)
                        S.op("dve", lambda e: e.tensor_tensor(out=tn[np_].t[:], in0=p3, in1=tn[cp].t[:], op=ALU.add),
                             reads=[p3_b, tn[cp].b], writes=[tn[np_].b])
                        cm, cn_, cp = nm_, nn_, np_
                    if estop <= 4.4:
                        continue
                    TT = tn[cp]
                    pu_, pu_b = slot()
                    mm(pu_, pu_b, TT.t[:], TT.b, tn["vb"].t[:], tn["vb"].b)
                    S.op("act", lambda e: e.copy(out=tn["u"].t[:], in_=pu_), reads=[pu_b], writes=[tn["u"].b])
                    pw, pw_b = slot()
                    mm(pw, pw_b, tn["kbg"].t[:], tn["kbg"].b, TT.t[:], TT.b)
                    S.op("act", lambda e: e.copy(out=tn["wT"].t[:], in_=pw), reads=[pw_b], writes=[tn["wT"].b])
                    if estop <= 4.5:
                        continue
                    Sh = St[h]
                    pvn, pvn_b = slot()
                    mm(pvn, pvn_b, tn["wT"].t[:], tn["wT"].b, Sh.t[:], Sh.b)
                    S.op("dve", lambda e: e.tensor_tensor(out=tn["vnew"].t[:], in0=tn["u"].t[:], in1=pvn, op=ALU.subtract),
                         reads=[tn["u"].b, pvn_b], writes=[tn["vnew"].b])
                    po_, po_b = slot()
                    mm(po_, po_b, tn["qdT"].t[:], tn["qdT"].b, Sh.t[:], Sh.b, start=True, stop=False)
                    mm(po_, po_b, tn["intraT"].t[:], tn["intraT"].b, tn["vnew"].t[:], tn["vnew"].b, start=False, stop=True)
                    pS, pS_b = slot()
                    mm(pS, pS_b, tn["kdec"].t[:], tn["kdec"].b, tn["vnew"].t[:], tn["vnew"].b)
                    S.op("dve", lambda e: e.scalar_tensor_tensor(out=Sh.t[:], in0=Sh.t[:], scalar=col(64), in1=pS, op0=ALU.mult, op1=ALU.add),
                         reads=[Sh.b, s2[14], pS_b], writes=[Sh.b])
                    if estop <= 4.6:
                        continue
                    S.op("act", lambda e: e.activation(out=tn["junk"].t[:], in_=po_, func=AF.Square, accum_out=sm2.t[:, 84 + h:85 + h]),
                         reads=[po_b], writes=[tn["junk"].b, s2[19]])
                    S.op("dve", lambda e: e.tensor_scalar(out=sm2.t[:, 88 + h:89 + h], in0=sm2.t[:, 84 + h:85 + h], scalar1=1.0 / 128, scalar2=RMS_EPS,
                                                          op0=ALU.mult, op1=ALU.add), reads=[s2[19]], writes=[s2[20]])
                    S.op("act", lambda e: e.activation(out=sm2.t[:, 88 + h:89 + h], in_=sm2.t[:, 88 + h:89 + h], func=AF.Sqrt), reads=[s2[20]], writes=[s2[20]])
                    S.op("dve", lambda e: e.reciprocal(out=sm2.t[:, 92 + h:93 + h], in_=sm2.t[:, 88 + h:89 + h]), reads=[s2[20]], writes=[s2[21]])
                    S.op("dve", lambda e: e.scalar_tensor_tensor(out=tn["yb"].t[:], in0=po_, scalar=sm2.t[:, 92 + h:93 + h], in1=gnw.t[:],
                                                                 op0=ALU.mult, op1=ALU.mult),
                         reads=[po_b, s2[21], gnw.b], writes=[tn["yb"].b])
                    S.op("pool", lambda e: e.tensor_tensor(out=mixt.t[:, 512 + h * 128:512 + (h + 1) * 128], in0=tn["yb"].t[:],
                                                           in1=sz.t[:, h * 128:(h + 1) * 128], op=ALU.mult),
                         reads=[tn["yb"].b, sz.b], writes=[mb])
                if estop < 5:
                    continue
                pT = k.ps[3]
                xTt = k.xTt[k.rr]
                k.rr ^= 1
                for c in range(8):
                    S.op("pe", lambda e: e.transpose(out=pT.t[:, c * 128:(c + 1) * 128], in_=mixt.t[:, c * 128:(c + 1) * 128], identity=ident.t[:]),
                         reads=[mb, ident.b], writes=[pT.bs[c // 4]], inc=(c % 4 == 3))
                S.op("act", lambda e: e.copy(out=xTt.t[:].rearrange("p c t -> p (c t)"), in_=pT.t[:]), reads=pT.bs, writes=[xTt.b])
                po2 = k.ps[0]
                for hb in range(2):
                    for c in range(8):
                        S.op("pe", lambda e: e.matmul(po2.t[:, hb * 512:(hb + 1) * 512], lhsT=xTt.t[:, c, :], rhs=w_out_b.t[:, c, hb * 512:(hb + 1) * 512],
                                                      start=(c == 0), stop=(c == 7)),
                             reads=[xTt.b, w_out_b.b], writes=[po2.bs[hb]], inc=(c == 7))
                epilogue_defer.append((t, po2))
                epilogue_now(k, t, work, lng, lnb, small, sm_b, po2)
        S.barrier()
    k.xcur = k.xres
    k.xcur_is_in = False


epilogue_defer = []


def epilogue_now(k, t, work, lng, lnb, small, sm_b, po2):
    epilogue(k, t, work, lng, lnb, small, sm_b, False, src=po2.t[:], src_bufs=po2.bs)


CONSTS = None
LAST_INPUTS = set()


def layer_inputs(inputs, b):
    m = {"x": np.ascontiguousarray(inputs["x"][b]), "ident": np.eye(128, dtype=np.float32)}
    inv_freq = (np.float32(10000.0) ** (-np.arange(0, 64, 2, dtype=np.float32) / np.float32(64))).astype(np.float32)
    ang = (np.arange(SEQ, dtype=np.float32)[:, None] * inv_freq[None, :]).astype(np.float32)
    cos2T = np.zeros((128, SEQ), np.float32)
    sin2T = np.zeros((128, SEQ), np.float32)
    cos2T[0:32] = np.cos(ang).T
    cos2T[32:64] = np.cos(ang).T
    sin2T[0:32] = np.sin(ang).T
    sin2T[32:64] = np.sin(ang).T
    m["cos2T"] = cos2T
    m["sin2T"] = sin2T
    m["tri"] = np.triu(np.ones((128, 128), np.float32))
    m["maskL"] = np.tril(np.ones((128, 128), np.float32), -1)
    for i in range(2):
        m["even_w_in_%d" % i] = inputs["even_w_in"][i]
        m["even_w_out_%d" % i] = inputs["even_w_out"][i]
        m["even_sgu_wT_%d" % i] = np.ascontiguousarray(np.transpose(inputs["even_sgu_w"][i], (2, 0, 1)))
        m["even_sgu_bT_%d" % i] = np.ascontiguousarray(inputs["even_sgu_b"][i].T)
        m["even_conv_wT_%d" % i] = np.ascontiguousarray(inputs["even_conv_w"][i].T.reshape(12, 128, 4).transpose(1, 0, 2))
        for nm in ("even_sgu_ln_g", "even_sgu_ln_b", "even_a_log", "even_dt_bias", "even_gdn_norm"):
            m["%s_%d" % (nm, i)] = inputs[nm][i][None, :]
    for i in range(2):
        for nm in ("odd_w_in", "odd_w_uq", "odd_w_ukv", "odd_w_out"):
            m["%s_%d" % (nm, i)] = inputs[nm][i]
        for nm in ("odd_q_norm", "odd_kv_norm"):
            m["%s_%d" % (nm, i)] = inputs[nm][i][None, :]
    for l in range(DEPTH):
        for nm in ("moe_w_router", "moe_w_gate_up", "moe_b_gate_up", "moe_w_down", "moe_b_down"):
            m["%s_%d" % (nm, l)] = inputs[nm][l]
        for nm in ("moe_b_router", "ln_ffn_g", "ln_ffn_b", "ln_mix_g", "ln_mix_b"):
            m["%s_%d" % (nm, l)] = inputs[nm][l][None, :]
    return m


def make_consts():
    return {"ident": np.eye(128, dtype=np.float32)}


def kernel(**inputs):
    phases = [("prep",)]
    for l in range(DEPTH):
        phases.append(("even", l) if l % 2 == 0 else ("odd", l))
        phases.append(("moe", l, l == DEPTH - 1))
    nc = build(phases)
    names = set(LAST_INPUTS)
    inputs = {k_: np.asarray(v) for k_, v in inputs.items()}
    n_cores = inputs["x"].shape[0]
    in_maps = []
    for b_ in range(n_cores):
        m = layer_inputs(inputs, b_)
        in_maps.append({k_: np.ascontiguousarray(v, dtype=np.float32) for k_, v in m.items() if k_ in names})
    res = run_bass_kernel_spmd(nc, in_maps, core_ids=list(range(n_cores)))
    return np.stack([np.asarray(r["y"], dtype=np.float32) for r in res.results], axis=0)
```

```python
import contextlib
import numpy as np
import concourse.bass as bass
import concourse.mybir as mybir
from concourse.bass_utils import run_bass_kernel_spmd

F32 = mybir.dt.float32
BF16 = mybir.dt.bfloat16
AF = mybir.ActivationFunctionType
ALU = mybir.AluOpType
AX = mybir.AxisListType

D = 1024
SEQ = 4096
NT = SEQ // 128
DEPTH = 4
NE = 32
ALPHA = (2 * DEPTH) ** 0.25
LN_EPS = 1e-5
TC = 1024
NCH = SEQ // TC
TPC = TC // 128
N_WD = 1
SILU_CAP = 11.913920224372246


class Buf:
    __slots__ = ("w", "r", "excl")

    def __init__(self, excl=False):
        self.w = None
        self.r = {}
        self.excl = excl


class Sched:
    N_DMA_SEMS = 16

    def __init__(self, nc, stack):
        self.nc = nc
        self.eng = {"pe": nc.tensor, "dve": nc.vector, "act": nc.scalar,
                    "pool": nc.gpsimd, "sp": nc.sync}
        self.sems = {}
        self.cnt = {}
        for e in self.eng:
            self.sems[e] = stack.enter_context(nc.semaphore("s_" + e))
            self.cnt[e] = 0
        for i in range(self.N_DMA_SEMS):
            k = ("d", i)
            self.sems[k] = stack.enter_context(nc.semaphore("s_d%d" % i))
            self.cnt[k] = 0
        self.seen = {e: {} for e in self.eng}
        self.dma_rr = 0
        self.n_ins = 0

    def _waits(self, e, reads, writes, extra=()):
        waits = {}

        def add(st):
            if st is None:
                return
            k, v = st
            if e == "pe" and k == "pe":
                return
            assert not (k == e and v > self.cnt[e]), "self-deadlock"
            if v > waits.get(k, 0):
                waits[k] = v
        for b in reads:
            add(b.w)
            if b.excl:
                for k, v in b.r.items():
                    if k != e:
                        add((k, v))
        for b in writes:
            add(b.w)
            for k, v in b.r.items():
                add((k, v))
        for st in extra:
            add(st)
        eng = self.eng[e]
        seen = self.seen[e]
        for k, v in waits.items():
            if seen.get(k, 0) >= v:
                continue
            seen[k] = v
            eng.wait_ge(self.sems[k], v)

    def _stamp(self, stamp, reads, writes):
        k, v = stamp
        for b in reads:
            if b.r.get(k, 0) < v:
                b.r[k] = v
        for b in writes:
            b.w = stamp
            b.r = {}

    def op(self, e, fn, reads=(), writes=(), inc=True):
        self._waits(e, reads, writes)
        ins = fn(self.eng[e])
        self.n_ins += 1
        if inc:
            self.cnt[e] += 1
            ins.then_inc(self.sems[e], 1)
            stamp = (e, self.cnt[e])
        else:
            stamp = (e, self.cnt[e] + 1)
        self._stamp(stamp, reads, writes)
        return ins

    def dma(self, out, in_, reads=(), writes=(), q="sp", **kw):
        i = self.dma_rr
        self.dma_rr = (self.dma_rr + 1) % self.N_DMA_SEMS
        k = ("d", i)
        extra = [(k, self.cnt[k])] if self.cnt[k] else []
        self._waits(q, reads, writes, extra)
        ins = self.eng[q].dma_start(out=out, in_=in_, **kw)
        self.n_ins += 1
        self.cnt[k] += 16
        ins.then_inc(self.sems[k], 16)
        self._stamp((k, self.cnt[k]), reads, writes)
        return ins

    def finish(self, bufs):
        self._waits("sp", bufs, ())

    def barrier(self):
        stamps = [(k, v) for k, v in self.cnt.items() if v > 0]
        for e in self.eng:
            self._waits(e, (), (), stamps)


class T:
    def __init__(self, t, nb=1, excl=False):
        self.t = t
        self.b = Buf(excl)
        self.bs = [Buf(excl) for _ in range(nb)]


class K:
    pass


def build(phases, n_layers=DEPTH, dbg=None, nc=None, ext_ins=None, ext_out=None):
    if nc is None:
        nc = bass.Bass("TRN2", target_bir_lowering=False)
    k = K()
    k.nc = nc
    k.dbg = dbg

    def din(name, shape, dt=F32):
        if ext_ins is not None:
            return ext_ins[name]
        return nc.dram_tensor(name, list(shape), dt, kind="ExternalInput").ap()

    I = {}

    def inp(name, shape, dt=F32):
        if name not in I:
            I[name] = din(name, shape, dt)
        return I[name]
    k.inp = inp
    inp("x", [SEQ, D])
    inp("ident", [128, 128])
    k.I = I
    k.y = ext_out if ext_out is not None else nc.dram_tensor("y", [SEQ, D], F32, kind="ExternalOutput").ap()
    k.xres = nc.dram_tensor("xres", [SEQ, D], F32, kind="Internal").ap()
    k.xT_d = nc.dram_tensor("xT_d", [8, 128, SEQ], BF16, kind="Internal").ap()
    k.xres_b = [Buf() for _ in range(NT)]
    k.xT_b = [Buf() for _ in range(NT)]
    k.y_b = [Buf() for _ in range(NT)]
    k.xcur = I["x"]
    k.xcur_is_in = True

    with contextlib.ExitStack() as st:
        S = Sched(nc, st)
        k.S = S
        k.st = st

        def sb(name, shape, dt=F32, nb=1):
            return T(st.enter_context(nc.sbuf_tensor("sb_" + name, list(shape), dt)), nb)
        k.sb = sb
        k.ps = [T(st.enter_context(nc.psum_tensor("ps%d" % i, [128, 1024], F32)), 2, excl=True)
                for i in range(4)]
        k.ident = sb("ident", [128, 128])
        S.dma(k.ident.t[:], I["ident"], writes=[k.ident.b])
        k.ones = sb("ones", [1, 128])
        S.op("dve", lambda e: e.memset(k.ones.t[:], 1.0), writes=[k.ones.b])
        k.xt = [sb("xt%d" % i, [128, D]) for i in range(2)]
        k.xTt = [sb("xTt%d" % i, [128, 8, 128], BF16) for i in range(2)]
        k.rr = 0

        for ph in phases:
            if ph[0] == "prep":
                prep_phase(k)
            elif ph[0] == "moe":
                moe_phase(k, ph[1], final=ph[2])
            elif ph[0] == "odd":
                odd_phase(k, ph[1])
            elif ph[0] == "even":
                even_phase(k, ph[1])
            elif ph[0] == "dump":
                for t in range(NT):
                    xt = k.xt[t % 2]
                    S.dma(xt.t[:], k.xcur[t * 128:(t + 1) * 128, :], reads=[k.xres_b[t]], writes=[xt.b])
                    S.dma(k.y[t * 128:(t + 1) * 128, :], xt.t[:], reads=[xt.b], writes=[k.y_b[t]])
        S.finish(k.y_b)
        S.barrier()
        global LAST_INPUTS
        LAST_INPUTS = set(I.keys())
        print("instructions:", S.n_ins, "counts:", {kk: v for kk, v in S.cnt.items() if not isinstance(kk, tuple)})
    return nc


def emit_xT(k, t, src, src_b):
    S = k.S
    j = k.rr
    k.rr ^= 1
    ps = k.ps[3]
    for c in range(8):
        S.op("pe", lambda e, c=c: e.transpose(out=ps.t[:, c * 128:(c + 1) * 128],
                                              in_=src[:, c * 128:(c + 1) * 128],
                                              identity=k.ident.t[:]),
             reads=[src_b, k.ident.b], writes=[ps.bs[c // 4]], inc=(c % 4 == 3))
    xTt = k.xTt[j]
    S.op("act", lambda e: e.copy(out=xTt.t[:].rearrange("p c t -> p (c t)"), in_=ps.t[:]),
         reads=ps.bs, writes=[xTt.b])
    S.dma(k.xT_d.rearrange("c p t -> p c t")[:, :, t * 128:(t + 1) * 128], xTt.t[:],
          reads=[xTt.b], writes=[k.xT_b[t]])


def prep_phase(k):
    S = k.S
    for t in range(NT):
        xt = k.xt[t % 2]
        S.dma(xt.t[:], k.xcur[t * 128:(t + 1) * 128, :], writes=[xt.b])
        emit_xT(k, t, xt.t, xt.b)


def moe_phase(k, l, final=False):
    S, nc, sb = k.S, k.nc, k.sb
    I = {}
    I["moe_w_router"] = k.inp("moe_w_router_%d" % l, [D, NE])
    I["moe_b_router"] = k.inp("moe_b_router_%d" % l, [1, NE])
    I["moe_w_gate_up"] = k.inp("moe_w_gate_up_%d" % l, [NE, D, 2 * D])
    I["moe_b_gate_up"] = k.inp("moe_b_gate_up_%d" % l, [NE, 2 * D])
    I["moe_w_down"] = k.inp("moe_w_down_%d" % l, [NE, D, D])
    I["moe_b_down"] = k.inp("moe_b_down_%d" % l, [NE, D])
    I["ln_ffn_g"] = k.inp("ln_ffn_g_%d" % l, [1, D])
    I["ln_ffn_b"] = k.inp("ln_ffn_b_%d" % l, [1, D])
    n_ch = (k.dbg or {}).get("n_ch", NCH)
    n_e = (k.dbg or {}).get("n_e", NE)
    with contextlib.ExitStack() as st:
        def sb(name, shape, dt=F32, nb=1):
            return T(st.enter_context(nc.sbuf_tensor("sbm%d_" % l + name, list(shape), dt)), nb)
        wr_f = sb("wr_f", [128, 8, NE])
        wr_b = sb("wr_b", [128, 8, NE], BF16)
        S.dma(wr_f.t[:], I["moe_w_router"].rearrange("(c p) e -> p c e", p=128), writes=[wr_f.b])
        S.op("dve", lambda e: e.tensor_copy(out=wr_b.t[:], in_=wr_f.t[:]), reads=[wr_f.b], writes=[wr_b.b])
        br = sb("br", [128, NE])
        S.dma(br.t[:], I["moe_b_router"].partition_broadcast(128), writes=[br.b])
        stg = [sb("stg%d" % i, [128, 2 * D]) for i in range(2)]
        bgu = T(stg[0].t[0:NE, :])
        bgu.b = stg[0].b
        S.dma(bgu.t[:], I["moe_b_gate_up"], writes=[bgu.b])
        bguT = sb("bguT", [128, 16, NE])
        ps = k.ps[3]
        for two in range(2):
            for m in range(8):
                i = two * 8 + m
                S.op("pe", lambda e, two=two, m=m, i=i: e.transpose(
                    out=ps.t[:, i * NE:(i + 1) * NE],
                    in_=bgu.t[:, 2 * m * 128 + two:2 * (m + 1) * 128:2],
                    identity=k.ident.t[0:NE, 0:NE]),
                    reads=[bgu.b, k.ident.b], writes=[ps.bs[0]], inc=(i == 15))
        S.op("dve", lambda e: e.tensor_copy(out=bguT.t[:].rearrange("p a b -> p (a b)"), in_=ps.t[:, 0:16 * NE]),
             reads=[ps.bs[0]], writes=[bguT.b])
        bgu2 = sb("bgu2", [128, 16, NE])
        S.op("dve", lambda e: e.tensor_scalar(out=bgu2.t[:, 0:8, :], in0=bguT.t[:, 0:8, :], scalar1=1.702, scalar2=None, op0=ALU.mult),
             reads=[bguT.b], writes=[bgu2.b])
        S.op("dve", lambda e: e.tensor_scalar(out=bgu2.t[:, 8:16, :], in0=bguT.t[:, 8:16, :], scalar1=1.0, scalar2=None, op0=ALU.add),
             reads=[bguT.b], writes=[bgu2.b])
        bd = sb("bd", [128, D])
        S.op("pool", lambda e: e.memset(bd.t[:], 0.0), writes=[bd.b])
        S.dma(bd.t[0:NE, :], I["moe_b_down"], writes=[bd.b])
        lng = sb("lng", [128, D])
        lnb = sb("lnb", [128, D])
        S.dma(lng.t[:], I["ln_ffn_g"].partition_broadcast(128), writes=[lng.b])
        S.dma(lnb.t[:], I["ln_ffn_b"].partition_broadcast(128), writes=[lnb.b])

        stop = (k.dbg or {}).get("stop", 99)
        if stop <= 1:
            S.barrier()
            return
        xTs = sb("xTs", [128, 8, TC], BF16)
        acc = [sb("acc%d" % i, [128, D]) for i in range(TPC)]
        gate = sb("gate", [128, TPC, NE], nb=TPC)
        wgu = [sb("wgu%d" % i, [128, 8, 2 * D], BF16, nb=8) for i in range(2)]
        wd = [sb("wd%d" % i, [128, 8, D], BF16, nb=4) for i in range(N_WD)]
        actb = [sb("actb%d" % i, [128, 8, 512], BF16, nb=8) for i in range(2)]
        tsl = [sb("tsl%d" % i, [128, 512]) for i in range(2)]
        tlc2 = [sb("tlc2%d" % i, [128, 512]) for i in range(2)]
        tslc = [sb("tslc%d" % i, [128, 512]) for i in range(2)]
        pair_i = [0]
        small = sb("small", [128, 64])
        sm_b = [Buf() for _ in range(8)]
        gT = sb("gT", [128, 128])
        S.op("pool", lambda e: e.memset(gT.t[:], 0.0), writes=[gT.b])
        lg = sb("lg", [128, NE])
        lm = sb("lm", [128, NE])

        wgu_src = I["moe_w_gate_up"]
        wd_src = I["moe_w_down"]
        stg_rr = [0]

        def prep_steps(e, wg, wdd):
            steps = []
            for c in range(8):
                def f(c=c):
                    s_ = stg[stg_rr[0]]
                    stg_rr[0] ^= 1
                    S.dma(s_.t[:], wgu_src[e, c * 128:(c + 1) * 128, :], writes=[s_.b])
                    S.op("dve", lambda en: en.tensor_copy(out=wg.t[:, c, 0:D], in_=s_.t[:, 0:2 * D:2]),
                         reads=[s_.b], writes=[wg.bs[c]])
                    S.op("act", lambda en: en.copy(out=wg.t[:, c, D:2 * D], in_=s_.t[:, 1:2 * D:2]),
                         reads=[s_.b], writes=[wg.bs[c]])
                steps.append(f)
            for c2 in range(4):
                def f(c2=c2):
                    s_ = stg[stg_rr[0]]
                    stg_rr[0] ^= 1
                    S.dma(s_.t[:].rearrange("p (c n) -> p c n", c=2),
                          wd_src[e, c2 * 256:(c2 + 1) * 256, :].rearrange("(c p) n -> p c n", p=128),
                          writes=[s_.b])
                    S.op("act", lambda en: en.mul(out=wdd.t[:, 2 * c2:2 * c2 + 2, :].rearrange("p c n -> p (c n)"),
                                                  in_=s_.t[:], mul=1.0 / 1.702),
                         reads=[s_.b], writes=[wdd.bs[c2]])
                steps.append(f)
            return steps

        if not (k.dbg or {}).get("skip_prep"):
            for f in prep_steps(0, wgu[0], wd[0]):
                f()

        for ch in range(n_ch):
            t0 = ch * TPC
            S.dma(xTs.t[:], k.xT_d.rearrange("c p t -> p c t")[:, :, ch * TC:(ch + 1) * TC],
                  reads=k.xT_b[t0:t0 + TPC], writes=[xTs.b])
            for tt in range(TPC):
                pr = k.ps[3]
                for c in range(8):
                    S.op("pe", lambda e, c=c, tt=tt: e.matmul(pr.t[:, 0:NE], lhsT=xTs.t[:, c, tt * 128:(tt + 1) * 128],
                                                             rhs=wr_b.t[:, c, :], start=(c == 0), stop=(c == 7)),
                         reads=[xTs.b, wr_b.b], writes=[pr.bs[0]], inc=(c == 7))
                S.op("dve", lambda e: e.tensor_tensor(out=lg.t[:], in0=pr.t[:, 0:NE], in1=br.t[:], op=ALU.add),
                     reads=[pr.bs[0], br.b], writes=[lg.b])
                if stop <= 2.1:
                    continue
                S.op("dve", lambda e: e.max(out=small.t[:, 0:8], in_=lg.t[:]), reads=[lg.b], writes=[sm_b[0]])
                if stop <= 2.2:
                    continue
                S.op("dve", lambda e: e.tensor_scalar(out=lm.t[:], in0=lg.t[:], scalar1=small.t[:, 3:4], scalar2=None,
                                                      op0=ALU.is_ge), reads=[lg.b, sm_b[0]], writes=[lm.b])
                S.op("dve", lambda e: e.tensor_scalar(out=small.t[:, 8:9], in0=small.t[:, 0:1], scalar1=-1.0,
                                                      scalar2=None, op0=ALU.mult), reads=[sm_b[0]], writes=[sm_b[1]])
                S.op("act", lambda e: e.activation(out=lg.t[:], in_=lg.t[:], func=AF.Exp, bias=small.t[:, 8:9], scale=1.0),
                     reads=[lg.b, sm_b[1]], writes=[lg.b])
                S.op("dve", lambda e: e.scalar_tensor_tensor(out=lm.t[:], in0=lg.t[:], scalar=1.0, in1=lm.t[:],
                                                             op0=ALU.mult, op1=ALU.mult, accum_out=small.t[:, 9:10]),
                     reads=[lg.b, lm.b], writes=[lm.b, sm_b[2]])
                S.op("dve", lambda e: e.reciprocal(out=small.t[:, 10:11], in_=small.t[:, 9:10]), reads=[sm_b[2]], writes=[sm_b[3]])
                S.op("dve", lambda e, tt=tt: e.tensor_scalar(out=gate.t[:, tt, :], in0=lm.t[:], scalar1=small.t[:, 10:11],
                                                             scalar2=None, op0=ALU.mult),
                     reads=[lm.b, sm_b[3]], writes=[gate.bs[tt]])
                if stop <= 2.3:
                    continue
                pt = k.ps[3]
                S.op("pe", lambda e, tt=tt: e.transpose(out=pt.t[0:NE, 512:640], in_=gate.t[:, tt, :], identity=k.ident.t[:]),
                     reads=[gate.bs[tt], k.ident.b], writes=[pt.bs[1]])
                S.op("dve", lambda e: e.tensor_copy(out=gT.t[0:NE, :], in_=pt.t[0:NE, 512:640]), reads=[pt.bs[1]], writes=[gT.b])
                if stop <= 2.4:
                    continue
                pa = k.ps[2]
                for h in range(2):
                    S.op("pe", lambda e, h=h: e.matmul(pa.t[:, h * 512:(h + 1) * 512], lhsT=gT.t[:], rhs=bd.t[:, h * 512:(h + 1) * 512],
                                                       start=True, stop=True),
                         reads=[gT.b, bd.b], writes=[pa.bs[h]])
                S.op("act", lambda e, tt=tt: e.copy(out=acc[tt].t[:], in_=pa.t[:]), reads=pa.bs, writes=[acc[tt].b])

            if stop < 3:
                continue
            def emit_gu(e_, sub, steps):
                wg = wgu[e_ % 2]
                ab = actb[sub % 2]
                tok = slice(sub * 512, (sub + 1) * 512)
                si = 0
                for m in range(8):
                    pg = k.ps[m % 2]
                    for half in range(2):
                        for c in range(8):
                            S.op("pe", lambda e: e.matmul(
                                pg.t[:, half * 512:(half + 1) * 512],
                                lhsT=wg.t[:, c, half * D + m * 128: half * D + (m + 1) * 128],
                                rhs=xTs.t[:, c, tok], start=(c == 0), stop=(c == 7)),
                                reads=[wg.bs[c], xTs.b], writes=[pg.bs[half]], inc=(c == 7))
                    bg = bgu2.t[:, m, e_:e_ + 1]
                    bl = bgu2.t[:, 8 + m, e_:e_ + 1]
                    pi_ = pair_i[0] % 2
                    pair_i[0] += 1
                    sl_, lc_, slc_ = tsl[pi_], tlc2[pi_], tslc[pi_]
                    S.op("act", lambda e: e.activation(out=sl_.t[:], in_=pg.t[:, 0:512], func=AF.Silu, bias=bg, scale=1.702),
                         reads=[pg.bs[0], bgu2.b], writes=[sl_.b])
                    S.op("dve", lambda e: e.tensor_scalar(out=lc_.t[:], in0=pg.t[:, 512:1024], scalar1=bl, scalar2=8.0,
                                                          op0=ALU.add, op1=ALU.min),
                         reads=[pg.bs[1], bgu2.b], writes=[lc_.b])
                    S.op("pool", lambda e: e.tensor_scalar(out=slc_.t[:], in0=sl_.t[:], scalar1=SILU_CAP, scalar2=-3.0e38,
                                                           op0=ALU.min, op1=ALU.max),
                         reads=[sl_.b], writes=[slc_.b])
                    S.op("dve", lambda e: e.scalar_tensor_tensor(out=ab.t[:, m, :], in0=lc_.t[:], scalar=-6.0, in1=slc_.t[:],
                                                                 op0=ALU.max, op1=ALU.mult),
                         reads=[lc_.b, slc_.b], writes=[ab.bs[m]])
                    if si < len(steps):
                        steps[si]()
                        si += 1
                while si < len(steps):
                    steps[si]()
                    si += 1

            def emit_down(e_, sub):
                ab = actb[sub % 2]
                wdd = wd[e_ % len(wd)]
                for j in range(4):
                    tt = sub * 4 + j
                    pd = k.ps[2]
                    for h in range(2):
                        for c in range(8):
                            S.op("pe", lambda e: e.matmul(
                                pd.t[:, h * 512:(h + 1) * 512],
                                lhsT=ab.t[:, c, j * 128:(j + 1) * 128],
                                rhs=wdd.t[:, c, h * 512:(h + 1) * 512], start=(c == 0), stop=(c == 7)),
                                reads=[ab.bs[c], wdd.bs[c // 2]], writes=[pd.bs[h]], inc=(c == 7))
                        S.op("dve", lambda e: e.scalar_tensor_tensor(
                            out=acc[tt].t[:, h * 512:(h + 1) * 512], in0=pd.t[:, h * 512:(h + 1) * 512],
                            scalar=gate.t[:, tt, e_:e_ + 1], in1=acc[tt].t[:, h * 512:(h + 1) * 512],
                            op0=ALU.mult, op1=ALU.add),
                            reads=[pd.bs[h], gate.bs[tt], acc[tt].b], writes=[acc[tt].b])

            blocks = [(e_, sub) for e_ in range(n_e) for sub in range(TC // 512)]
            d_for = {}
            last_sub = TC // 512 - 1
            for bi, (e_, sub) in enumerate(blocks):
                steps = []
                if sub == 0:
                    if e_ + 1 < n_e:
                        nx = prep_steps(e_ + 1, wgu[(e_ + 1) % 2], wd[0])
                    elif ch + 1 < n_ch:
                        nx = prep_steps(0, wgu[0], wd[0])
                    else:
                        nx = []
                    steps, d_for[e_ + 1] = nx[:8], nx[8:]
                emit_gu(e_, sub, steps)
                if bi >= 1:
                    pe_, psub = blocks[bi - 1]
                    emit_down(pe_, psub)
                    if psub == last_sub:
                        for f in d_for.pop(pe_ + 1, []):
                            f()
            pe_, psub = blocks[-1]
            emit_down(pe_, psub)
            for f in d_for.pop(pe_ + 1, []):
                f()
            if stop < 4:
                continue
            for tt in range(TPC):
                t = t0 + tt
                epilogue(k, t, acc[tt], lng, lnb, small, sm_b, final)
        S.barrier()
    k.xcur = k.xres
    k.xcur_is_in = False


def epilogue(k, t, acc, lng, lnb, small, sm_b, final, src=None, src_bufs=None):
    S = k.S
    xt = k.xt[t % 2]
    src_b = k.xres_b[t] if not k.xcur_is_in else Buf()
    if src is None:
        src, src_bufs = acc.t[:], [acc.b]
    S.dma(xt.t[:], k.xcur[t * 128:(t + 1) * 128, :], reads=[src_b], writes=[xt.b])
    S.op("dve", lambda e: e.scalar_tensor_tensor(out=acc.t[:], in0=xt.t[:], scalar=ALPHA, in1=src,
                                                 op0=ALU.mult, op1=ALU.add),
         reads=[xt.b] + list(src_bufs), writes=[acc.b])
    S.op("dve", lambda e: e.bn_stats(out=small.t[:, 16:22], in_=acc.t[:, 0:512]), reads=[acc.b], writes=[sm_b[4]])
    S.op("dve", lambda e: e.bn_stats(out=small.t[:, 22:28], in_=acc.t[:, 512:1024]), reads=[acc.b], writes=[sm_b[5]])
    S.op("dve", lambda e: e.bn_aggr(out=small.t[:, 28:30], in_=small.t[:, 16:28]), reads=[sm_b[4], sm_b[5]], writes=[sm_b[6]])
    S.op("dve", lambda e: e.tensor_scalar(out=small.t[:, 30:31], in0=small.t[:, 29:30], scalar1=LN_EPS, scalar2=None, op0=ALU.add),
         reads=[sm_b[6]], writes=[sm_b[7]])
    S.op("act", lambda e: e.activation(out=small.t[:, 31:32], in_=small.t[:, 30:31], func=AF.Sqrt),
         reads=[sm_b[7]], writes=[sm_b[7]])
    S.op("dve", lambda e: e.reciprocal(out=small.t[:, 32:33], in_=small.t[:, 31:32]), reads=[sm_b[7]], writes=[sm_b[7]])
    S.op("dve", lambda e: e.tensor_scalar(out=acc.t[:], in0=acc.t[:], scalar1=small.t[:, 28:29], scalar2=small.t[:, 32:33],
                                          op0=ALU.subtract, op1=ALU.mult),
         reads=[acc.b, sm_b[6], sm_b[7]], writes=[acc.b])
    S.op("pool", lambda e: e.tensor_tensor(out=acc.t[:], in0=acc.t[:], in1=lng.t[:], op=ALU.mult),
         reads=[acc.b, lng.b], writes=[acc.b])
    S.op("pool", lambda e: e.tensor_tensor(out=xt.t[:], in0=acc.t[:], in1=lnb.t[:], op=ALU.add),
         reads=[acc.b, lnb.b], writes=[xt.b])
    if final:
        S.dma(k.y[t * 128:(t + 1) * 128, :], xt.t[:], reads=[xt.b], writes=[k.y_b[t]])
    else:
        S.dma(k.xres[t * 128:(t + 1) * 128, :], xt.t[:], reads=[xt.b], writes=[k.xres_b[t]])
        emit_xT(k, t, xt.t, xt.b)


QL, KVL, ROPE = 384, 256, 64
QK_SCALE = 192.0 ** -0.5
RMS_EPS = 1e-6


def load_cast(k, S, stg, dst_fn, src_ap, width, eng="act"):
    S.dma(stg.t[:, 0:width], src_ap, writes=[stg.b])


def odd_phase(k, l):
    S, nc = k.S, k.nc
    i = l // 2
    w_in = k.inp("odd_w_in_%d" % i, [D, 704])
    q_norm = k.inp("odd_q_norm_%d" % i, [1, QL])
    w_uq = k.inp("odd_w_uq_%d" % i, [QL, 1536])
    kv_norm = k.inp("odd_kv_norm_%d" % i, [1, KVL])
    w_ukv = k.inp("odd_w_ukv_%d" % i, [KVL, 2048])
    w_out = k.inp("odd_w_out_%d" % i, [D, D])
    ln_g = k.inp("ln_mix_g_%d" % l, [1, D])
    ln_b = k.inp("ln_mix_b_%d" % l, [1, D])
    cos2T = k.inp("cos2T", [128, SEQ])
    sin2T = k.inp("sin2T", [128, SEQ])
    oT_d = nc.dram_tensor("oT_d_%d" % l, [8, 128, SEQ], BF16, kind="Internal").ap()
    oT_b = [Buf() for _ in range(8)]
    n_h = (k.dbg or {}).get("n_h", 8)
    with contextlib.ExitStack() as st:
        def sb(name, shape, dt=F32, nb=1):
            return T(st.enter_context(nc.sbuf_tensor("sbo%d_" % l + name, list(shape), dt)), nb)
        w_in_b = sb("w_in_b", [128, 8, 704], BF16)
        wkp = sb("wkp", [128, 8, 128], BF16)
        wkr = sb("wkr", [128, 8, 128], BF16)
        w_uq_b = sb("w_uq_b", [128, 3, 1536], BF16)
        wqr = sb("wqr", [128, 3, 8, 128], BF16)
        wqrot = sb("wqrot", [128, 3, 8, 128], BF16)
        w_ukv_b = sb("w_ukv_b", [128, 2, 2048], BF16)
        qn_bc = sb("qn_bc", [128, QL])
        kvn_bc = sb("kvn_bc", [128, KVL])
        lng = sb("lng", [128, D])
        lnb = sb("lnb", [128, D])
        ones_b = sb("ones_b", [128, 128], BF16)
        S.op("pool", lambda e: e.memset(ones_b.t[:], 1.0), writes=[ones_b.b])
        S.dma(qn_bc.t[:], q_norm.partition_broadcast(128), writes=[qn_bc.b])
        S.dma(kvn_bc.t[:], kv_norm.partition_broadcast(128), writes=[kvn_bc.b])
        S.dma(lng.t[:], ln_g.partition_broadcast(128), writes=[lng.b])
        S.dma(lnb.t[:], ln_b.partition_broadcast(128), writes=[lnb.b])
        cqnT = sb("cqnT", [128, 3, SEQ], BF16, nb=8)
        ckvnT = sb("ckvnT", [128, 2, SEQ], BF16, nb=8)
        kpeT = sb("kpeT", [128, SEQ], BF16, nb=8)
        xTc = [sb("xTc%d" % j, [128, 8, 512], BF16) for j in range(2)]
        cosc = [sb("cosc%d" % j, [128, 512]) for j in range(2)]
        sinc = [sb("sinc%d" % j, [128, 512]) for j in range(2)]
        tmp1 = sb("tmp1", [128, 512])
        tmp2 = sb("tmp2", [128, 512])
        junk = sb("junk", [128, 512])
        cn = sb("cn", [128, 640])
        small = sb("small", [128, 64])
        sm_b = [Buf() for _ in range(8)]
        ssb = [Buf() for _ in range(4)]
        rr = [0]
        stA = contextlib.ExitStack()
        stg = [T(stA.enter_context(nc.sbuf_tensor("sbo%d_stg%d" % (l, j), [128, 2048], F32))) for j in range(2)]

        def ld(dst_ap, src_ap, shape3, dstT):
            s_ = stg[rr[0]]
            rr[0] ^= 1
            c_, n_ = shape3
            S.dma(s_.t[:, 0:c_ * n_].rearrange("p (c n) -> p c n", c=c_), src_ap, writes=[s_.b])
            S.op("act", lambda e: e.copy(out=dst_ap, in_=s_.t[:, 0:c_ * n_].rearrange("p (c n) -> p c n", c=c_)),
                 reads=[s_.b], writes=[dstT.b])
        for c2 in range(4):
            ld(w_in_b.t[:, 2 * c2:2 * c2 + 2, :], w_in[c2 * 256:(c2 + 1) * 256, :].rearrange("(c p) n -> p c n", p=128), (2, 704), w_in_b)
        for j in range(3):
            ld(w_uq_b.t[:, j:j + 1, :], w_uq[j * 128:(j + 1) * 128, :].rearrange("(c p) n -> p c n", p=128), (1, 1536), w_uq_b)
        for j in range(2):
            ld(w_ukv_b.t[:, j:j + 1, :], w_ukv[j * 128:(j + 1) * 128, :].rearrange("(c p) n -> p c n", p=128), (1, 2048), w_ukv_b)
        S.barrier()
        stA.close()
        S.op("pool", lambda e: e.memset(wkp.t[:], 0.0), writes=[wkp.b])
        S.op("pool", lambda e: e.memset(wkr.t[:], 0.0), writes=[wkr.b])
        S.op("pool", lambda e: e.memset(wqr.t[:], 0.0), writes=[wqr.b])
        S.op("pool", lambda e: e.memset(wqrot.t[:], 0.0), writes=[wqrot.b])
        S.op("dve", lambda e: e.tensor_copy(out=wkp.t[:, :, 0:64], in_=w_in_b.t[:, :, 640:704]), reads=[w_in_b.b], writes=[wkp.b])
        S.op("dve", lambda e: e.tensor_scalar(out=wkr.t[:, :, 0:32], in0=w_in_b.t[:, :, 672:704], scalar1=-1.0, scalar2=None, op0=ALU.mult),
             reads=[w_in_b.b], writes=[wkr.b])
        S.op("dve", lambda e: e.tensor_copy(out=wkr.t[:, :, 32:64], in_=w_in_b.t[:, :, 640:672]), reads=[w_in_b.b], writes=[wkr.b])
        for j in range(3):
            v_ = w_uq_b.t[:, j, :].rearrange("p (h d) -> p h d", h=8)
            S.op("dve", lambda e: e.tensor_copy(out=wqr.t[:, j, :, 0:64], in_=v_[:, :, 128:192]), reads=[w_uq_b.b], writes=[wqr.b])
            S.op("dve", lambda e: e.tensor_scalar(out=wqrot.t[:, j, :, 0:32], in0=v_[:, :, 160:192], scalar1=-1.0, scalar2=None, op0=ALU.mult),
                 reads=[w_uq_b.b], writes=[wqrot.b])
            S.op("dve", lambda e: e.tensor_copy(out=wqrot.t[:, j, :, 32:64], in_=v_[:, :, 128:160]), reads=[w_uq_b.b], writes=[wqrot.b])


        def rope_combine(pa, pa_b, pb, pb_b, cc, sc, dst_ap, dst_b):
            S.op("dve", lambda e: e.tensor_tensor(out=tmp1.t[:], in0=pa, in1=cc.t[:], op=ALU.mult),
                 reads=[pa_b, cc.b], writes=[tmp1.b])
            S.op("dve", lambda e: e.tensor_tensor(out=tmp2.t[:], in0=pb, in1=sc.t[:], op=ALU.mult),
                 reads=[pb_b, sc.b], writes=[tmp2.b])
            S.op("pool", lambda e: e.tensor_tensor(out=dst_ap, in0=tmp1.t[:], in1=tmp2.t[:], op=ALU.add),
                 reads=[tmp1.b, tmp2.b], writes=[dst_b])

        ostop = (k.dbg or {}).get("ostop", 99)
        for ch in range(8 if ostop >= 1 else 0):
            xc = xTc[ch % 2]
            cc, sc = cosc[ch % 2], sinc[ch % 2]
            tok = slice(ch * 512, (ch + 1) * 512)
            S.dma(xc.t[:], k.xT_d.rearrange("c p t -> p c t")[:, :, tok], reads=k.xT_b[ch * 4:ch * 4 + 4], writes=[xc.b])
            S.dma(cc.t[:], cos2T[:, tok], writes=[cc.b])
            S.dma(sc.t[:], sin2T[:, tok], writes=[sc.b])
            for j4 in range(4):
                t = ch * 4 + j4
                tl_ = slice(j4 * 128, (j4 + 1) * 128)
                pp = k.ps[j4 % 2]
                for (a, b_, hb) in ((0, 512, 0), (512, 704, 1)):
                    for c in range(8):
                        S.op("pe", lambda e: e.matmul(pp.t[:, a:b_], lhsT=xc.t[:, c, tl_], rhs=w_in_b.t[:, c, a:b_],
                                                      start=(c == 0), stop=(c == 7)),
                             reads=[xc.b, w_in_b.b], writes=[pp.bs[hb]], inc=(c == 7))
                S.op("act", lambda e: e.activation(out=junk.t[:, 0:QL], in_=pp.t[:, 0:QL], func=AF.Square, accum_out=small.t[:, 0:1]),
                     reads=[pp.bs[0]], writes=[junk.b, ssb[0]])
                S.op("act", lambda e: e.activation(out=junk.t[:, 0:KVL], in_=pp.t[:, QL:QL + KVL], func=AF.Square, accum_out=small.t[:, 1:2]),
                     reads=pp.bs, writes=[junk.b, ssb[1]])
                S.op("dve", lambda e: e.tensor_scalar(out=small.t[:, 2:3], in0=small.t[:, 0:1], scalar1=1.0 / QL, scalar2=RMS_EPS,
                                                      op0=ALU.mult, op1=ALU.add), reads=[ssb[0]], writes=[ssb[2]])
                S.op("dve", lambda e: e.tensor_scalar(out=small.t[:, 3:4], in0=small.t[:, 1:2], scalar1=1.0 / KVL, scalar2=RMS_EPS,
                                                      op0=ALU.mult, op1=ALU.add), reads=[ssb[1]], writes=[ssb[2]])
                S.op("act", lambda e: e.activation(out=small.t[:, 4:6], in_=small.t[:, 2:4], func=AF.Sqrt), reads=[ssb[2]], writes=[ssb[3]])
                S.op("dve", lambda e: e.reciprocal(out=small.t[:, 6:8], in_=small.t[:, 4:6]), reads=[ssb[3]], writes=[ssb[3]])
                S.op("dve", lambda e: e.scalar_tensor_tensor(out=cn.t[:, 0:QL], in0=pp.t[:, 0:QL], scalar=small.t[:, 6:7], in1=qn_bc.t[:],
                                                             op0=ALU.mult, op1=ALU.mult),
                     reads=[pp.bs[0], ssb[3], qn_bc.b], writes=[cn.b])
                S.op("dve", lambda e: e.scalar_tensor_tensor(out=cn.t[:, QL:640], in0=pp.t[:, QL:640], scalar=small.t[:, 7:8], in1=kvn_bc.t[:],
                                                             op0=ALU.mult, op1=ALU.mult),
                     reads=pp.bs + [ssb[3], kvn_bc.b], writes=[cn.b])
                pt = k.ps[2]
                for j in range(5):
                    S.op("pe", lambda e: e.transpose(out=pt.t[:, j * 128:(j + 1) * 128], in_=cn.t[:, j * 128:(j + 1) * 128], identity=k.ident.t[:]),
                         reads=[cn.b, k.ident.b], writes=[pt.bs[j // 4]], inc=(j in (3, 4)))
                S.op("act", lambda e: e.copy(out=cqnT.t[:, :, t * 128:(t + 1) * 128], in_=pt.t[:, 0:384].rearrange("p (j t) -> p j t", j=3)),
                     reads=[pt.bs[0]], writes=[cqnT.bs[ch]])
                S.op("act", lambda e: e.copy(out=ckvnT.t[:, :, t * 128:(t + 1) * 128], in_=pt.t[:, 384:640].rearrange("p (j t) -> p j t", j=2)),
                     reads=pt.bs, writes=[ckvnT.bs[ch]])
            pk = k.ps[3]
            for (wt, hb) in ((wkp, 0), (wkr, 1)):
                for c in range(8):
                    S.op("pe", lambda e: e.matmul(pk.t[:, hb * 512:(hb + 1) * 512], lhsT=wt.t[:, c, :], rhs=xc.t[:, c, :],
                                                  start=(c == 0), stop=(c == 7)),
                         reads=[wt.b, xc.b], writes=[pk.bs[hb]], inc=(c == 7))
            rope_combine(pk.t[:, 0:512], pk.bs[0], pk.t[:, 512:1024], pk.bs[1], cc, sc, kpeT.t[:, tok], kpeT.bs[ch])

        stB = contextlib.ExitStack()

        def sb(name, shape, dt=F32, nb=1):
            return T(stB.enter_context(nc.sbuf_tensor("sbo%d_" % l + name, list(shape), dt)), nb)
        qn = sb("qn", [128, SEQ], BF16, nb=8)
        qr = sb("qr", [128, SEQ], BF16, nb=8)
        kn = sb("kn", [128, SEQ], BF16, nb=8)
        vv = sb("vv", [128, NT, 128], BF16, nb=8)
        oTh = sb("oTh", [128, SEQ], BF16)
        sq = sb("sq", [128, 512], BF16)
        PT = [sb("PT%d" % j, [128, 512], BF16) for j in range(2)]
        rs = sb("rs", [128, 128])
        mq = sb("mq", [128, 16])
        negB = sb("negB", [128, 8])
        for h in range(n_h if ostop >= 2 else 0):
            for ch in range(8):
                tok = slice(ch * 512, (ch + 1) * 512)
                cc, sc = cosc[ch % 2], sinc[ch % 2]
                S.dma(cc.t[:], cos2T[:, tok], writes=[cc.b])
                S.dma(sc.t[:], sin2T[:, tok], writes=[sc.b])
                p0 = k.ps[0]
                for j in range(3):
                    S.op("pe", lambda e: e.matmul(p0.t[:, 0:512], lhsT=w_uq_b.t[:, j, h * 192:h * 192 + 128], rhs=cqnT.t[:, j, tok],
                                                  start=(j == 0), stop=(j == 2)),
                         reads=[w_uq_b.b, cqnT.bs[ch]], writes=[p0.bs[0]], inc=(j == 2))
                S.op("act", lambda e: e.copy(out=qn.t[:, tok], in_=p0.t[:, 0:512]), reads=[p0.bs[0]], writes=[qn.bs[ch]])
                for j in range(2):
                    S.op("pe", lambda e: e.matmul(p0.t[:, 512:1024], lhsT=w_ukv_b.t[:, j, h * 256:h * 256 + 128], rhs=ckvnT.t[:, j, tok],
                                                  start=(j == 0), stop=(j == 1)),
                         reads=[w_ukv_b.b, ckvnT.bs[ch]], writes=[p0.bs[1]], inc=(j == 1))
                S.op("act", lambda e: e.copy(out=kn.t[:, tok], in_=p0.t[:, 512:1024]), reads=[p0.bs[1]], writes=[kn.bs[ch]])
                p1 = k.ps[1]
                for (wt, hb) in ((wqr, 0), (wqrot, 1)):
                    for j in range(3):
                        S.op("pe", lambda e: e.matmul(p1.t[:, hb * 512:(hb + 1) * 512], lhsT=wt.t[:, j, h, :], rhs=cqnT.t[:, j, tok],
                                                      start=(j == 0), stop=(j == 2)),
                             reads=[wt.b, cqnT.bs[ch]], writes=[p1.bs[hb]], inc=(j == 2))
                rope_combine(p1.t[:, 0:512], p1.bs[0], p1.t[:, 512:1024], p1.bs[1], cc, sc, qr.t[:, tok], qr.bs[ch])
                p2 = k.ps[2]
                for j4 in range(4):
                    t = ch * 4 + j4
                    for j in range(2):
                        S.op("pe", lambda e: e.matmul(p2.t[:, j4 * 128:(j4 + 1) * 128], lhsT=ckvnT.t[:, j, t * 128:(t + 1) * 128],
                                                      rhs=w_ukv_b.t[:, j, h * 256 + 128:h * 256 + 256], start=(j == 0), stop=(j == 1)),
                             reads=[w_ukv_b.b, ckvnT.bs[ch]], writes=[p2.bs[0]], inc=(j == 1 and j4 == 3))
                S.op("act", lambda e: e.copy(out=vv.t[:, ch * 4:ch * 4 + 4, :], in_=p2.t[:, 0:512].rearrange("p (a b) -> p a b", a=4)),
                     reads=[p2.bs[0]], writes=[vv.bs[ch]])
                p3 = k.ps[3]
                for half, (srcs) in enumerate((((qn, qn.bs[ch]), (qr, qr.bs[ch])), ((kn, kn.bs[ch]), (kpeT, kpeT.bs[ch])))):
                    for si, (tt_, tb_) in enumerate(srcs):
                        S.op("act", lambda e: e.activation(out=sq.t[:], in_=tt_.t[:, tok], func=AF.Square), reads=[tb_], writes=[sq.b])
                        S.op("pe", lambda e: e.matmul(p3.t[:, half * 512:(half + 1) * 512], lhsT=ones_b.t[:], rhs=sq.t[:],
                                                      start=(si == 0), stop=(si == 1)),
                             reads=[ones_b.b, sq.b], writes=[p3.bs[half]])
                    S.op("dve", lambda e: e.tensor_reduce(out=mq.t[:, half * 8 + ch:half * 8 + ch + 1], in_=p3.t[:, half * 512:(half + 1) * 512],
                                                          axis=AX.X, op=ALU.max),
                         reads=[p3.bs[half]], writes=[mq.b])
            S.op("dve", lambda e: e.tensor_reduce(out=small.t[:, 8:9], in_=mq.t[:, 0:8], axis=AX.X, op=ALU.max), reads=[mq.b], writes=[sm_b[0]])
            S.op("dve", lambda e: e.tensor_reduce(out=small.t[:, 9:10], in_=mq.t[:, 8:16], axis=AX.X, op=ALU.max), reads=[mq.b], writes=[sm_b[0]])
            S.op("dve", lambda e: e.tensor_tensor(out=small.t[:, 10:11], in0=small.t[:, 8:9], in1=small.t[:, 9:10], op=ALU.mult),
                 reads=[sm_b[0]], writes=[sm_b[1]])
            S.op("act", lambda e: e.activation(out=small.t[:, 11:12], in_=small.t[:, 10:11], func=AF.Sqrt), reads=[sm_b[1]], writes=[sm_b[1]])
            S.op("dve", lambda e: e.tensor_scalar(out=negB.t[:, h:h + 1], in0=small.t[:, 11:12], scalar1=-QK_SCALE, scalar2=None, op0=ALU.mult),
                 reads=[sm_b[1]], writes=[negB.b])
            gi = 0
            for qt in range(NT if ostop >= 3 else 0):
                qs = slice(qt * 128, (qt + 1) * 128)
                po = k.ps[2 + (qt % 2)]
                nblk = qt + 1
                for g0 in range(0, nblk, 4):
                    nb_ = min(4, nblk - g0)
                    psS = k.ps[(gi // 2) % 2]
                    hb = gi % 2
                    pt_ = PT[gi % 2]
                    gi += 1
                    for bi in range(nb_):
                        kb = g0 + bi
                        ks = slice(kb * 128, (kb + 1) * 128)
                        o_ = psS.t[:, hb * 512 + bi * 128:hb * 512 + (bi + 1) * 128]
                        S.op("pe", lambda e: e.matmul(o_, lhsT=kn.t[:, ks], rhs=qn.t[:, qs], start=True, stop=False),
                             reads=[kn.bs[kb // 4], qn.bs[qt // 4]], writes=[psS.bs[hb]], inc=False)
                        S.op("pe", lambda e: e.matmul(o_, lhsT=kpeT.t[:, ks], rhs=qr.t[:, qs], start=False, stop=True),
                             reads=[kpeT.bs[kb // 4], qr.bs[qt // 4]], writes=[psS.bs[hb]], inc=(bi == nb_ - 1))
                    S.op("act", lambda e: e.activation(out=pt_.t[:, 0:nb_ * 128], in_=psS.t[:, hb * 512:hb * 512 + nb_ * 128], func=AF.Exp,
                                                       bias=negB.t[:, h:h + 1], scale=QK_SCALE),
                         reads=[psS.bs[hb], negB.b], writes=[pt_.b])
                    if g0 + nb_ == nblk:
                        bi = nb_ - 1
                        S.op("pool", lambda e: e.memset(pt_.t[64:128, bi * 128:bi * 128 + 64], 0.0), writes=[pt_.b])
                    for bi in range(nb_):
                        kb = g0 + bi
                        S.op("pe", lambda e: e.matmul(po.t[:, 0:128], lhsT=vv.t[:, kb, :], rhs=pt_.t[:, bi * 128:(bi + 1) * 128],
                                                      start=(kb == 0), stop=(kb == nblk - 1)),
                             reads=[vv.bs[kb // 4], pt_.b], writes=[po.bs[0]], inc=False)
                        S.op("pe", lambda e: e.matmul(po.t[:, 128:256], lhsT=ones_b.t[:], rhs=pt_.t[:, bi * 128:(bi + 1) * 128],
                                                      start=False, stop=(kb == nblk - 1)),
                             reads=[ones_b.b, pt_.b], writes=[po.bs[0]], inc=(bi == nb_ - 1))
                S.op("dve", lambda e: e.reciprocal(out=rs.t[:], in_=po.t[:, 128:256]), reads=[po.bs[0]], writes=[rs.b])
                S.op("dve", lambda e: e.tensor_tensor(out=oTh.t[:, qs], in0=po.t[:, 0:128], in1=rs.t[:], op=ALU.mult),
                     reads=[po.bs[0], rs.b], writes=[oTh.b])
            S.dma(oT_d[h], oTh.t[:], reads=[oTh.b], writes=[oT_b[h]])

        S.barrier()
        stB.close()

        def sb(name, shape, dt=F32, nb=1):
            return T(st.enter_context(nc.sbuf_tensor("sbo%d_" % l + name, list(shape), dt)), nb)
        stg = [sb("stgC", [128, 2048])]
        rr[0] = 0
        w_out_b = sb("w_out_b", [128, 8, D], BF16)
        for c2 in range(4):
            ld(w_out_b.t[:, 2 * c2:2 * c2 + 2, :], w_out[c2 * 256:(c2 + 1) * 256, :].rearrange("(c p) n -> p c n", p=128), (2, D), w_out_b)
            rr[0] = 0
        work = [sb("work%d" % j, [128, D]) for j in range(1)]
        for ch in range(8 if ostop >= 4 else 0):
            xc = xTc[ch % 2]
            tok = slice(ch * 512, (ch + 1) * 512)
            S.dma(xc.t[:], oT_d.rearrange("c p t -> p c t")[:, :, tok], reads=oT_b, writes=[xc.b])
            for j4 in range(4):
                t = ch * 4 + j4
                pp = k.ps[j4 % 2]
                for hb in range(2):
                    for c in range(8):
                        S.op("pe", lambda e: e.matmul(pp.t[:, hb * 512:(hb + 1) * 512], lhsT=xc.t[:, c, j4 * 128:(j4 + 1) * 128],
                                                      rhs=w_out_b.t[:, c, hb * 512:(hb + 1) * 512], start=(c == 0), stop=(c == 7)),
                             reads=[xc.b, w_out_b.b], writes=[pp.bs[hb]], inc=(c == 7))
                epilogue(k, t, work[0], lng, lnb, small, sm_b, False, src=pp.t[:], src_bufs=pp.bs)
        S.barrier()
    k.xcur = k.xres
    k.xcur_is_in = False


def even_phase(k, l):
    S, nc = k.S, k.nc
    i = l // 2
    w_in = k.inp("even_w_in_%d" % i, [D, 3080])
    w_out = k.inp("even_w_out_%d" % i, [D, D])
    sgT = k.inp("even_sgu_wT_%d" % i, [128, 4, 128])
    bsT_d = k.inp("even_sgu_bT_%d" % i, [128, 4])
    sg_d = k.inp("even_sgu_ln_g_%d" % i, [1, 512])
    sb_d = k.inp("even_sgu_ln_b_%d" % i, [1, 512])
    cw_d = k.inp("even_conv_wT_%d" % i, [128, 12, 4])
    alog_d = k.inp("even_a_log_%d" % i, [1, 4])
    dtb_d = k.inp("even_dt_bias_%d" % i, [1, 4])
    gnw_d = k.inp("even_gdn_norm_%d" % i, [1, 128])
    ln_g = k.inp("ln_mix_g_%d" % l, [1, D])
    ln_b = k.inp("ln_mix_b_%d" % l, [1, D])
    tri_d = k.inp("tri", [128, 128])
    maskL_d = k.inp("maskL", [128, 128])
    n_chunks = (k.dbg or {}).get("e_ch", 8)
    with contextlib.ExitStack() as st:
        def sb(name, shape, dt=F32, nb=1):
            return T(st.enter_context(nc.sbuf_tensor("sbe%d_" % l + name, list(shape), dt)), nb)
        w_in_b = sb("w_in_b", [128, 8, 3080], BF16)
        w_out_b = sb("w_out_b", [128, 8, D], BF16)
        wsT = sb("wsT", [128, 4, 128], BF16)
        bsT = sb("bsT", [128, 4])
        sg_bc = sb("sg_bc", [128, 512])
        sb_bc = sb("sb_bc", [128, 512])
        cw = sb("cw", [128, 12, 4])
        negA = sb("negA", [128, 4])
        dtb = sb("dtb", [128, 4])
        gnw = sb("gnw", [128, 128])
        lng = sb("lng", [128, D])
        lnb = sb("lnb", [128, D])
        tri = sb("tri", [128, 128])
        maskL = sb("maskL", [128, 128])
        ones_f = sb("ones_f", [128, 128])
        S.op("pool", lambda e: e.memset(ones_f.t[:], 1.0), writes=[ones_f.b])
        for (dst, src) in ((bsT, bsT_d), (cw, cw_d), (tri, tri_d), (maskL, maskL_d)):
            S.dma(dst.t[:], src, writes=[dst.b])
        for (dst, src) in ((sg_bc, sg_d), (sb_bc, sb_d), (negA, alog_d), (dtb, dtb_d), (gnw, gnw_d), (lng, ln_g), (lnb, ln_b)):
            S.dma(dst.t[:], src.partition_broadcast(128), writes=[dst.b])
        S.op("act", lambda e: e.activation(out=negA.t[:], in_=negA.t[:], func=AF.Exp), reads=[negA.b], writes=[negA.b])
        S.op("dve", lambda e: e.tensor_scalar(out=negA.t[:], in0=negA.t[:], scalar1=-1.0, scalar2=None, op0=ALU.mult),
             reads=[negA.b], writes=[negA.b])
        stA = contextlib.ExitStack()
        stg = [T(stA.enter_context(nc.sbuf_tensor("sbe%d_stg%d" % (l, j), [128, 3080], F32))) for j in range(2)]
        for c in range(8):
            s_ = stg[c % 2]
            S.dma(s_.t[:], w_in[c * 128:(c + 1) * 128, :], writes=[s_.b])
            S.op("act" if c % 2 else "pool", lambda e: (e.copy if c % 2 else e.tensor_copy)(out=w_in_b.t[:, c, :], in_=s_.t[:]),
                 reads=[s_.b], writes=[w_in_b.b])
        for c2 in range(4):
            s_ = stg[c2 % 2]
            S.dma(s_.t[:, 0:2048].rearrange("p (c n) -> p c n", c=2), w_out[c2 * 256:(c2 + 1) * 256, :].rearrange("(c p) n -> p c n", p=128),
                  writes=[s_.b])
            S.op("act", lambda e: e.copy(out=w_out_b.t[:, 2 * c2:2 * c2 + 2, :], in_=s_.t[:, 0:2048].rearrange("p (c n) -> p c n", c=2)),
                 reads=[s_.b], writes=[w_out_b.b])
        s_ = stg[0]
        S.dma(s_.t[:, 0:512].rearrange("p (g i) -> p g i", g=4), sgT, writes=[s_.b])
        S.op("dve", lambda e: e.tensor_copy(out=wsT.t[:], in_=s_.t[:, 0:512].rearrange("p (g i) -> p g i", g=4)), reads=[s_.b], writes=[wsT.b])
        S.op("dve", lambda e: e.memset(wsT.t[64:128, :, 0:64], 0.0), writes=[wsT.b])
        S.barrier()
        stA.close()

        xTc = [sb("xTc%d" % j, [128, 8, 512], BF16) for j in range(2)]
        raw = sb("raw", [128, 12, 515], nb=12)
        qkv = sb("qkv", [128, 12, 512], nb=12)
        S.op("pool", lambda e: e.memset(raw.t[:, :, 0:3], 0.0), writes=raw.bs)
        cvt = sb("cvt", [128, 512])
        sqt = sb("sqt", [128, 512])
        rst = sb("rst", [128, 512])
        u_sb = sb("u_sb", [128, 512])
        vg = sb("vg", [128, 512])
        vnb = sb("vnb", [128, 512], BF16)
        sz = sb("sz", [128, 512])
        mixt = sb("mixt", [128, D], nb=8)
        work = sb("work", [128, D])
        small = sb("small", [128, 64])
        sm_b = [Buf() for _ in range(8)]
        sm2 = sb("sm2", [128, 96])
        s2 = [Buf() for _ in range(24)]
        St = [sb("St%d" % h, [128, 128]) for h in range(4)]
        for h in range(4):
            S.op("pool", lambda e: e.memset(St[h].t[:], 0.0), writes=[St[h].b])
        tn_sets = []
        for si_ in range(2):
            tn = {}
            for nm in ("kbg", "kdec", "vb", "gB", "t1", "t2", "Dn", "DT", "egr", "qdT", "M0", "M1", "N0", "N1", "P0", "P1", "intraT", "u", "wT",
                       "vnew", "junk", "yb"):
                tn[nm] = sb("g%d_" % si_ + nm, [128, 128])
            tn_sets.append(tn)
        slots = [(k.ps[2].t[:, 0:128], k.ps[2].bs[0]), (k.ps[2].t[:, 512:640], k.ps[2].bs[1]),
                 (k.ps[3].t[:, 0:128], k.ps[3].bs[0]), (k.ps[3].t[:, 512:640], k.ps[3].bs[1])]
        sl_i = [0]

        def slot():
            r = slots[sl_i[0] % len(slots)]
            sl_i[0] += 1
            return r
        ident = k.ident

        def mm(out_ap, out_b, lhsT, lb, rhs, rb, start=True, stop=True):
            S.op("pe", lambda e: e.matmul(out_ap, lhsT=lhsT, rhs=rhs, start=start, stop=stop), reads=[lb, rb], writes=[out_b])

        estop = (k.dbg or {}).get("estop", 99)
        for ch in range(n_chunks if estop >= 1 else 0):
            xc = xTc[ch % 2]
            tok = slice(ch * 512, (ch + 1) * 512)
            S.dma(xc.t[:], k.xT_d.rearrange("c p t -> p c t")[:, :, tok], reads=k.xT_b[ch * 4:ch * 4 + 4], writes=[xc.b])
            for cc in range(12):
                pp = k.ps[1]
                hb = cc % 2
                for c in range(8):
                    S.op("pe", lambda e: e.matmul(pp.t[:, hb * 512:(hb + 1) * 512], lhsT=w_in_b.t[:, c, 1024 + cc * 128:1024 + (cc + 1) * 128],
                                                  rhs=xc.t[:, c, :], start=(c == 0), stop=(c == 7)),
                         reads=[w_in_b.b, xc.b], writes=[pp.bs[hb]], inc=(c == 7))
                if ch > 0:
                    S.op("dve", lambda e: e.tensor_copy(out=raw.t[:, cc, 0:3], in_=raw.t[:, cc, 512:515]), reads=[raw.bs[cc]], writes=[raw.bs[cc]])
                S.op("act", lambda e: e.copy(out=raw.t[:, cc, 3:515], in_=pp.t[:, hb * 512:(hb + 1) * 512]), reads=[pp.bs[hb]], writes=[raw.bs[cc]])
                S.op("act", lambda e: e.activation(out=cvt.t[:], in_=raw.t[:, cc, 3:515], func=AF.Copy, scale=cw.t[:, cc, 3:4]),
                     reads=[raw.bs[cc], cw.b], writes=[cvt.b])
                for s_ in (1, 2, 3):
                    S.op("dve", lambda e: e.scalar_tensor_tensor(out=cvt.t[:], in0=raw.t[:, cc, 3 - s_:515 - s_], scalar=cw.t[:, cc, 3 - s_:4 - s_],
                                                                 in1=cvt.t[:], op0=ALU.mult, op1=ALU.add),
                         reads=[raw.bs[cc], cw.b, cvt.b], writes=[cvt.b])
                S.op("act", lambda e: e.activation(out=qkv.t[:, cc, :], in_=cvt.t[:], func=AF.Silu), reads=[cvt.b], writes=[qkv.bs[cc]])
                if cc < 8:
                    S.op("act", lambda e: e.activation(out=sqt.t[:], in_=qkv.t[:, cc, :], func=AF.Square), reads=[qkv.bs[cc]], writes=[sqt.b])
                    pn = k.ps[0]
                    S.op("pe", lambda e: e.matmul(pn.t[:, hb * 512:(hb + 1) * 512], lhsT=ones_f.t[:], rhs=sqt.t[:], start=True, stop=True),
                         reads=[ones_f.b, sqt.b], writes=[pn.bs[hb]])
                    S.op("dve", lambda e: e.tensor_scalar(out=rst.t[:], in0=pn.t[:, hb * 512:(hb + 1) * 512], scalar1=RMS_EPS, scalar2=None, op0=ALU.add),
                         reads=[pn.bs[hb]], writes=[rst.b])
                    S.op("act", lambda e: e.activation(out=rst.t[:], in_=rst.t[:], func=AF.Sqrt), reads=[rst.b], writes=[rst.b])
                    S.op("dve", lambda e: e.reciprocal(out=rst.t[:], in_=rst.t[:]), reads=[rst.b], writes=[rst.b])
                    sc_ = (128.0 ** -0.5) if cc < 4 else 1.0
                    S.op("dve", lambda e: e.scalar_tensor_tensor(out=qkv.t[:, cc, :], in0=qkv.t[:, cc, :], scalar=sc_, in1=rst.t[:],
                                                                 op0=ALU.mult, op1=ALU.mult),
                         reads=[qkv.bs[cc], rst.b], writes=[qkv.bs[cc]])
            for j4 in range(4 if estop >= 2 else 0):
                t = ch * 4 + j4
                tl_ = slice(j4 * 128, (j4 + 1) * 128)
                pa = k.ps[0]
                for hb in range(2):
                    for c in range(8):
                        S.op("pe", lambda e: e.matmul(pa.t[:, hb * 512:(hb + 1) * 512], lhsT=xc.t[:, c, tl_], rhs=w_in_b.t[:, c, hb * 512:(hb + 1) * 512],
                                                      start=(c == 0), stop=(c == 7)),
                             reads=[xc.b, w_in_b.b], writes=[pa.bs[hb]], inc=(c == 7))
                S.op("act", lambda e: e.activation(out=u_sb.t[:], in_=pa.t[:, 0:512], func=AF.Gelu), reads=[pa.bs[0]], writes=[u_sb.b])
                S.op("act", lambda e: e.activation(out=vg.t[:], in_=pa.t[:, 512:1024], func=AF.Gelu), reads=[pa.bs[1]], writes=[vg.b])
                for g in range(4):
                    S.op("dve", lambda e: e.bn_stats(out=sm2.t[:, g * 6:(g + 1) * 6], in_=vg.t[:, g * 128:(g + 1) * 128]), reads=[vg.b], writes=[s2[g]])
                    S.op("dve", lambda e: e.bn_aggr(out=sm2.t[:, 24 + 2 * g:26 + 2 * g], in_=sm2.t[:, g * 6:(g + 1) * 6]), reads=[s2[g]], writes=[s2[4 + g]])
                    S.op("dve", lambda e: e.tensor_scalar(out=sm2.t[:, 32 + g:33 + g], in0=sm2.t[:, 25 + 2 * g:26 + 2 * g], scalar1=LN_EPS, scalar2=None,
                                                          op0=ALU.add), reads=[s2[4 + g]], writes=[s2[8]])
                S.op("act", lambda e: e.activation(out=sm2.t[:, 36:40], in_=sm2.t[:, 32:36], func=AF.Sqrt), reads=[s2[8]], writes=[s2[9]])
                S.op("dve", lambda e: e.reciprocal(out=sm2.t[:, 40:44], in_=sm2.t[:, 36:40]), reads=[s2[9]], writes=[s2[9]])
                for g in range(4):
                    S.op("dve", lambda e: e.tensor_scalar(out=vg.t[:, g * 128:(g + 1) * 128], in0=vg.t[:, g * 128:(g + 1) * 128],
                                                          scalar1=sm2.t[:, 24 + 2 * g:25 + 2 * g], scalar2=sm2.t[:, 40 + g:41 + g],
                                                          op0=ALU.subtract, op1=ALU.mult),
                         reads=[vg.b, s2[4 + g], s2[9]], writes=[vg.b])
                S.op("pool", lambda e: e.tensor_tensor(out=vg.t[:], in0=vg.t[:], in1=sg_bc.t[:], op=ALU.mult), reads=[vg.b, sg_bc.b], writes=[vg.b])
                S.op("pool", lambda e: e.tensor_tensor(out=vnb.t[:], in0=vg.t[:], in1=sb_bc.t[:], op=ALU.add), reads=[vg.b, sb_bc.b], writes=[vnb.b])
                pm = k.ps[1]
                for g in range(4):
                    S.op("pe", lambda e: e.matmul(pm.t[:, g * 128:(g + 1) * 128], lhsT=wsT.t[:, g, :], rhs=vnb.t[:, g * 128:(g + 1) * 128],
                                                  start=True, stop=True),
                         reads=[wsT.b, vnb.b], writes=[pm.bs[0]], inc=(g == 3))
                mb = mixt.bs[0]
                for g in range(4):
                    S.op("dve", lambda e: e.scalar_tensor_tensor(out=mixt.t[:, g * 128:(g + 1) * 128], in0=pm.t[:, g * 128:(g + 1) * 128],
                                                                 scalar=bsT.t[:, g:g + 1], in1=u_sb.t[:, g * 128:(g + 1) * 128],
                                                                 op0=ALU.add, op1=ALU.mult),
                         reads=[pm.bs[0], bsT.b, u_sb.b], writes=[mb])
                if estop < 3:
                    continue
                for c in range(8):
                    S.op("pe", lambda e: e.matmul(pm.t[:, 512:1024], lhsT=xc.t[:, c, tl_], rhs=w_in_b.t[:, c, 2560:3072], start=(c == 0), stop=(c == 7)),
                         reads=[xc.b, w_in_b.b], writes=[pm.bs[1]], inc=(c == 7))
                S.op("act", lambda e: e.activation(out=sz.t[:], in_=pm.t[:, 512:1024], func=AF.Silu), reads=[pm.bs[1]], writes=[sz.b])
                pab, pab_b = slot()
                for c in range(8):
                    S.op("pe", lambda e: e.matmul(pab[:, 0:8], lhsT=xc.t[:, c, tl_], rhs=w_in_b.t[:, c, 3072:3080], start=(c == 0), stop=(c == 7)),
                         reads=[xc.b, w_in_b.b], writes=[pab_b], inc=(c == 7))
                S.op("act", lambda e: e.activation(out=sm2.t[:, 48:52], in_=pab[:, 4:8], func=AF.Sigmoid), reads=[pab_b], writes=[s2[10]])
                S.op("dve", lambda e: e.tensor_scalar(out=sm2.t[:, 80:84], in0=sm2.t[:, 48:52], scalar1=-1.0, scalar2=None, op0=ALU.mult),
                     reads=[s2[10]], writes=[s2[18]])
                S.op("dve", lambda e: e.tensor_tensor(out=sm2.t[:, 76:80], in0=pab[:, 0:4], in1=dtb.t[:], op=ALU.add), reads=[pab_b, dtb.b], writes=[s2[11]])
                S.op("act", lambda e: e.activation(out=sm2.t[:, 76:80], in_=sm2.t[:, 76:80], func=AF.Exp), reads=[s2[11]], writes=[s2[11]])
                S.op("dve", lambda e: e.tensor_scalar(out=sm2.t[:, 76:80], in0=sm2.t[:, 76:80], scalar1=1.0, scalar2=None, op0=ALU.add),
                     reads=[s2[11]], writes=[s2[11]])
                S.op("act", lambda e: e.activation(out=sm2.t[:, 76:80], in_=sm2.t[:, 76:80], func=AF.Ln), reads=[s2[11]], writes=[s2[11]])
                S.op("dve", lambda e: e.tensor_tensor(out=sm2.t[:, 52:56], in0=sm2.t[:, 76:80], in1=negA.t[:], op=ALU.mult), reads=[s2[11], negA.b], writes=[s2[12]])
                pg, pg_b = slot()
                mm(pg[:, 0:4], pg_b, tri.t[:], tri.b, sm2.t[:, 52:56], s2[12])
                mm(pg[:, 4:8], pg_b, ones_f.t[:], ones_f.b, sm2.t[:, 52:56], s2[12])
                S.op("dve", lambda e: e.tensor_copy(out=sm2.t[:, 56:64], in_=pg[:, 0:8]), reads=[pg_b], writes=[s2[13]])
                S.op("act", lambda e: e.activation(out=sm2.t[:, 64:68], in_=sm2.t[:, 60:64], func=AF.Exp), reads=[s2[13]], writes=[s2[14]])
                S.op("act", lambda e: e.activation(out=sm2.t[:, 68:72], in_=sm2.t[:, 56:60], func=AF.Exp), reads=[s2[13]], writes=[s2[15]])
                S.op("dve", lambda e: e.tensor_tensor(out=sm2.t[:, 68:72], in0=sm2.t[:, 68:72], in1=sm2.t[:, 48:52], op=ALU.mult), reads=[s2[15], s2[10]], writes=[s2[15]])
                S.op("dve", lambda e: e.tensor_tensor(out=sm2.t[:, 72:76], in0=sm2.t[:, 60:64], in1=sm2.t[:, 56:60], op=ALU.subtract), reads=[s2[13]], writes=[s2[16]])
                S.op("act", lambda e: e.activation(out=sm2.t[:, 72:76], in_=sm2.t[:, 72:76], func=AF.Exp), reads=[s2[16]], writes=[s2[16]])

                if estop < 4:
                    continue
                def head_gen(h, tn):
                    qT = qkv.t[:, h, tl_]
                    kT = qkv.t[:, 4 + h, tl_]
                    vT = qkv.t[:, 8 + h, tl_]
                    qb, kb_, vb_ = qkv.bs[h], qkv.bs[4 + h], qkv.bs[8 + h]
                    col = lambda a: sm2.t[:, a + h:a + h + 1]
                    yield
                    pk, pk_b = slot()
                    mm(pk, pk_b, kT, kb_, ident.t[:], ident.b)
                    yield
                    pv, pv_b = slot()
                    mm(pv, pv_b, vT, vb_, ident.t[:], ident.b)
                    if estop <= 4.05:
                        return
                    S.op("dve", lambda e: e.tensor_scalar(out=tn["kbg"].t[:], in0=pk, scalar1=col(68), scalar2=None, op0=ALU.mult),
                         reads=[pk_b, s2[15]], writes=[tn["kbg"].b])
                    S.op("act", lambda e: e.activation(out=tn["kdec"].t[:], in_=pk, func=AF.Copy, scale=col(72)),
                         reads=[pk_b, s2[16]], writes=[tn["kdec"].b])
                    S.op("dve", lambda e: e.tensor_scalar(out=tn["vb"].t[:], in0=pv, scalar1=col(48), scalar2=None, op0=ALU.mult),
                         reads=[pv_b, s2[10]], writes=[tn["vb"].b])
                    if estop <= 4.1:
                        return
                    S.op("pool", lambda e: e.tensor_scalar(out=tn["gB"].t[:], in0=ones_f.t[:], scalar1=col(52), scalar2=None, op0=ALU.mult),
                         reads=[ones_f.b, s2[12]], writes=[tn["gB"].b])
                    yield
                    pr, pr_b = slot()
                    mm(pr, pr_b, tn["gB"].t[:], tn["gB"].b, tri.t[:], tri.b)
                    S.op("dve", lambda e: e.tensor_scalar(out=tn["t1"].t[:], in0=pr, scalar1=col(56), scalar2=0.0, op0=ALU.subtract, op1=ALU.max),
                         reads=[pr_b, s2[13]], writes=[tn["t1"].b])
                    S.op("act", lambda e: e.activation(out=tn["t1"].t[:], in_=tn["t1"].t[:], func=AF.Exp, scale=-1.0), reads=[tn["t1"].b], writes=[tn["t1"].b])
                    S.op("pool", lambda e: e.tensor_tensor(out=tn["Dn"].t[:], in0=tn["t1"].t[:], in1=maskL.t[:], op=ALU.mult),
                         reads=[tn["t1"].b, maskL.b], writes=[tn["Dn"].b])
                    S.op("dve", lambda e: e.tensor_scalar(out=tn["t2"].t[:], in0=pr, scalar1=col(56), scalar2=0.0, op0=ALU.subtract, op1=ALU.min),
                         reads=[pr_b, s2[13]], writes=[tn["t2"].b])
                    S.op("act", lambda e: e.activation(out=tn["t2"].t[:], in_=tn["t2"].t[:], func=AF.Exp), reads=[tn["t2"].b], writes=[tn["t2"].b])
                    S.op("pool", lambda e: e.tensor_tensor(out=tn["DT"].t[:], in0=tn["t2"].t[:], in1=tri.t[:], op=ALU.mult),
                         reads=[tn["t2"].b, tri.b], writes=[tn["DT"].b])
                    S.op("act", lambda e: e.activation(out=tn["egr"].t[:], in_=pr, func=AF.Exp), reads=[pr_b], writes=[tn["egr"].b])
                    S.op("pool", lambda e: e.tensor_tensor(out=tn["qdT"].t[:], in0=qT, in1=tn["egr"].t[:], op=ALU.mult),
                         reads=[qb, tn["egr"].b], writes=[tn["qdT"].b])
                    if estop <= 4.2:
                        return
                    yield
                    pkk, pkk_b = slot()
                    mm(pkk, pkk_b, kT, kb_, kT, kb_)
                    S.op("dve", lambda e: e.scalar_tensor_tensor(out=tn["M0"].t[:], in0=pkk, scalar=col(80), in1=tn["Dn"].t[:], op0=ALU.mult, op1=ALU.mult),
                         reads=[pkk_b, s2[18], tn["Dn"].b], writes=[tn["M0"].b])
                    yield
                    pn_, pn_b = slot()
                    mm(pn_, pn_b, tn["M0"].t[:], tn["M0"].b, ident.t[:], ident.b)
                    S.op("act", lambda e: e.copy(out=tn["N0"].t[:], in_=pn_), reads=[pn_b], writes=[tn["N0"].b])
                    S.op("dve", lambda e: e.tensor_tensor(out=tn["P0"].t[:], in0=pn_, in1=ident.t[:], op=ALU.add), reads=[pn_b, ident.b], writes=[tn["P0"].b])
                    yield
                    pqk, pqk_b = slot()
                    mm(pqk, pqk_b, kT, kb_, qT, qb)
                    S.op("dve", lambda e: e.tensor_tensor(out=tn["intraT"].t[:], in0=pqk, in1=tn["DT"].t[:], op=ALU.mult),
                         reads=[pqk_b, tn["DT"].b], writes=[tn["intraT"].b])
                    if estop <= 4.3:
                        return
                    cm, cn_, cp = "M0", "N0", "P0"
                    for s_ in range(6):
                        nm_, nn_, np_ = ("M1", "N1", "P1") if cm == "M0" else ("M0", "N0", "P0")
                        yield
                        p1, p1_b = slot()
                        mm(p1, p1_b, tn[cn_].t[:], tn[cn_].b, tn[cm].t[:], tn[cm].b)
                        S.op("act", lambda e: e.copy(out=tn[nm_].t[:], in_=p1), reads=[p1_b], writes=[tn[nm_].b])
                        if s_ < 5:
                            yield
                            p2, p2_b = slot()
                            mm(p2, p2_b, tn[cm].t[:], tn[cm].b, tn[cn_].t[:], tn[cn_].b)
                            S.op("dve", lambda e: e.tensor_copy(out=tn[nn_].t[:], in_=p2), reads=[p2_b], writes=[tn[nn_].b])
                        yield
                        p3, p3_b = slot()
                        mm(p3, p3_b, tn[nm_].t[:], tn[nm_].b, tn[cp].t[:], tn[cp].b)
                        S.op("dve", lambda e: e.tensor_tensor(out=tn[np_].t[:], in0=p3, in1=tn[cp].t[:], op=ALU.add),
                             reads=[p3_b, tn[cp].b], writes=[tn[np_].b])
                        cm, cn_, cp = nm_, nn_, np_
                    if estop <= 4.4:
                        return
                    TT = tn[cp]
                    yield
                    pu_, pu_b = slot()
                    mm(pu_, pu_b, TT.t[:], TT.b, tn["vb"].t[:], tn["vb"].b)
                    S.op("act", lambda e: e.copy(out=tn["u"].t[:], in_=pu_), reads=[pu_b], writes=[tn["u"].b])
                    yield
                    pw, pw_b = slot()
                    mm(pw, pw_b, tn["kbg"].t[:], tn["kbg"].b, TT.t[:], TT.b)
                    S.op("act", lambda e: e.copy(out=tn["wT"].t[:], in_=pw), reads=[pw_b], writes=[tn["wT"].b])
                    if estop <= 4.5:
                        return
                    Sh = St[h]
                    yield
                    pvn, pvn_b = slot()
                    mm(pvn, pvn_b, tn["wT"].t[:], tn["wT"].b, Sh.t[:], Sh.b)
                    S.op("dve", lambda e: e.tensor_tensor(out=tn["vnew"].t[:], in0=tn["u"].t[:], in1=pvn, op=ALU.subtract),
                         reads=[tn["u"].b, pvn_b], writes=[tn["vnew"].b])
                    yield
                    po_, po_b = slot()
                    mm(po_, po_b, tn["qdT"].t[:], tn["qdT"].b, Sh.t[:], Sh.b, start=True, stop=False)
                    mm(po_, po_b, tn["intraT"].t[:], tn["intraT"].b, tn["vnew"].t[:], tn["vnew"].b, start=False, stop=True)
                    yield
                    pS, pS_b = slot()
                    mm(pS, pS_b, tn["kdec"].t[:], tn["kdec"].b, tn["vnew"].t[:], tn["vnew"].b)
                    S.op("dve", lambda e: e.scalar_tensor_tensor(out=Sh.t[:], in0=Sh.t[:], scalar=col(64), in1=pS, op0=ALU.mult, op1=ALU.add),
                         reads=[Sh.b, s2[14], pS_b], writes=[Sh.b])
                    if estop <= 4.6:
                        return
                    S.op("act", lambda e: e.activation(out=tn["junk"].t[:], in_=po_, func=AF.Square, accum_out=sm2.t[:, 84 + h:85 + h]),
                         reads=[po_b], writes=[tn["junk"].b, s2[19]])
                    S.op("dve", lambda e: e.tensor_scalar(out=sm2.t[:, 88 + h:89 + h], in0=sm2.t[:, 84 + h:85 + h], scalar1=1.0 / 128, scalar2=RMS_EPS,
                                                          op0=ALU.mult, op1=ALU.add), reads=[s2[19]], writes=[s2[20]])
                    S.op("act", lambda e: e.activation(out=sm2.t[:, 88 + h:89 + h], in_=sm2.t[:, 88 + h:89 + h], func=AF.Sqrt), reads=[s2[20]], writes=[s2[20]])
                    S.op("dve", lambda e: e.reciprocal(out=sm2.t[:, 92 + h:93 + h], in_=sm2.t[:, 88 + h:89 + h]), reads=[s2[20]], writes=[s2[21]])
                    S.op("dve", lambda e: e.scalar_tensor_tensor(out=tn["yb"].t[:], in0=po_, scalar=sm2.t[:, 92 + h:93 + h], in1=gnw.t[:],
                                                                 op0=ALU.mult, op1=ALU.mult),
                         reads=[po_b, s2[21], gnw.b], writes=[tn["yb"].b])
                    S.op("pool", lambda e: e.tensor_tensor(out=mixt.t[:, 512 + h * 128:512 + (h + 1) * 128], in0=tn["yb"].t[:],
                                                           in1=sz.t[:, h * 128:(h + 1) * 128], op=ALU.mult),
                         reads=[tn["yb"].b, sz.b], writes=[mb])
                gens = [head_gen(h, tn_sets[h % 2]) for h in range(4)]
                for grp in ((0, 1), (2, 3)):
                    live = [gens[g] for g in grp]
                    while live:
                        for g in list(live):
                            try:
                                next(g)
                            except StopIteration:
                                live.remove(g)
                if estop < 5:
                    continue
                pT = k.ps[3]
                xTt = k.xTt[k.rr]
                k.rr ^= 1
                for c in range(8):
                    S.op("pe", lambda e: e.transpose(out=pT.t[:, c * 128:(c + 1) * 128], in_=mixt.t[:, c * 128:(c + 1) * 128], identity=ident.t[:]),
                         reads=[mb, ident.b], writes=[pT.bs[c // 4]], inc=(c % 4 == 3))
                S.op("act", lambda e: e.copy(out=xTt.t[:].rearrange("p c t -> p (c t)"), in_=pT.t[:]), reads=pT.bs, writes=[xTt.b])
                po2 = k.ps[0]
                for hb in range(2):
                    for c in range(8):
                        S.op("pe", lambda e: e.matmul(po2.t[:, hb * 512:(hb + 1) * 512], lhsT=xTt.t[:, c, :], rhs=w_out_b.t[:, c, hb * 512:(hb + 1) * 512],
                                                      start=(c == 0), stop=(c == 7)),
                             reads=[xTt.b, w_out_b.b], writes=[po2.bs[hb]], inc=(c == 7))
                epilogue_defer.append((t, po2))
                epilogue_now(k, t, work, lng, lnb, small, sm_b, po2)
        S.barrier()
    k.xcur = k.xres
    k.xcur_is_in = False


epilogue_defer = []


def epilogue_now(k, t, work, lng, lnb, small, sm_b, po2):
    epilogue(k, t, work, lng, lnb, small, sm_b, False, src=po2.t[:], src_bufs=po2.bs)


CONSTS = None
LAST_INPUTS = set()


def layer_inputs(inputs, b):
    m = {"x": np.ascontiguousarray(inputs["x"][b]), "ident": np.eye(128, dtype=np.float32)}
    inv_freq = (np.float32(10000.0) ** (-np.arange(0, 64, 2, dtype=np.float32) / np.float32(64))).astype(np.float32)
    ang = (np.arange(SEQ, dtype=np.float32)[:, None] * inv_freq[None, :]).astype(np.float32)
    cos2T = np.zeros((128, SEQ), np.float32)
    sin2T = np.zeros((128, SEQ), np.float32)
    cos2T[0:32] = np.cos(ang).T
    cos2T[32:64] = np.cos(ang).T
    sin2T[0:32] = np.sin(ang).T
    sin2T[32:64] = np.sin(ang).T
    m["cos2T"] = cos2T
    m["sin2T"] = sin2T
    m["tri"] = np.triu(np.ones((128, 128), np.float32))
    m["maskL"] = np.tril(np.ones((128, 128), np.float32), -1)
    for i in range(2):
        m["even_w_in_%d" % i] = inputs["even_w_in"][i]
        m["even_w_out_%d" % i] = inputs["even_w_out"][i]
        m["even_sgu_wT_%d" % i] = np.ascontiguousarray(np.transpose(inputs["even_sgu_w"][i], (2, 0, 1)))
        m["even_sgu_bT_%d" % i] = np.ascontiguousarray(inputs["even_sgu_b"][i].T)
        m["even_conv_wT_%d" % i] = np.ascontiguousarray(inputs["even_conv_w"][i].T.reshape(12, 128, 4).transpose(1, 0, 2))
        for nm in ("even_sgu_ln_g", "even_sgu_ln_b", "even_a_log", "even_dt_bias", "even_gdn_norm"):
            m["%s_%d" % (nm, i)] = inputs[nm][i][None, :]
    for i in range(2):
        for nm in ("odd_w_in", "odd_w_uq", "odd_w_ukv", "odd_w_out"):
            m["%s_%d" % (nm, i)] = inputs[nm][i]
        for nm in ("odd_q_norm", "odd_kv_norm"):
            m["%s_%d" % (nm, i)] = inputs[nm][i][None, :]
    for l in range(DEPTH):
        for nm in ("moe_w_router", "moe_w_gate_up", "moe_b_gate_up", "moe_w_down", "moe_b_down"):
            m["%s_%d" % (nm, l)] = inputs[nm][l]
        for nm in ("moe_b_router", "ln_ffn_g", "ln_ffn_b", "ln_mix_g", "ln_mix_b"):
            m["%s_%d" % (nm, l)] = inputs[nm][l][None, :]
    return m


def make_consts():
    return {"ident": np.eye(128, dtype=np.float32)}


def kernel(**inputs):
    phases = [("prep",)]
    for l in range(DEPTH):
        phases.append(("even", l) if l % 2 == 0 else ("odd", l))
        phases.append(("moe", l, l == DEPTH - 1))
    nc = build(phases)
    names = set(LAST_INPUTS)
    inputs = {k_: np.asarray(v) for k_, v in inputs.items()}
    n_cores = inputs["x"].shape[0]
    in_maps = []
    for b_ in range(n_cores):
        m = layer_inputs(inputs, b_)
        in_maps.append({k_: np.ascontiguousarray(v, dtype=np.float32) for k_, v in m.items() if k_ in names})
    res = run_bass_kernel_spmd(nc, in_maps, core_ids=list(range(n_cores)))
    return np.stack([np.asarray(r["y"], dtype=np.float32) for r in res.results], axis=0)
```

```python
import contextlib
import numpy as np
import concourse.bass as bass
import concourse.mybir as mybir
from concourse.bass_utils import run_bass_kernel_spmd

F32 = mybir.dt.float32
BF16 = mybir.dt.bfloat16
AF = mybir.ActivationFunctionType
ALU = mybir.AluOpType
AX = mybir.AxisListType

D = 1024
SEQ = 4096
NT = SEQ // 128
DEPTH = 4
NE = 32
ALPHA = (2 * DEPTH) ** 0.25
LN_EPS = 1e-5
TC = 1024
NCH = SEQ // TC
TPC = TC // 128
N_WD = 1
SILU_CAP = 11.913920224372246


class Buf:
    __slots__ = ("w", "r", "excl")

    def __init__(self, excl=False):
        self.w = None
        self.r = {}
        self.excl = excl


class Sched:
    N_DMA_SEMS = 16

    def __init__(self, nc, stack):
        self.nc = nc
        self.eng = {"pe": nc.tensor, "dve": nc.vector, "act": nc.scalar,
                    "pool": nc.gpsimd, "sp": nc.sync}
        self.sems = {}
        self.cnt = {}
        for e in self.eng:
            self.sems[e] = stack.enter_context(nc.semaphore("s_" + e))
            self.cnt[e] = 0
        for i in range(self.N_DMA_SEMS):
            k = ("d", i)
            self.sems[k] = stack.enter_context(nc.semaphore("s_d%d" % i))
            self.cnt[k] = 0
        self.seen = {e: {} for e in self.eng}
        self.dma_rr = 0
        self.n_ins = 0

    def _waits(self, e, reads, writes, extra=()):
        waits = {}

        def add(st):
            if st is None:
                return
            k, v = st
            if e == "pe" and k == "pe":
                return
            assert not (k == e and v > self.cnt[e]), "self-deadlock"
            if v > waits.get(k, 0):
                waits[k] = v
        for b in reads:
            add(b.w)
            if b.excl:
                for k, v in b.r.items():
                    if k != e:
                        add((k, v))
        for b in writes:
            add(b.w)
            for k, v in b.r.items():
                add((k, v))
        for st in extra:
            add(st)
        eng = self.eng[e]
        seen = self.seen[e]
        for k, v in waits.items():
            if seen.get(k, 0) >= v:
                continue
            seen[k] = v
            eng.wait_ge(self.sems[k], v)

    def _stamp(self, stamp, reads, writes):
        k, v = stamp
        for b in reads:
            if b.r.get(k, 0) < v:
                b.r[k] = v
        for b in writes:
            b.w = stamp
            b.r = {}

    def op(self, e, fn, reads=(), writes=(), inc=True):
        self._waits(e, reads, writes)
        ins = fn(self.eng[e])
        self.n_ins += 1
        if inc:
            self.cnt[e] += 1
            ins.then_inc(self.sems[e], 1)
            stamp = (e, self.cnt[e])
        else:
            stamp = (e, self.cnt[e] + 1)
        self._stamp(stamp, reads, writes)
        return ins

    def dma(self, out, in_, reads=(), writes=(), q="sp", **kw):
        i = self.dma_rr
        self.dma_rr = (self.dma_rr + 1) % self.N_DMA_SEMS
        k = ("d", i)
        extra = [(k, self.cnt[k])] if self.cnt[k] else []
        self._waits(q, reads, writes, extra)
        ins = self.eng[q].dma_start(out=out, in_=in_, **kw)
        self.n_ins += 1
        self.cnt[k] += 16
        ins.then_inc(self.sems[k], 16)
        self._stamp((k, self.cnt[k]), reads, writes)
        return ins

    def finish(self, bufs):
        self._waits("sp", bufs, ())

    def barrier(self):
        stamps = [(k, v) for k, v in self.cnt.items() if v > 0]
        for e in self.eng:
            self._waits(e, (), (), stamps)


class T:
    def __init__(self, t, nb=1, excl=False):
        self.t = t
        self.b = Buf(excl)
        self.bs = [Buf(excl) for _ in range(nb)]


class K:
    pass


def build(phases, n_layers=DEPTH, dbg=None, nc=None, ext_ins=None, ext_out=None):
    if nc is None:
        nc = bass.Bass("TRN2", target_bir_lowering=False)
    k = K()
    k.nc = nc
    k.dbg = dbg

    def din(name, shape, dt=F32):
        if ext_ins is not None:
            return ext_ins[name]
        return nc.dram_tensor(name, list(shape), dt, kind="ExternalInput").ap()

    I = {}

    def inp(name, shape, dt=F32):
        if name not in I:
            I[name] = din(name, shape, dt)
        return I[name]
    k.inp = inp
    inp("x", [SEQ, D])
    inp("ident", [128, 128])
    k.I = I
    k.y = ext_out if ext_out is not None else nc.dram_tensor("y", [SEQ, D], F32, kind="ExternalOutput").ap()
    k.xres = nc.dram_tensor("xres", [SEQ, D], F32, kind="Internal").ap()
    k.xT_d = nc.dram_tensor("xT_d", [8, 128, SEQ], BF16, kind="Internal").ap()
    k.xres_b = [Buf() for _ in range(NT)]
    k.xT_b = [Buf() for _ in range(NT)]
    k.y_b = [Buf() for _ in range(NT)]
    k.xcur = I["x"]
    k.xcur_is_in = True

    with contextlib.ExitStack() as st:
        S = Sched(nc, st)
        k.S = S
        k.st = st

        def sb(name, shape, dt=F32, nb=1):
            return T(st.enter_context(nc.sbuf_tensor("sb_" + name, list(shape), dt)), nb)
        k.sb = sb
        k.ps = [T(st.enter_context(nc.psum_tensor("ps%d" % i, [128, 1024], F32)), 2, excl=True)
                for i in range(4)]
        k.ident = sb("ident", [128, 128])
        S.dma(k.ident.t[:], I["ident"], writes=[k.ident.b])
        k.ones = sb("ones", [1, 128])
        S.op("dve", lambda e: e.memset(k.ones.t[:], 1.0), writes=[k.ones.b])
        k.xt = [sb("xt%d" % i, [128, D]) for i in range(2)]
        k.xTt = [sb("xTt%d" % i, [128, 8, 128], BF16) for i in range(2)]
        k.rr = 0

        for ph in phases:
            if ph[0] == "prep":
                prep_phase(k)
            elif ph[0] == "moe":
                moe_phase(k, ph[1], final=ph[2])
            elif ph[0] == "odd":
                odd_phase(k, ph[1])
            elif ph[0] == "even":
                even_phase(k, ph[1])
            elif ph[0] == "dump":
                for t in range(NT):
                    xt = k.xt[t % 2]
                    S.dma(xt.t[:], k.xcur[t * 128:(t + 1) * 128, :], reads=[k.xres_b[t]], writes=[xt.b])
                    S.dma(k.y[t * 128:(t + 1) * 128, :], xt.t[:], reads=[xt.b], writes=[k.y_b[t]])
        S.finish(k.y_b)
        S.barrier()
        global LAST_INPUTS
        LAST_INPUTS = set(I.keys())
        print("instructions:", S.n_ins, "counts:", {kk: v for kk, v in S.cnt.items() if not isinstance(kk, tuple)})
    return nc


def emit_xT(k, t, src, src_b):
    S = k.S
    j = k.rr
    k.rr ^= 1
    ps = k.ps[3]
    for c in range(8):
        S.op("pe", lambda e, c=c: e.transpose(out=ps.t[:, c * 128:(c + 1) * 128],
                                              in_=src[:, c * 128:(c + 1) * 128],
                                              identity=k.ident.t[:]),
             reads=[src_b, k.ident.b], writes=[ps.bs[c // 4]], inc=(c % 4 == 3))
    xTt = k.xTt[j]
    S.op("act", lambda e: e.copy(out=xTt.t[:].rearrange("p c t -> p (c t)"), in_=ps.t[:]),
         reads=ps.bs, writes=[xTt.b])
    S.dma(k.xT_d.rearrange("c p t -> p c t")[:, :, t * 128:(t + 1) * 128], xTt.t[:],
          reads=[xTt.b], writes=[k.xT_b[t]])


def prep_phase(k):
    S = k.S
    for t in range(NT):
        xt = k.xt[t % 2]
        S.dma(xt.t[:], k.xcur[t * 128:(t + 1) * 128, :], writes=[xt.b])
        emit_xT(k, t, xt.t, xt.b)


def moe_phase(k, l, final=False):
    S, nc, sb = k.S, k.nc, k.sb
    I = {}
    I["moe_w_router"] = k.inp("moe_w_router_%d" % l, [D, NE])
    I["moe_b_router"] = k.inp("moe_b_router_%d" % l, [1, NE])
    I["moe_w_gate_up"] = k.inp("moe_w_gate_up_%d" % l, [NE, D, 2 * D])
    I["moe_b_gate_up"] = k.inp("moe_b_gate_up_%d" % l, [NE, 2 * D])
    I["moe_w_down"] = k.inp("moe_w_down_%d" % l, [NE, D, D])
    I["moe_b_down"] = k.inp("moe_b_down_%d" % l, [NE, D])
    I["ln_ffn_g"] = k.inp("ln_ffn_g_%d" % l, [1, D])
    I["ln_ffn_b"] = k.inp("ln_ffn_b_%d" % l, [1, D])
    n_ch = (k.dbg or {}).get("n_ch", NCH)
    n_e = (k.dbg or {}).get("n_e", NE)
    with contextlib.ExitStack() as st:
        def sb(name, shape, dt=F32, nb=1):
            return T(st.enter_context(nc.sbuf_tensor("sbm%d_" % l + name, list(shape), dt)), nb)
        wr_f = sb("wr_f", [128, 8, NE])
        wr_b = sb("wr_b", [128, 8, NE], BF16)
        S.dma(wr_f.t[:], I["moe_w_router"].rearrange("(c p) e -> p c e", p=128), writes=[wr_f.b])
        S.op("dve", lambda e: e.tensor_copy(out=wr_b.t[:], in_=wr_f.t[:]), reads=[wr_f.b], writes=[wr_b.b])
        br = sb("br", [128, NE])
        S.dma(br.t[:], I["moe_b_router"].partition_broadcast(128), writes=[br.b])
        stg = [sb("stg%d" % i, [128, 2 * D]) for i in range(2)]
        bgu = T(stg[0].t[0:NE, :])
        bgu.b = stg[0].b
        S.dma(bgu.t[:], I["moe_b_gate_up"], writes=[bgu.b])
        bguT = sb("bguT", [128, 16, NE])
        ps = k.ps[3]
        for two in range(2):
            for m in range(8):
                i = two * 8 + m
                S.op("pe", lambda e, two=two, m=m, i=i: e.transpose(
                    out=ps.t[:, i * NE:(i + 1) * NE],
                    in_=bgu.t[:, 2 * m * 128 + two:2 * (m + 1) * 128:2],
                    identity=k.ident.t[0:NE, 0:NE]),
                    reads=[bgu.b, k.ident.b], writes=[ps.bs[0]], inc=(i == 15))
        S.op("dve", lambda e: e.tensor_copy(out=bguT.t[:].rearrange("p a b -> p (a b)"), in_=ps.t[:, 0:16 * NE]),
             reads=[ps.bs[0]], writes=[bguT.b])
        bgu2 = sb("bgu2", [128, 16, NE])
        S.op("dve", lambda e: e.tensor_scalar(out=bgu2.t[:, 0:8, :], in0=bguT.t[:, 0:8, :], scalar1=1.702, scalar2=None, op0=ALU.mult),
             reads=[bguT.b], writes=[bgu2.b])
        S.op("dve", lambda e: e.tensor_scalar(out=bgu2.t[:, 8:16, :], in0=bguT.t[:, 8:16, :], scalar1=1.0, scalar2=None, op0=ALU.add),
             reads=[bguT.b], writes=[bgu2.b])
        bd = sb("bd", [128, D])
        S.op("pool", lambda e: e.memset(bd.t[:], 0.0), writes=[bd.b])
        S.dma(bd.t[0:NE, :], I["moe_b_down"], writes=[bd.b])
        lng = sb("lng", [128, D])
        lnb = sb("lnb", [128, D])
        S.dma(lng.t[:], I["ln_ffn_g"].partition_broadcast(128), writes=[lng.b])
        S.dma(lnb.t[:], I["ln_ffn_b"].partition_broadcast(128), writes=[lnb.b])

        stop = (k.dbg or {}).get("stop", 99)
        if stop <= 1:
            S.barrier()
            return
        xTs = sb("xTs", [128, 8, TC], BF16)
        acc = [sb("acc%d" % i, [128, D]) for i in range(TPC)]
        gate = sb("gate", [128, TPC, NE], nb=TPC)
        wgu = [sb("wgu%d" % i, [128, 8, 2 * D], BF16, nb=8) for i in range(2)]
        wd = [sb("wd%d" % i, [128, 8, D], BF16, nb=4) for i in range(N_WD)]
        actb = [sb("actb%d" % i, [128, 8, 512], BF16, nb=8) for i in range(2)]
        tsl = [sb("tsl%d" % i, [128, 512]) for i in range(2)]
        tlc2 = [sb("tlc2%d" % i, [128, 512]) for i in range(2)]
        tslc = [sb("tslc%d" % i, [128, 512]) for i in range(2)]
        pair_i = [0]
        small = sb("small", [128, 64])
        sm_b = [Buf() for _ in range(8)]
        gT = sb("gT", [128, 128])
        S.op("pool", lambda e: e.memset(gT.t[:], 0.0), writes=[gT.b])
        lg = sb("lg", [128, NE])
        lm = sb("lm", [128, NE])

        wgu_src = I["moe_w_gate_up"]
        wd_src = I["moe_w_down"]
        stg_rr = [0]

        def prep_steps(e, wg, wdd):
            steps = []
            for c in range(8):
                def f(c=c):
                    s_ = stg[stg_rr[0]]
                    stg_rr[0] ^= 1
                    S.dma(s_.t[:], wgu_src[e, c * 128:(c + 1) * 128, :], writes=[s_.b])
                    S.op("dve", lambda en: en.tensor_copy(out=wg.t[:, c, 0:D], in_=s_.t[:, 0:2 * D:2]),
                         reads=[s_.b], writes=[wg.bs[c]])
                    S.op("act", lambda en: en.copy(out=wg.t[:, c, D:2 * D], in_=s_.t[:, 1:2 * D:2]),
                         reads=[s_.b], writes=[wg.bs[c]])
                steps.append(f)
            for c2 in range(4):
                def f(c2=c2):
                    s_ = stg[stg_rr[0]]
                    stg_rr[0] ^= 1
                    S.dma(s_.t[:].rearrange("p (c n) -> p c n", c=2),
                          wd_src[e, c2 * 256:(c2 + 1) * 256, :].rearrange("(c p) n -> p c n", p=128),
                          writes=[s_.b])
                    S.op("act", lambda en: en.mul(out=wdd.t[:, 2 * c2:2 * c2 + 2, :].rearrange("p c n -> p (c n)"),
                                                  in_=s_.t[:], mul=1.0 / 1.702),
                         reads=[s_.b], writes=[wdd.bs[c2]])
                steps.append(f)
            return steps

        if not (k.dbg or {}).get("skip_prep"):
            for f in prep_steps(0, wgu[0], wd[0]):
                f()

        for ch in range(n_ch):
            t0 = ch * TPC
            S.dma(xTs.t[:], k.xT_d.rearrange("c p t -> p c t")[:, :, ch * TC:(ch + 1) * TC],
                  reads=k.xT_b[t0:t0 + TPC], writes=[xTs.b])
            for tt in range(TPC):
                pr = k.ps[3]
                for c in range(8):
                    S.op("pe", lambda e, c=c, tt=tt: e.matmul(pr.t[:, 0:NE], lhsT=xTs.t[:, c, tt * 128:(tt + 1) * 128],
                                                             rhs=wr_b.t[:, c, :], start=(c == 0), stop=(c == 7)),
                         reads=[xTs.b, wr_b.b], writes=[pr.bs[0]], inc=(c == 7))
                S.op("dve", lambda e: e.tensor_tensor(out=lg.t[:], in0=pr.t[:, 0:NE], in1=br.t[:], op=ALU.add),
                     reads=[pr.bs[0], br.b], writes=[lg.b])
                if stop <= 2.1:
                    continue
                S.op("dve", lambda e: e.max(out=small.t[:, 0:8], in_=lg.t[:]), reads=[lg.b], writes=[sm_b[0]])
                if stop <= 2.2:
                    continue
                S.op("dve", lambda e: e.tensor_scalar(out=lm.t[:], in0=lg.t[:], scalar1=small.t[:, 3:4], scalar2=None,
                                                      op0=ALU.is_ge), reads=[lg.b, sm_b[0]], writes=[lm.b])
                S.op("dve", lambda e: e.tensor_scalar(out=small.t[:, 8:9], in0=small.t[:, 0:1], scalar1=-1.0,
                                                      scalar2=None, op0=ALU.mult), reads=[sm_b[0]], writes=[sm_b[1]])
                S.op("act", lambda e: e.activation(out=lg.t[:], in_=lg.t[:], func=AF.Exp, bias=small.t[:, 8:9], scale=1.0),
                     reads=[lg.b, sm_b[1]], writes=[lg.b])
                S.op("dve", lambda e: e.scalar_tensor_tensor(out=lm.t[:], in0=lg.t[:], scalar=1.0, in1=lm.t[:],
                                                             op0=ALU.mult, op1=ALU.mult, accum_out=small.t[:, 9:10]),
                     reads=[lg.b, lm.b], writes=[lm.b, sm_b[2]])
                S.op("dve", lambda e: e.reciprocal(out=small.t[:, 10:11], in_=small.t[:, 9:10]), reads=[sm_b[2]], writes=[sm_b[3]])
                S.op("dve", lambda e, tt=tt: e.tensor_scalar(out=gate.t[:, tt, :], in0=lm.t[:], scalar1=small.t[:, 10:11],
                                                             scalar2=None, op0=ALU.mult),
                     reads=[lm.b, sm_b[3]], writes=[gate.bs[tt]])
                if stop <= 2.3:
                    continue
                pt = k.ps[3]
                S.op("pe", lambda e, tt=tt: e.transpose(out=pt.t[0:NE, 512:640], in_=gate.t[:, tt, :], identity=k.ident.t[:]),
                     reads=[gate.bs[tt], k.ident.b], writes=[pt.bs[1]])
                S.op("dve", lambda e: e.tensor_copy(out=gT.t[0:NE, :], in_=pt.t[0:NE, 512:640]), reads=[pt.bs[1]], writes=[gT.b])
                if stop <= 2.4:
                    continue
                pa = k.ps[2]
                for h in range(2):
                    S.op("pe", lambda e, h=h: e.matmul(pa.t[:, h * 512:(h + 1) * 512], lhsT=gT.t[:], rhs=bd.t[:, h * 512:(h + 1) * 512],
                                                       start=True, stop=True),
                         reads=[gT.b, bd.b], writes=[pa.bs[h]])
                S.op("act", lambda e, tt=tt: e.copy(out=acc[tt].t[:], in_=pa.t[:]), reads=pa.bs, writes=[acc[tt].b])

            if stop < 3:
                continue
            def emit_gu(e_, sub, steps):
                wg = wgu[e_ % 2]
                ab = actb[sub % 2]
                tok = slice(sub * 512, (sub + 1) * 512)
                si = 0
                for m in range(8):
                    pg = k.ps[m % 2]
                    for half in range(2):
                        for c in range(8):
                            S.op("pe", lambda e: e.matmul(
                                pg.t[:, half * 512:(half + 1) * 512],
                                lhsT=wg.t[:, c, half * D + m * 128: half * D + (m + 1) * 128],
                                rhs=xTs.t[:, c, tok], start=(c == 0), stop=(c == 7)),
                                reads=[wg.bs[c], xTs.b], writes=[pg.bs[half]], inc=(c == 7))
                    bg = bgu2.t[:, m, e_:e_ + 1]
                    bl = bgu2.t[:, 8 + m, e_:e_ + 1]
                    pi_ = pair_i[0] % 2
                    pair_i[0] += 1
                    sl_, lc_, slc_ = tsl[pi_], tlc2[pi_], tslc[pi_]
                    S.op("act", lambda e: e.activation(out=sl_.t[:], in_=pg.t[:, 0:512], func=AF.Silu, bias=bg, scale=1.702),
                         reads=[pg.bs[0], bgu2.b], writes=[sl_.b])
                    S.op("dve", lambda e: e.tensor_scalar(out=lc_.t[:], in0=pg.t[:, 512:1024], scalar1=bl, scalar2=8.0,
                                                          op0=ALU.add, op1=ALU.min),
                         reads=[pg.bs[1], bgu2.b], writes=[lc_.b])
                    S.op("pool", lambda e: e.tensor_scalar(out=slc_.t[:], in0=sl_.t[:], scalar1=SILU_CAP, scalar2=-3.0e38,
                                                           op0=ALU.min, op1=ALU.max),
                         reads=[sl_.b], writes=[slc_.b])
                    S.op("dve", lambda e: e.scalar_tensor_tensor(out=ab.t[:, m, :], in0=lc_.t[:], scalar=-6.0, in1=slc_.t[:],
                                                                 op0=ALU.max, op1=ALU.mult),
                         reads=[lc_.b, slc_.b], writes=[ab.bs[m]])
                    if si < len(steps):
                        steps[si]()
                        si += 1
                while si < len(steps):
                    steps[si]()
                    si += 1

            def emit_down(e_, sub):
                ab = actb[sub % 2]
                wdd = wd[e_ % len(wd)]
                for j in range(4):
                    tt = sub * 4 + j
                    pd = k.ps[2]
                    for h in range(2):
                        for c in range(8):
                            S.op("pe", lambda e: e.matmul(
                                pd.t[:, h * 512:(h + 1) * 512],
                                lhsT=ab.t[:, c, j * 128:(j + 1) * 128],
                                rhs=wdd.t[:, c, h * 512:(h + 1) * 512], start=(c == 0), stop=(c == 7)),
                                reads=[ab.bs[c], wdd.bs[c // 2]], writes=[pd.bs[h]], inc=(c == 7))
                        S.op("dve", lambda e: e.scalar_tensor_tensor(
                            out=acc[tt].t[:, h * 512:(h + 1) * 512], in0=pd.t[:, h * 512:(h + 1) * 512],
                            scalar=gate.t[:, tt, e_:e_ + 1], in1=acc[tt].t[:, h * 512:(h + 1) * 512],
                            op0=ALU.mult, op1=ALU.add),
                            reads=[pd.bs[h], gate.bs[tt], acc[tt].b], writes=[acc[tt].b])

            blocks = [(e_, sub) for e_ in range(n_e) for sub in range(TC // 512)]
            d_for = {}
            last_sub = TC // 512 - 1
            for bi, (e_, sub) in enumerate(blocks):
                steps = []
                if sub == 0:
                    if e_ + 1 < n_e:
                        nx = prep_steps(e_ + 1, wgu[(e_ + 1) % 2], wd[0])
                    elif ch + 1 < n_ch:
                        nx = prep_steps(0, wgu[0], wd[0])
                    else:
                        nx = []
                    steps, d_for[e_ + 1] = nx[:8], nx[8:]
                emit_gu(e_, sub, steps)
                if bi >= 1:
                    pe_, psub = blocks[bi - 1]
                    emit_down(pe_, psub)
                    if psub == last_sub:
                        for f in d_for.pop(pe_ + 1, []):
                            f()
            pe_, psub = blocks[-1]
            emit_down(pe_, psub)
            for f in d_for.pop(pe_ + 1, []):
                f()
            if stop < 4:
                continue
            for tt in range(TPC):
                t = t0 + tt
                epilogue(k, t, acc[tt], lng, lnb, small, sm_b, final)
        S.barrier()
    k.xcur = k.xres
    k.xcur_is_in = False


def epilogue(k, t, acc, lng, lnb, small, sm_b, final, src=None, src_bufs=None):
    S = k.S
    xt = k.xt[t % 2]
    src_b = k.xres_b[t] if not k.xcur_is_in else Buf()
    if src is None:
        src, src_bufs = acc.t[:], [acc.b]
    S.dma(xt.t[:], k.xcur[t * 128:(t + 1) * 128, :], reads=[src_b], writes=[xt.b])
    S.op("dve", lambda e: e.scalar_tensor_tensor(out=acc.t[:], in0=xt.t[:], scalar=ALPHA, in1=src,
                                                 op0=ALU.mult, op1=ALU.add),
         reads=[xt.b] + list(src_bufs), writes=[acc.b])
    S.op("dve", lambda e: e.bn_stats(out=small.t[:, 16:22], in_=acc.t[:, 0:512]), reads=[acc.b], writes=[sm_b[4]])
    S.op("dve", lambda e: e.bn_stats(out=small.t[:, 22:28], in_=acc.t[:, 512:1024]), reads=[acc.b], writes=[sm_b[5]])
    S.op("dve", lambda e: e.bn_aggr(out=small.t[:, 28:30], in_=small.t[:, 16:28]), reads=[sm_b[4], sm_b[5]], writes=[sm_b[6]])
    S.op("dve", lambda e: e.tensor_scalar(out=small.t[:, 30:31], in0=small.t[:, 29:30], scalar1=LN_EPS, scalar2=None, op0=ALU.add),
         reads=[sm_b[6]], writes=[sm_b[7]])
    S.op("act", lambda e: e.activation(out=small.t[:, 31:32], in_=small.t[:, 30:31], func=AF.Sqrt),
         reads=[sm_b[7]], writes=[sm_b[7]])
    S.op("dve", lambda e: e.reciprocal(out=small.t[:, 32:33], in_=small.t[:, 31:32]), reads=[sm_b[7]], writes=[sm_b[7]])
    S.op("dve", lambda e: e.tensor_scalar(out=acc.t[:], in0=acc.t[:], scalar1=small.t[:, 28:29], scalar2=small.t[:, 32:33],
                                          op0=ALU.subtract, op1=ALU.mult),
         reads=[acc.b, sm_b[6], sm_b[7]], writes=[acc.b])
    S.op("pool", lambda e: e.tensor_tensor(out=acc.t[:], in0=acc.t[:], in1=lng.t[:], op=ALU.mult),
         reads=[acc.b, lng.b], writes=[acc.b])
    S.op("pool", lambda e: e.tensor_tensor(out=xt.t[:], in0=acc.t[:], in1=lnb.t[:], op=ALU.add),
         reads=[acc.b, lnb.b], writes=[xt.b])
    if final:
        S.dma(k.y[t * 128:(t + 1) * 128, :], xt.t[:], reads=[xt.b], writes=[k.y_b[t]])
    else:
        S.dma(k.xres[t * 128:(t + 1) * 128, :], xt.t[:], reads=[xt.b], writes=[k.xres_b[t]])
        emit_xT(k, t, xt.t, xt.b)


QL, KVL, ROPE = 384, 256, 64
QK_SCALE = 192.0 ** -0.5
RMS_EPS = 1e-6


def load_cast(k, S, stg, dst_fn, src_ap, width, eng="act"):
    S.dma(stg.t[:, 0:width], src_ap, writes=[stg.b])


def odd_phase(k, l):
    S, nc = k.S, k.nc
    i = l // 2
    w_in = k.inp("odd_w_in_%d" % i, [D, 704])
    q_norm = k.inp("odd_q_norm_%d" % i, [1, QL])
    w_uq = k.inp("odd_w_uq_%d" % i, [QL, 1536])
    kv_norm = k.inp("odd_kv_norm_%d" % i, [1, KVL])
    w_ukv = k.inp("odd_w_ukv_%d" % i, [KVL, 2048])
    w_out = k.inp("odd_w_out_%d" % i, [D, D])
    ln_g = k.inp("ln_mix_g_%d" % l, [1, D])
    ln_b = k.inp("ln_mix_b_%d" % l, [1, D])
    cos2T = k.inp("cos2T", [128, SEQ])
    sin2T = k.inp("sin2T", [128, SEQ])
    oT_d = nc.dram_tensor("oT_d_%d" % l, [8, 128, SEQ], BF16, kind="Internal").ap()
    oT_b = [Buf() for _ in range(8)]
    n_h = (k.dbg or {}).get("n_h", 8)
    with contextlib.ExitStack() as st:
        def sb(name, shape, dt=F32, nb=1):
            return T(st.enter_context(nc.sbuf_tensor("sbo%d_" % l + name, list(shape), dt)), nb)
        w_in_b = sb("w_in_b", [128, 8, 704], BF16)
        wkp = sb("wkp", [128, 8, 128], BF16)
        wkr = sb("wkr", [128, 8, 128], BF16)
        w_uq_b = sb("w_uq_b", [128, 3, 1536], BF16)
        wqr = sb("wqr", [128, 3, 8, 128], BF16)
        wqrot = sb("wqrot", [128, 3, 8, 128], BF16)
        w_ukv_b = sb("w_ukv_b", [128, 2, 2048], BF16)
        qn_bc = sb("qn_bc", [128, QL])
        kvn_bc = sb("kvn_bc", [128, KVL])
        lng = sb("lng", [128, D])
        lnb = sb("lnb", [128, D])
        ones_b = sb("ones_b", [128, 128], BF16)
        S.op("pool", lambda e: e.memset(ones_b.t[:], 1.0), writes=[ones_b.b])
        S.dma(qn_bc.t[:], q_norm.partition_broadcast(128), writes=[qn_bc.b])
        S.dma(kvn_bc.t[:], kv_norm.partition_broadcast(128), writes=[kvn_bc.b])
        S.dma(lng.t[:], ln_g.partition_broadcast(128), writes=[lng.b])
        S.dma(lnb.t[:], ln_b.partition_broadcast(128), writes=[lnb.b])
        cqnT = sb("cqnT", [128, 3, SEQ], BF16, nb=8)
        ckvnT = sb("ckvnT", [128, 2, SEQ], BF16, nb=8)
        kpeT = sb("kpeT", [128, SEQ], BF16, nb=8)
        xTc = [sb("xTc%d" % j, [128, 8, 512], BF16) for j in range(2)]
        cosc = [sb("cosc%d" % j, [128, 512]) for j in range(2)]
        sinc = [sb("sinc%d" % j, [128, 512]) for j in range(2)]
        tmp1 = sb("tmp1", [128, 512])
        tmp2 = sb("tmp2", [128, 512])
        junk = sb("junk", [128, 512])
        cn = sb("cn", [128, 640])
        small = sb("small", [128, 64])
        sm_b = [Buf() for _ in range(8)]
        ssb = [Buf() for _ in range(4)]
        rr = [0]
        stA = contextlib.ExitStack()
        stg = [T(stA.enter_context(nc.sbuf_tensor("sbo%d_stg%d" % (l, j), [128, 2048], F32))) for j in range(2)]

        def ld(dst_ap, src_ap, shape3, dstT):
            s_ = stg[rr[0]]
            rr[0] ^= 1
            c_, n_ = shape3
            S.dma(s_.t[:, 0:c_ * n_].rearrange("p (c n) -> p c n", c=c_), src_ap, writes=[s_.b])
            S.op("act", lambda e: e.copy(out=dst_ap, in_=s_.t[:, 0:c_ * n_].rearrange("p (c n) -> p c n", c=c_)),
                 reads=[s_.b], writes=[dstT.b])
        for c2 in range(4):
            ld(w_in_b.t[:, 2 * c2:2 * c2 + 2, :], w_in[c2 * 256:(c2 + 1) * 256, :].rearrange("(c p) n -> p c n", p=128), (2, 704), w_in_b)
        for j in range(3):
            ld(w_uq_b.t[:, j:j + 1, :], w_uq[j * 128:(j + 1) * 128, :].rearrange("(c p) n -> p c n", p=128), (1, 1536), w_uq_b)
        for j in range(2):
            ld(w_ukv_b.t[:, j:j + 1, :], w_ukv[j * 128:(j + 1) * 128, :].rearrange("(c p) n -> p c n", p=128), (1, 2048), w_ukv_b)
        S.barrier()
        stA.close()
        S.op("pool", lambda e: e.memset(wkp.t[:], 0.0), writes=[wkp.b])
        S.op("pool", lambda e: e.memset(wkr.t[:], 0.0), writes=[wkr.b])
        S.op("pool", lambda e: e.memset(wqr.t[:], 0.0), writes=[wqr.b])
        S.op("pool", lambda e: e.memset(wqrot.t[:], 0.0), writes=[wqrot.b])
        S.op("dve", lambda e: e.tensor_copy(out=wkp.t[:, :, 0:64], in_=w_in_b.t[:, :, 640:704]), reads=[w_in_b.b], writes=[wkp.b])
        S.op("dve", lambda e: e.tensor_scalar(out=wkr.t[:, :, 0:32], in0=w_in_b.t[:, :, 672:704], scalar1=-1.0, scalar2=None, op0=ALU.mult),
             reads=[w_in_b.b], writes=[wkr.b])
        S.op("dve", lambda e: e.tensor_copy(out=wkr.t[:, :, 32:64], in_=w_in_b.t[:, :, 640:672]), reads=[w_in_b.b], writes=[wkr.b])
        for j in range(3):
            v_ = w_uq_b.t[:, j, :].rearrange("p (h d) -> p h d", h=8)
            S.op("dve", lambda e: e.tensor_copy(out=wqr.t[:, j, :, 0:64], in_=v_[:, :, 128:192]), reads=[w_uq_b.b], writes=[wqr.b])
            S.op("dve", lambda e: e.tensor_scalar(out=wqrot.t[:, j, :, 0:32], in0=v_[:, :, 160:192], scalar1=-1.0, scalar2=None, op0=ALU.mult),
                 reads=[w_uq_b.b], writes=[wqrot.b])
            S.op("dve", lambda e: e.tensor_copy(out=wqrot.t[:, j, :, 32:64], in_=v_[:, :, 128:160]), reads=[w_uq_b.b], writes=[wqrot.b])


        def rope_combine(pa, pa_b, pb, pb_b, cc, sc, dst_ap, dst_b):
            S.op("dve", lambda e: e.tensor_tensor(out=tmp1.t[:], in0=pa, in1=cc.t[:], op=ALU.mult),
                 reads=[pa_b, cc.b], writes=[tmp1.b])
            S.op("dve", lambda e: e.tensor_tensor(out=tmp2.t[:], in0=pb, in1=sc.t[:], op=ALU.mult),
                 reads=[pb_b, sc.b], writes=[tmp2.b])
            S.op("pool", lambda e: e.tensor_tensor(out=dst_ap, in0=tmp1.t[:], in1=tmp2.t[:], op=ALU.add),
                 reads=[tmp1.b, tmp2.b], writes=[dst_b])

        ostop = (k.dbg or {}).get("ostop", 99)
        for ch in range(8 if ostop >= 1 else 0):
            xc = xTc[ch % 2]
            cc, sc = cosc[ch % 2], sinc[ch % 2]
            tok = slice(ch * 512, (ch + 1) * 512)
            S.dma(xc.t[:], k.xT_d.rearrange("c p t -> p c t")[:, :, tok], reads=k.xT_b[ch * 4:ch * 4 + 4], writes=[xc.b])
            S.dma(cc.t[:], cos2T[:, tok], writes=[cc.b])
            S.dma(sc.t[:], sin2T[:, tok], writes=[sc.b])
            for j4 in range(4):
                t = ch * 4 + j4
                tl_ = slice(j4 * 128, (j4 + 1) * 128)
                pp = k.ps[j4 % 2]
                for (a, b_, hb) in ((0, 512, 0), (512, 704, 1)):
                    for c in range(8):
                        S.op("pe", lambda e: e.matmul(pp.t[:, a:b_], lhsT=xc.t[:, c, tl_], rhs=w_in_b.t[:, c, a:b_],
                                                      start=(c == 0), stop=(c == 7)),
                             reads=[xc.b, w_in_b.b], writes=[pp.bs[hb]], inc=(c == 7))
                S.op("act", lambda e: e.activation(out=junk.t[:, 0:QL], in_=pp.t[:, 0:QL], func=AF.Square, accum_out=small.t[:, 0:1]),
                     reads=[pp.bs[0]], writes=[junk.b, ssb[0]])
                S.op("act", lambda e: e.activation(out=junk.t[:, 0:KVL], in_=pp.t[:, QL:QL + KVL], func=AF.Square, accum_out=small.t[:, 1:2]),
                     reads=pp.bs, writes=[junk.b, ssb[1]])
                S.op("dve", lambda e: e.tensor_scalar(out=small.t[:, 2:3], in0=small.t[:, 0:1], scalar1=1.0 / QL, scalar2=RMS_EPS,
                                                      op0=ALU.mult, op1=ALU.add), reads=[ssb[0]], writes=[ssb[2]])
                S.op("dve", lambda e: e.tensor_scalar(out=small.t[:, 3:4], in0=small.t[:, 1:2], scalar1=1.0 / KVL, scalar2=RMS_EPS,
                                                      op0=ALU.mult, op1=ALU.add), reads=[ssb[1]], writes=[ssb[2]])
                S.op("act", lambda e: e.activation(out=small.t[:, 4:6], in_=small.t[:, 2:4], func=AF.Sqrt), reads=[ssb[2]], writes=[ssb[3]])
                S.op("dve", lambda e: e.reciprocal(out=small.t[:, 6:8], in_=small.t[:, 4:6]), reads=[ssb[3]], writes=[ssb[3]])
                S.op("dve", lambda e: e.scalar_tensor_tensor(out=cn.t[:, 0:QL], in0=pp.t[:, 0:QL], scalar=small.t[:, 6:7], in1=qn_bc.t[:],
                                                             op0=ALU.mult, op1=ALU.mult),
                     reads=[pp.bs[0], ssb[3], qn_bc.b], writes=[cn.b])
                S.op("dve", lambda e: e.scalar_tensor_tensor(out=cn.t[:, QL:640], in0=pp.t[:, QL:640], scalar=small.t[:, 7:8], in1=kvn_bc.t[:],
                                                             op0=ALU.mult, op1=ALU.mult),
                     reads=pp.bs + [ssb[3], kvn_bc.b], writes=[cn.b])
                pt = k.ps[2]
                for j in range(5):
                    S.op("pe", lambda e: e.transpose(out=pt.t[:, j * 128:(j + 1) * 128], in_=cn.t[:, j * 128:(j + 1) * 128], identity=k.ident.t[:]),
                         reads=[cn.b, k.ident.b], writes=[pt.bs[j // 4]], inc=(j in (3, 4)))
                S.op("act", lambda e: e.copy(out=cqnT.t[:, :, t * 128:(t + 1) * 128], in_=pt.t[:, 0:384].rearrange("p (j t) -> p j t", j=3)),
                     reads=[pt.bs[0]], writes=[cqnT.bs[ch]])
                S.op("act", lambda e: e.copy(out=ckvnT.t[:, :, t * 128:(t + 1) * 128], in_=pt.t[:, 384:640].rearrange("p (j t) -> p j t", j=2)),
                     reads=pt.bs, writes=[ckvnT.bs[ch]])
            pk = k.ps[3]
            for (wt, hb) in ((wkp, 0), (wkr, 1)):
                for c in range(8):
                    S.op("pe", lambda e: e.matmul(pk.t[:, hb * 512:(hb + 1) * 512], lhsT=wt.t[:, c, :], rhs=xc.t[:, c, :],
                                                  start=(c == 0), stop=(c == 7)),
                         reads=[wt.b, xc.b], writes=[pk.bs[hb]], inc=(c == 7))
            rope_combine(pk.t[:, 0:512], pk.bs[0], pk.t[:, 512:1024], pk.bs[1], cc, sc, kpeT.t[:, tok], kpeT.bs[ch])

        stB = contextlib.ExitStack()

        def sb(name, shape, dt=F32, nb=1):
            return T(stB.enter_context(nc.sbuf_tensor("sbo%d_" % l + name, list(shape), dt)), nb)
        qn = sb("qn", [128, SEQ], BF16, nb=8)
        qr = sb("qr", [128, SEQ], BF16, nb=8)
        kn = sb("kn", [128, SEQ], BF16, nb=8)
        vv = sb("vv", [128, NT, 128], BF16, nb=8)
        oTh = sb("oTh", [128, SEQ], BF16)
        sq = sb("sq", [128, 512], BF16)
        PT = [sb("PT%d" % j, [128, 512], BF16) for j in range(2)]
        rs = sb("rs", [128, 128])
        mq = sb("mq", [128, 16])
        negB = sb("negB", [128, 8])
        for h in range(n_h if ostop >= 2 else 0):
            for ch in range(8):
                tok = slice(ch * 512, (ch + 1) * 512)
                cc, sc = cosc[ch % 2], sinc[ch % 2]
                S.dma(cc.t[:], cos2T[:, tok], writes=[cc.b])
                S.dma(sc.t[:], sin2T[:, tok], writes=[sc.b])
                p0 = k.ps[0]
                for j in range(3):
                    S.op("pe", lambda e: e.matmul(p0.t[:, 0:512], lhsT=w_uq_b.t[:, j, h * 192:h * 192 + 128], rhs=cqnT.t[:, j, tok],
                                                  start=(j == 0), stop=(j == 2)),
                         reads=[w_uq_b.b, cqnT.bs[ch]], writes=[p0.bs[0]], inc=(j == 2))
                S.op("act", lambda e: e.copy(out=qn.t[:, tok], in_=p0.t[:, 0:512]), reads=[p0.bs[0]], writes=[qn.bs[ch]])
                for j in range(2):
                    S.op("pe", lambda e: e.matmul(p0.t[:, 512:1024], lhsT=w_ukv_b.t[:, j, h * 256:h * 256 + 128], rhs=ckvnT.t[:, j, tok],
                                                  start=(j == 0), stop=(j == 1)),
                         reads=[w_ukv_b.b, ckvnT.bs[ch]], writes=[p0.bs[1]], inc=(j == 1))
                S.op("act", lambda e: e.copy(out=kn.t[:, tok], in_=p0.t[:, 512:1024]), reads=[p0.bs[1]], writes=[kn.bs[ch]])
                p1 = k.ps[1]
                for (wt, hb) in ((wqr, 0), (wqrot, 1)):
                    for j in range(3):
                        S.op("pe", lambda e: e.matmul(p1.t[:, hb * 512:(hb + 1) * 512], lhsT=wt.t[:, j, h, :], rhs=cqnT.t[:, j, tok],
                                                      start=(j == 0), stop=(j == 2)),
                             reads=[wt.b, cqnT.bs[ch]], writes=[p1.bs[hb]], inc=(j == 2))
                rope_combine(p1.t[:, 0:512], p1.bs[0], p1.t[:, 512:1024], p1.bs[1], cc, sc, qr.t[:, tok], qr.bs[ch])
                p2 = k.ps[2]
                for j4 in range(4):
                    t = ch * 4 + j4
                    for j in range(2):
                        S.op("pe", lambda e: e.matmul(p2.t[:, j4 * 128:(j4 + 1) * 128], lhsT=ckvnT.t[:, j, t * 128:(t + 1) * 128],
                                                      rhs=w_ukv_b.t[:, j, h * 256 + 128:h * 256 + 256], start=(j == 0), stop=(j == 1)),
                             reads=[w_ukv_b.b, ckvnT.bs[ch]], writes=[p2.bs[0]], inc=(j == 1 and j4 == 3))
                S.op("act", lambda e: e.copy(out=vv.t[:, ch * 4:ch * 4 + 4, :], in_=p2.t[:, 0:512].rearrange("p (a b) -> p a b", a=4)),
                     reads=[p2.bs[0]], writes=[vv.bs[ch]])
                p3 = k.ps[3]
                for half, (srcs) in enumerate((((qn, qn.bs[ch]), (qr, qr.bs[ch])), ((kn, kn.bs[ch]), (kpeT, kpeT.bs[ch])))):
                    for si, (tt_, tb_) in enumerate(srcs):
                        S.op("act", lambda e: e.activation(out=sq.t[:], in_=tt_.t[:, tok], func=AF.Square), reads=[tb_], writes=[sq.b])
                        S.op("pe", lambda e: e.matmul(p3.t[:, half * 512:(half + 1) * 512], lhsT=ones_b.t[:], rhs=sq.t[:],
                                                      start=(si == 0), stop=(si == 1)),
                             reads=[ones_b.b, sq.b], writes=[p3.bs[half]])
                    S.op("dve", lambda e: e.tensor_reduce(out=mq.t[:, half * 8 + ch:half * 8 + ch + 1], in_=p3.t[:, half * 512:(half + 1) * 512],
                                                          axis=AX.X, op=ALU.max),
                         reads=[p3.bs[half]], writes=[mq.b])
            S.op("dve", lambda e: e.tensor_reduce(out=small.t[:, 8:9], in_=mq.t[:, 0:8], axis=AX.X, op=ALU.max), reads=[mq.b], writes=[sm_b[0]])
            S.op("dve", lambda e: e.tensor_reduce(out=small.t[:, 9:10], in_=mq.t[:, 8:16], axis=AX.X, op=ALU.max), reads=[mq.b], writes=[sm_b[0]])
            S.op("dve", lambda e: e.tensor_tensor(out=small.t[:, 10:11], in0=small.t[:, 8:9], in1=small.t[:, 9:10], op=ALU.mult),
                 reads=[sm_b[0]], writes=[sm_b[1]])
            S.op("act", lambda e: e.activation(out=small.t[:, 11:12], in_=small.t[:, 10:11], func=AF.Sqrt), reads=[sm_b[1]], writes=[sm_b[1]])
            S.op("dve", lambda e: e.tensor_scalar(out=negB.t[:, h:h + 1], in0=small.t[:, 11:12], scalar1=-QK_SCALE, scalar2=None, op0=ALU.mult),
                 reads=[sm_b[1]], writes=[negB.b])
            groups = []
            for qt in range(NT if ostop >= 3 else 0):
                nblk = qt + 1
                for g0 in range(0, nblk, 4):
                    groups.append((qt, g0, min(4, nblk - g0)))

            def emit_scores(gi, qt, g0, nb_):
                qs = slice(qt * 128, (qt + 1) * 128)
                psS = k.ps[(gi // 2) % 2]
                hb = gi % 2
                pt_ = PT[gi % 2]
                for bi in range(nb_):
                    kb = g0 + bi
                    ks = slice(kb * 128, (kb + 1) * 128)
                    o_ = psS.t[:, hb * 512 + bi * 128:hb * 512 + (bi + 1) * 128]
                    S.op("pe", lambda e: e.matmul(o_, lhsT=kn.t[:, ks], rhs=qn.t[:, qs], start=True, stop=False),
                         reads=[kn.bs[kb // 4], qn.bs[qt // 4]], writes=[psS.bs[hb]], inc=False)
                    S.op("pe", lambda e: e.matmul(o_, lhsT=kpeT.t[:, ks], rhs=qr.t[:, qs], start=False, stop=True),
                         reads=[kpeT.bs[kb // 4], qr.bs[qt // 4]], writes=[psS.bs[hb]], inc=(bi == nb_ - 1))
                S.op("act", lambda e: e.activation(out=pt_.t[:, 0:nb_ * 128], in_=psS.t[:, hb * 512:hb * 512 + nb_ * 128], func=AF.Exp,
                                                   bias=negB.t[:, h:h + 1], scale=QK_SCALE),
                     reads=[psS.bs[hb], negB.b], writes=[pt_.b])
                if g0 + nb_ == qt + 1:
                    bi = nb_ - 1
                    S.op("pool", lambda e: e.memset(pt_.t[64:128, bi * 128:bi * 128 + 64], 0.0), writes=[pt_.b])

            def emit_pv(gi, qt, g0, nb_):
                qs = slice(qt * 128, (qt + 1) * 128)
                po = k.ps[2 + (qt % 2)]
                pt_ = PT[gi % 2]
                nblk = qt + 1
                for bi in range(nb_):
                    kb = g0 + bi
                    S.op("pe", lambda e: e.matmul(po.t[:, 0:128], lhsT=vv.t[:, kb, :], rhs=pt_.t[:, bi * 128:(bi + 1) * 128],
                                                  start=(kb == 0), stop=(kb == nblk - 1)),
                         reads=[vv.bs[kb // 4], pt_.b], writes=[po.bs[0]], inc=False)
                    S.op("pe", lambda e: e.matmul(po.t[:, 128:256], lhsT=ones_b.t[:], rhs=pt_.t[:, bi * 128:(bi + 1) * 128],
                                                  start=False, stop=(kb == nblk - 1)),
                         reads=[ones_b.b, pt_.b], writes=[po.bs[0]], inc=(bi == nb_ - 1))
                if g0 + nb_ == nblk:
                    S.op("dve", lambda e: e.reciprocal(out=rs.t[:], in_=po.t[:, 128:256]), reads=[po.bs[0]], writes=[rs.b])
                    S.op("dve", lambda e: e.tensor_tensor(out=oTh.t[:, qs], in0=po.t[:, 0:128], in1=rs.t[:], op=ALU.mult),
                         reads=[po.bs[0], rs.b], writes=[oTh.b])

            for gi, grp in enumerate(groups):
                emit_scores(gi, *grp)
                if gi >= 1:
                    emit_pv(gi - 1, *groups[gi - 1])
            if groups:
                emit_pv(len(groups) - 1, *groups[-1])
            S.dma(oT_d[h], oTh.t[:], reads=[oTh.b], writes=[oT_b[h]])

        S.barrier()
        stB.close()

        def sb(name, shape, dt=F32, nb=1):
            return T(st.enter_context(nc.sbuf_tensor("sbo%d_" % l + name, list(shape), dt)), nb)
        stg = [sb("stgC", [128, 2048])]
        rr[0] = 0
        w_out_b = sb("w_out_b", [128, 8, D], BF16)
        for c2 in range(4):
            ld(w_out_b.t[:, 2 * c2:2 * c2 + 2, :], w_out[c2 * 256:(c2 + 1) * 256, :].rearrange("(c p) n -> p c n", p=128), (2, D), w_out_b)
            rr[0] = 0
        work = [sb("work%d" % j, [128, D]) for j in range(1)]
        for ch in range(8 if ostop >= 4 else 0):
            xc = xTc[ch % 2]
            tok = slice(ch * 512, (ch + 1) * 512)
            S.dma(xc.t[:], oT_d.rearrange("c p t -> p c t")[:, :, tok], reads=oT_b, writes=[xc.b])
            for j4 in range(4):
                t = ch * 4 + j4
                pp = k.ps[j4 % 2]
                for hb in range(2):
                    for c in range(8):
                        S.op("pe", lambda e: e.matmul(pp.t[:, hb * 512:(hb + 1) * 512], lhsT=xc.t[:, c, j4 * 128:(j4 + 1) * 128],
                                                      rhs=w_out_b.t[:, c, hb * 512:(hb + 1) * 512], start=(c == 0), stop=(c == 7)),
                             reads=[xc.b, w_out_b.b], writes=[pp.bs[hb]], inc=(c == 7))
                epilogue(k, t, work[0], lng, lnb, small, sm_b, False, src=pp.t[:], src_bufs=pp.bs)
        S.barrier()
    k.xcur = k.xres
    k.xcur_is_in = False


def even_phase(k, l):
    S, nc = k.S, k.nc
    i = l // 2
    w_in = k.inp("even_w_in_%d" % i, [D, 3080])
    w_out = k.inp("even_w_out_%d" % i, [D, D])
    sgT = k.inp("even_sgu_wT_%d" % i, [128, 4, 128])
    bsT_d = k.inp("even_sgu_bT_%d" % i, [128, 4])
    sg_d = k.inp("even_sgu_ln_g_%d" % i, [1, 512])
    sb_d = k.inp("even_sgu_ln_b_%d" % i, [1, 512])
    cw_d = k.inp("even_conv_wT_%d" % i, [128, 12, 4])
    alog_d = k.inp("even_a_log_%d" % i, [1, 4])
    dtb_d = k.inp("even_dt_bias_%d" % i, [1, 4])
    gnw_d = k.inp("even_gdn_norm_%d" % i, [1, 128])
    ln_g = k.inp("ln_mix_g_%d" % l, [1, D])
    ln_b = k.inp("ln_mix_b_%d" % l, [1, D])
    tri_d = k.inp("tri", [128, 128])
    maskL_d = k.inp("maskL", [128, 128])
    n_chunks = (k.dbg or {}).get("e_ch", 8)
    with contextlib.ExitStack() as st:
        def sb(name, shape, dt=F32, nb=1):
            return T(st.enter_context(nc.sbuf_tensor("sbe%d_" % l + name, list(shape), dt)), nb)
        w_in_b = sb("w_in_b", [128, 8, 3080], BF16)
        w_out_b = sb("w_out_b", [128, 8, D], BF16)
        wsT = sb("wsT", [128, 4, 128], BF16)
        bsT = sb("bsT", [128, 4])
        sg_bc = sb("sg_bc", [128, 512])
        sb_bc = sb("sb_bc", [128, 512])
        cw = sb("cw", [128, 12, 4])
        negA = sb("negA", [128, 4])
        dtb = sb("dtb", [128, 4])
        gnw = sb("gnw", [128, 128])
        lng = sb("lng", [128, D])
        lnb = sb("lnb", [128, D])
        tri = sb("tri", [128, 128])
        maskL = sb("maskL", [128, 128])
        ones_f = sb("ones_f", [128, 128])
        S.op("pool", lambda e: e.memset(ones_f.t[:], 1.0), writes=[ones_f.b])
        for (dst, src) in ((bsT, bsT_d), (cw, cw_d), (tri, tri_d), (maskL, maskL_d)):
            S.dma(dst.t[:], src, writes=[dst.b])
        for (dst, src) in ((sg_bc, sg_d), (sb_bc, sb_d), (negA, alog_d), (dtb, dtb_d), (gnw, gnw_d), (lng, ln_g), (lnb, ln_b)):
            S.dma(dst.t[:], src.partition_broadcast(128), writes=[dst.b])
        S.op("act", lambda e: e.activation(out=negA.t[:], in_=negA.t[:], func=AF.Exp), reads=[negA.b], writes=[negA.b])
        S.op("dve", lambda e: e.tensor_scalar(out=negA.t[:], in0=negA.t[:], scalar1=-1.0, scalar2=None, op0=ALU.mult),
             reads=[negA.b], writes=[negA.b])
        stA = contextlib.ExitStack()
        stg = [T(stA.enter_context(nc.sbuf_tensor("sbe%d_stg%d" % (l, j), [128, 3080], F32))) for j in range(2)]
        for c in range(8):
            s_ = stg[c % 2]
            S.dma(s_.t[:], w_in[c * 128:(c + 1) * 128, :], writes=[s_.b])
            S.op("act" if c % 2 else "pool", lambda e: (e.copy if c % 2 else e.tensor_copy)(out=w_in_b.t[:, c, :], in_=s_.t[:]),
                 reads=[s_.b], writes=[w_in_b.b])
        for c2 in range(4):
            s_ = stg[c2 % 2]
            S.dma(s_.t[:, 0:2048].rearrange("p (c n) -> p c n", c=2), w_out[c2 * 256:(c2 + 1) * 256, :].rearrange("(c p) n -> p c n", p=128),
                  writes=[s_.b])
            S.op("act", lambda e: e.copy(out=w_out_b.t[:, 2 * c2:2 * c2 + 2, :], in_=s_.t[:, 0:2048].rearrange("p (c n) -> p c n", c=2)),
                 reads=[s_.b], writes=[w_out_b.b])
        s_ = stg[0]
        S.dma(s_.t[:, 0:512].rearrange("p (g i) -> p g i", g=4), sgT, writes=[s_.b])
        S.op("dve", lambda e: e.tensor_copy(out=wsT.t[:], in_=s_.t[:, 0:512].rearrange("p (g i) -> p g i", g=4)), reads=[s_.b], writes=[wsT.b])
        S.op("dve", lambda e: e.memset(wsT.t[64:128, :, 0:64], 0.0), writes=[wsT.b])
        S.barrier()
        stA.close()

        xTc = [sb("xTc%d" % j, [128, 8, 512], BF16) for j in range(2)]
        raw = sb("raw", [128, 12, 515], nb=12)
        qkv = sb("qkv", [128, 12, 512], nb=12)
        S.op("pool", lambda e: e.memset(raw.t[:, :, 0:3], 0.0), writes=raw.bs)
        cvt = sb("cvt", [128, 512])
        sqt = sb("sqt", [128, 512])
        rst = sb("rst", [128, 512])
        u_sb = sb("u_sb", [128, 512])
        vg = sb("vg", [128, 512])
        vnb = sb("vnb", [128, 512], BF16)
        sz = sb("sz", [128, 512])
        mixt = sb("mixt", [128, D], nb=8)
        work = sb("work", [128, D])
        small = sb("small", [128, 64])
        sm_b = [Buf() for _ in range(8)]
        sm2 = sb("sm2", [128, 96])
        s2 = [Buf() for _ in range(24)]
        St = [sb("St%d" % h, [128, 128]) for h in range(4)]
        for h in range(4):
            S.op("pool", lambda e: e.memset(St[h].t[:], 0.0), writes=[St[h].b])
        tn_sets = []
        for si_ in range(2):
            tn = {}
            for nm in ("kbg", "kdec", "vb", "gB", "t1", "t2", "Dn", "DT", "egr", "qdT", "M0", "M1", "N0", "N1", "P0", "P1", "intraT", "u", "wT",
                       "vnew", "junk", "yb"):
                tn[nm] = sb("g%d_" % si_ + nm, [128, 128])
            tn_sets.append(tn)
        slots = [(k.ps[2].t[:, 0:128], k.ps[2].bs[0]), (k.ps[2].t[:, 512:640], k.ps[2].bs[1]),
                 (k.ps[3].t[:, 0:128], k.ps[3].bs[0]), (k.ps[3].t[:, 512:640], k.ps[3].bs[1])]
        sl_i = [0]

        def slot():
            r = slots[sl_i[0] % len(slots)]
            sl_i[0] += 1
            return r
        ident = k.ident

        def mm(out_ap, out_b, lhsT, lb, rhs, rb, start=True, stop=True):
            S.op("pe", lambda e: e.matmul(out_ap, lhsT=lhsT, rhs=rhs, start=start, stop=stop), reads=[lb, rb], writes=[out_b])

        estop = (k.dbg or {}).get("estop", 99)
        for ch in range(n_chunks if estop >= 1 else 0):
            xc = xTc[ch % 2]
            tok = slice(ch * 512, (ch + 1) * 512)
            S.dma(xc.t[:], k.xT_d.rearrange("c p t -> p c t")[:, :, tok], reads=k.xT_b[ch * 4:ch * 4 + 4], writes=[xc.b])
            for cc in range(12):
                pp = k.ps[1]
                hb = cc % 2
                for c in range(8):
                    S.op("pe", lambda e: e.matmul(pp.t[:, hb * 512:(hb + 1) * 512], lhsT=w_in_b.t[:, c, 1024 + cc * 128:1024 + (cc + 1) * 128],
                                                  rhs=xc.t[:, c, :], start=(c == 0), stop=(c == 7)),
                         reads=[w_in_b.b, xc.b], writes=[pp.bs[hb]], inc=(c == 7))
                if ch > 0:
                    S.op("dve", lambda e: e.tensor_copy(out=raw.t[:, cc, 0:3], in_=raw.t[:, cc, 512:515]), reads=[raw.bs[cc]], writes=[raw.bs[cc]])
                S.op("act", lambda e: e.copy(out=raw.t[:, cc, 3:515], in_=pp.t[:, hb * 512:(hb + 1) * 512]), reads=[pp.bs[hb]], writes=[raw.bs[cc]])
                S.op("act", lambda e: e.activation(out=cvt.t[:], in_=raw.t[:, cc, 3:515], func=AF.Copy, scale=cw.t[:, cc, 3:4]),
                     reads=[raw.bs[cc], cw.b], writes=[cvt.b])
                for s_ in (1, 2, 3):
                    S.op("dve", lambda e: e.scalar_tensor_tensor(out=cvt.t[:], in0=raw.t[:, cc, 3 - s_:515 - s_], scalar=cw.t[:, cc, 3 - s_:4 - s_],
                                                                 in1=cvt.t[:], op0=ALU.mult, op1=ALU.add),
                         reads=[raw.bs[cc], cw.b, cvt.b], writes=[cvt.b])
                S.op("act", lambda e: e.activation(out=qkv.t[:, cc, :], in_=cvt.t[:], func=AF.Silu), reads=[cvt.b], writes=[qkv.bs[cc]])
                if cc < 8:
                    S.op("act", lambda e: e.activation(out=sqt.t[:], in_=qkv.t[:, cc, :], func=AF.Square), reads=[qkv.bs[cc]], writes=[sqt.b])
                    pn = k.ps[0]
                    S.op("pe", lambda e: e.matmul(pn.t[:, hb * 512:(hb + 1) * 512], lhsT=ones_f.t[:], rhs=sqt.t[:], start=True, stop=True),
                         reads=[ones_f.b, sqt.b], writes=[pn.bs[hb]])
                    S.op("dve", lambda e: e.tensor_scalar(out=rst.t[:], in0=pn.t[:, hb * 512:(hb + 1) * 512], scalar1=RMS_EPS, scalar2=None, op0=ALU.add),
                         reads=[pn.bs[hb]], writes=[rst.b])
                    S.op("act", lambda e: e.activation(out=rst.t[:], in_=rst.t[:], func=AF.Sqrt), reads=[rst.b], writes=[rst.b])
                    S.op("dve", lambda e: e.reciprocal(out=rst.t[:], in_=rst.t[:]), reads=[rst.b], writes=[rst.b])
                    sc_ = (128.0 ** -0.5) if cc < 4 else 1.0
                    S.op("dve", lambda e: e.scalar_tensor_tensor(out=qkv.t[:, cc, :], in0=qkv.t[:, cc, :], scalar=sc_, in1=rst.t[:],
                                                                 op0=ALU.mult, op1=ALU.mult),
                         reads=[qkv.bs[cc], rst.b], writes=[qkv.bs[cc]])
            for j4 in range(4 if estop >= 2 else 0):
                t = ch * 4 + j4
                tl_ = slice(j4 * 128, (j4 + 1) * 128)
                pa = k.ps[0]
                for hb in range(2):
                    for c in range(8):
                        S.op("pe", lambda e: e.matmul(pa.t[:, hb * 512:(hb + 1) * 512], lhsT=xc.t[:, c, tl_], rhs=w_in_b.t[:, c, hb * 512:(hb + 1) * 512],
                                                      start=(c == 0), stop=(c == 7)),
                             reads=[xc.b, w_in_b.b], writes=[pa.bs[hb]], inc=(c == 7))
                S.op("act", lambda e: e.activation(out=u_sb.t[:], in_=pa.t[:, 0:512], func=AF.Gelu), reads=[pa.bs[0]], writes=[u_sb.b])
                S.op("act", lambda e: e.activation(out=vg.t[:], in_=pa.t[:, 512:1024], func=AF.Gelu), reads=[pa.bs[1]], writes=[vg.b])
                for g in range(4):
                    S.op("dve", lambda e: e.bn_stats(out=sm2.t[:, g * 6:(g + 1) * 6], in_=vg.t[:, g * 128:(g + 1) * 128]), reads=[vg.b], writes=[s2[g]])
                    S.op("dve", lambda e: e.bn_aggr(out=sm2.t[:, 24 + 2 * g:26 + 2 * g], in_=sm2.t[:, g * 6:(g + 1) * 6]), reads=[s2[g]], writes=[s2[4 + g]])
                    S.op("dve", lambda e: e.tensor_scalar(out=sm2.t[:, 32 + g:33 + g], in0=sm2.t[:, 25 + 2 * g:26 + 2 * g], scalar1=LN_EPS, scalar2=None,
                                                          op0=ALU.add), reads=[s2[4 + g]], writes=[s2[8]])
                S.op("act", lambda e: e.activation(out=sm2.t[:, 36:40], in_=sm2.t[:, 32:36], func=AF.Sqrt), reads=[s2[8]], writes=[s2[9]])
                S.op("dve", lambda e: e.reciprocal(out=sm2.t[:, 40:44], in_=sm2.t[:, 36:40]), reads=[s2[9]], writes=[s2[9]])
                for g in range(4):
                    S.op("dve", lambda e: e.tensor_scalar(out=vg.t[:, g * 128:(g + 1) * 128], in0=vg.t[:, g * 128:(g + 1) * 128],
                                                          scalar1=sm2.t[:, 24 + 2 * g:25 + 2 * g], scalar2=sm2.t[:, 40 + g:41 + g],
                                                          op0=ALU.subtract, op1=ALU.mult),
                         reads=[vg.b, s2[4 + g], s2[9]], writes=[vg.b])
                S.op("pool", lambda e: e.tensor_tensor(out=vg.t[:], in0=vg.t[:], in1=sg_bc.t[:], op=ALU.mult), reads=[vg.b, sg_bc.b], writes=[vg.b])
                S.op("pool", lambda e: e.tensor_tensor(out=vnb.t[:], in0=vg.t[:], in1=sb_bc.t[:], op=ALU.add), reads=[vg.b, sb_bc.b], writes=[vnb.b])
                pm = k.ps[1]
                for g in range(4):
                    S.op("pe", lambda e: e.matmul(pm.t[:, g * 128:(g + 1) * 128], lhsT=wsT.t[:, g, :], rhs=vnb.t[:, g * 128:(g + 1) * 128],
                                                  start=True, stop=True),
                         reads=[wsT.b, vnb.b], writes=[pm.bs[0]], inc=(g == 3))
                mb = mixt.bs[0]
                for g in range(4):
                    S.op("dve", lambda e: e.scalar_tensor_tensor(out=mixt.t[:, g * 128:(g + 1) * 128], in0=pm.t[:, g * 128:(g + 1) * 128],
                                                                 scalar=bsT.t[:, g:g + 1], in1=u_sb.t[:, g * 128:(g + 1) * 128],
                                                                 op0=ALU.add, op1=ALU.mult),
                         reads=[pm.bs[0], bsT.b, u_sb.b], writes=[mb])
                if estop < 3:
                    continue
                for c in range(8):
                    S.op("pe", lambda e: e.matmul(pm.t[:, 512:1024], lhsT=xc.t[:, c, tl_], rhs=w_in_b.t[:, c, 2560:3072], start=(c == 0), stop=(c == 7)),
                         reads=[xc.b, w_in_b.b], writes=[pm.bs[1]], inc=(c == 7))
                S.op("act", lambda e: e.activation(out=sz.t[:], in_=pm.t[:, 512:1024], func=AF.Silu), reads=[pm.bs[1]], writes=[sz.b])
                pab, pab_b = slot()
                for c in range(8):
                    S.op("pe", lambda e: e.matmul(pab[:, 0:8], lhsT=xc.t[:, c, tl_], rhs=w_in_b.t[:, c, 3072:3080], start=(c == 0), stop=(c == 7)),
                         reads=[xc.b, w_in_b.b], writes=[pab_b], inc=(c == 7))
                S.op("act", lambda e: e.activation(out=sm2.t[:, 48:52], in_=pab[:, 4:8], func=AF.Sigmoid), reads=[pab_b], writes=[s2[10]])
                S.op("dve", lambda e: e.tensor_scalar(out=sm2.t[:, 80:84], in0=sm2.t[:, 48:52], scalar1=-1.0, scalar2=None, op0=ALU.mult),
                     reads=[s2[10]], writes=[s2[18]])
                S.op("dve", lambda e: e.tensor_tensor(out=sm2.t[:, 76:80], in0=pab[:, 0:4], in1=dtb.t[:], op=ALU.add), reads=[pab_b, dtb.b], writes=[s2[11]])
                S.op("act", lambda e: e.activation(out=sm2.t[:, 76:80], in_=sm2.t[:, 76:80], func=AF.Exp), reads=[s2[11]], writes=[s2[11]])
                S.op("dve", lambda e: e.tensor_scalar(out=sm2.t[:, 76:80], in0=sm2.t[:, 76:80], scalar1=1.0, scalar2=None, op0=ALU.add),
                     reads=[s2[11]], writes=[s2[11]])
                S.op("act", lambda e: e.activation(out=sm2.t[:, 76:80], in_=sm2.t[:, 76:80], func=AF.Ln), reads=[s2[11]], writes=[s2[11]])
                S.op("dve", lambda e: e.tensor_tensor(out=sm2.t[:, 52:56], in0=sm2.t[:, 76:80], in1=negA.t[:], op=ALU.mult), reads=[s2[11], negA.b], writes=[s2[12]])
                pg, pg_b = slot()
                mm(pg[:, 0:4], pg_b, tri.t[:], tri.b, sm2.t[:, 52:56], s2[12])
                mm(pg[:, 4:8], pg_b, ones_f.t[:], ones_f.b, sm2.t[:, 52:56], s2[12])
                S.op("dve", lambda e: e.tensor_copy(out=sm2.t[:, 56:64], in_=pg[:, 0:8]), reads=[pg_b], writes=[s2[13]])
                S.op("act", lambda e: e.activation(out=sm2.t[:, 64:68], in_=sm2.t[:, 60:64], func=AF.Exp), reads=[s2[13]], writes=[s2[14]])
                S.op("act", lambda e: e.activation(out=sm2.t[:, 68:72], in_=sm2.t[:, 56:60], func=AF.Exp), reads=[s2[13]], writes=[s2[15]])
                S.op("dve", lambda e: e.tensor_tensor(out=sm2.t[:, 68:72], in0=sm2.t[:, 68:72], in1=sm2.t[:, 48:52], op=ALU.mult), reads=[s2[15], s2[10]], writes=[s2[15]])
                S.op("dve", lambda e: e.tensor_tensor(out=sm2.t[:, 72:76], in0=sm2.t[:, 60:64], in1=sm2.t[:, 56:60], op=ALU.subtract), reads=[s2[13]], writes=[s2[16]])
                S.op("act", lambda e: e.activation(out=sm2.t[:, 72:76], in_=sm2.t[:, 72:76], func=AF.Exp), reads=[s2[16]], writes=[s2[16]])

                if estop < 4:
                    continue
                def head_gen(h, tn):
                    qT = qkv.t[:, h, tl_]
                    kT = qkv.t[:, 4 + h, tl_]
                    vT = qkv.t[:, 8 + h, tl_]
                    qb, kb_, vb_ = qkv.bs[h], qkv.bs[4 + h], qkv.bs[8 + h]
                    col = lambda a: sm2.t[:, a + h:a + h + 1]
                    yield
                    pk, pk_b = slot()
                    mm(pk, pk_b, kT, kb_, ident.t[:], ident.b)
                    yield
                    pv, pv_b = slot()
                    mm(pv, pv_b, vT, vb_, ident.t[:], ident.b)
                    if estop <= 4.05:
                        return
                    S.op("dve", lambda e: e.tensor_scalar(out=tn["kbg"].t[:], in0=pk, scalar1=col(68), scalar2=None, op0=ALU.mult),
                         reads=[pk_b, s2[15]], writes=[tn["kbg"].b])
                    S.op("act", lambda e: e.activation(out=tn["kdec"].t[:], in_=pk, func=AF.Copy, scale=col(72)),
                         reads=[pk_b, s2[16]], writes=[tn["kdec"].b])
                    S.op("dve", lambda e: e.tensor_scalar(out=tn["vb"].t[:], in0=pv, scalar1=col(48), scalar2=None, op0=ALU.mult),
                         reads=[pv_b, s2[10]], writes=[tn["vb"].b])
                    if estop <= 4.1:
                        return
                    S.op("pool", lambda e: e.tensor_scalar(out=tn["gB"].t[:], in0=ones_f.t[:], scalar1=col(52), scalar2=None, op0=ALU.mult),
                         reads=[ones_f.b, s2[12]], writes=[tn["gB"].b])
                    yield
                    pr, pr_b = slot()
                    mm(pr, pr_b, tn["gB"].t[:], tn["gB"].b, tri.t[:], tri.b)
                    S.op("dve", lambda e: e.tensor_scalar(out=tn["t1"].t[:], in0=pr, scalar1=col(56), scalar2=0.0, op0=ALU.subtract, op1=ALU.max),
                         reads=[pr_b, s2[13]], writes=[tn["t1"].b])
                    S.op("act", lambda e: e.activation(out=tn["t1"].t[:], in_=tn["t1"].t[:], func=AF.Exp, scale=-1.0), reads=[tn["t1"].b], writes=[tn["t1"].b])
                    S.op("pool", lambda e: e.tensor_tensor(out=tn["Dn"].t[:], in0=tn["t1"].t[:], in1=maskL.t[:], op=ALU.mult),
                         reads=[tn["t1"].b, maskL.b], writes=[tn["Dn"].b])
                    S.op("dve", lambda e: e.tensor_scalar(out=tn["t2"].t[:], in0=pr, scalar1=col(56), scalar2=0.0, op0=ALU.subtract, op1=ALU.min),
                         reads=[pr_b, s2[13]], writes=[tn["t2"].b])
                    S.op("act", lambda e: e.activation(out=tn["t2"].t[:], in_=tn["t2"].t[:], func=AF.Exp), reads=[tn["t2"].b], writes=[tn["t2"].b])
                    S.op("pool", lambda e: e.tensor_tensor(out=tn["DT"].t[:], in0=tn["t2"].t[:], in1=tri.t[:], op=ALU.mult),
                         reads=[tn["t2"].b, tri.b], writes=[tn["DT"].b])
                    S.op("act", lambda e: e.activation(out=tn["egr"].t[:], in_=pr, func=AF.Exp), reads=[pr_b], writes=[tn["egr"].b])
                    S.op("pool", lambda e: e.tensor_tensor(out=tn["qdT"].t[:], in0=qT, in1=tn["egr"].t[:], op=ALU.mult),
                         reads=[qb, tn["egr"].b], writes=[tn["qdT"].b])
                    if estop <= 4.2:
                        return
                    yield
                    pkk, pkk_b = slot()
                    mm(pkk, pkk_b, kT, kb_, kT, kb_)
                    S.op("dve", lambda e: e.scalar_tensor_tensor(out=tn["M0"].t[:], in0=pkk, scalar=col(80), in1=tn["Dn"].t[:], op0=ALU.mult, op1=ALU.mult),
                         reads=[pkk_b, s2[18], tn["Dn"].b], writes=[tn["M0"].b])
                    yield
                    pn_, pn_b = slot()
                    mm(pn_, pn_b, tn["M0"].t[:], tn["M0"].b, ident.t[:], ident.b)
                    S.op("act", lambda e: e.copy(out=tn["N0"].t[:], in_=pn_), reads=[pn_b], writes=[tn["N0"].b])
                    S.op("dve", lambda e: e.tensor_tensor(out=tn["P0"].t[:], in0=pn_, in1=ident.t[:], op=ALU.add), reads=[pn_b, ident.b], writes=[tn["P0"].b])
                    yield
                    pqk, pqk_b = slot()
                    mm(pqk, pqk_b, kT, kb_, qT, qb)
                    S.op("dve", lambda e: e.tensor_tensor(out=tn["intraT"].t[:], in0=pqk, in1=tn["DT"].t[:], op=ALU.mult),
                         reads=[pqk_b, tn["DT"].b], writes=[tn["intraT"].b])
                    if estop <= 4.3:
                        return
                    cm, cn_, cp = "M0", "N0", "P0"
                    for s_ in range(6):
                        nm_, nn_, np_ = ("M1", "N1", "P1") if cm == "M0" else ("M0", "N0", "P0")
                        yield
                        p1, p1_b = slot()
                        mm(p1, p1_b, tn[cn_].t[:], tn[cn_].b, tn[cm].t[:], tn[cm].b)
                        S.op("act", lambda e: e.copy(out=tn[nm_].t[:], in_=p1), reads=[p1_b], writes=[tn[nm_].b])
                        if s_ < 5:
                            yield
                            p2, p2_b = slot()
                            mm(p2, p2_b, tn[cm].t[:], tn[cm].b, tn[cn_].t[:], tn[cn_].b)
                            S.op("dve", lambda e: e.tensor_copy(out=tn[nn_].t[:], in_=p2), reads=[p2_b], writes=[tn[nn_].b])
                        yield
                        p3, p3_b = slot()
                        mm(p3, p3_b, tn[nm_].t[:], tn[nm_].b, tn[cp].t[:], tn[cp].b)
                        S.op("dve", lambda e: e.tensor_tensor(out=tn[np_].t[:], in0=p3, in1=tn[cp].t[:], op=ALU.add),
                             reads=[p3_b, tn[cp].b], writes=[tn[np_].b])
                        cm, cn_, cp = nm_, nn_, np_
                    if estop <= 4.4:
                        return
                    TT = tn[cp]
                    yield
                    pu_, pu_b = slot()
                    mm(pu_, pu_b, TT.t[:], TT.b, tn["vb"].t[:], tn["vb"].b)
                    S.op("act", lambda e: e.copy(out=tn["u"].t[:], in_=pu_), reads=[pu_b], writes=[tn["u"].b])
                    yield
                    pw, pw_b = slot()
                    mm(pw, pw_b, tn["kbg"].t[:], tn["kbg"].b, TT.t[:], TT.b)
                    S.op("act", lambda e: e.copy(out=tn["wT"].t[:], in_=pw), reads=[pw_b], writes=[tn["wT"].b])
                    if estop <= 4.5:
                        return
                    Sh = St[h]
                    yield
                    pvn, pvn_b = slot()
                    mm(pvn, pvn_b, tn["wT"].t[:], tn["wT"].b, Sh.t[:], Sh.b)
                    S.op("dve", lambda e: e.tensor_tensor(out=tn["vnew"].t[:], in0=tn["u"].t[:], in1=pvn, op=ALU.subtract),
                         reads=[tn["u"].b, pvn_b], writes=[tn["vnew"].b])
                    yield
                    po_, po_b = slot()
                    mm(po_, po_b, tn["qdT"].t[:], tn["qdT"].b, Sh.t[:], Sh.b, start=True, stop=False)
                    mm(po_, po_b, tn["intraT"].t[:], tn["intraT"].b, tn["vnew"].t[:], tn["vnew"].b, start=False, stop=True)
                    yield
                    pS, pS_b = slot()
                    mm(pS, pS_b, tn["kdec"].t[:], tn["kdec"].b, tn["vnew"].t[:], tn["vnew"].b)
                    S.op("dve", lambda e: e.scalar_tensor_tensor(out=Sh.t[:], in0=Sh.t[:], scalar=col(64), in1=pS, op0=ALU.mult, op1=ALU.add),
                         reads=[Sh.b, s2[14], pS_b], writes=[Sh.b])
                    if estop <= 4.6:
                        return
                    S.op("act", lambda e: e.activation(out=tn["junk"].t[:], in_=po_, func=AF.Square, accum_out=sm2.t[:, 84 + h:85 + h]),
                         reads=[po_b], writes=[tn["junk"].b, s2[19]])
                    S.op("dve", lambda e: e.tensor_scalar(out=sm2.t[:, 88 + h:89 + h], in0=sm2.t[:, 84 + h:85 + h], scalar1=1.0 / 128, scalar2=RMS_EPS,
                                                          op0=ALU.mult, op1=ALU.add), reads=[s2[19]], writes=[s2[20]])
                    S.op("act", lambda e: e.activation(out=sm2.t[:, 88 + h:89 + h], in_=sm2.t[:, 88 + h:89 + h], func=AF.Sqrt), reads=[s2[20]], writes=[s2[20]])
                    S.op("dve", lambda e: e.reciprocal(out=sm2.t[:, 92 + h:93 + h], in_=sm2.t[:, 88 + h:89 + h]), reads=[s2[20]], writes=[s2[21]])
                    S.op("dve", lambda e: e.scalar_tensor_tensor(out=tn["yb"].t[:], in0=po_, scalar=sm2.t[:, 92 + h:93 + h], in1=gnw.t[:],
                                                                 op0=ALU.mult, op1=ALU.mult),
                         reads=[po_b, s2[21], gnw.b], writes=[tn["yb"].b])
                    S.op("pool", lambda e: e.tensor_tensor(out=mixt.t[:, 512 + h * 128:512 + (h + 1) * 128], in0=tn["yb"].t[:],
                                                           in1=sz.t[:, h * 128:(h + 1) * 128], op=ALU.mult),
                         reads=[tn["yb"].b, sz.b], writes=[mb])
                gens = [head_gen(h, tn_sets[h % 2]) for h in range(4)]
                for grp in ((0, 1), (2, 3)):
                    live = [gens[g] for g in grp]
                    while live:
                        for g in list(live):
                            try:
                                next(g)
                            except StopIteration:
                                live.remove(g)
                if estop < 5:
                    continue
                pT = k.ps[3]
                xTt = k.xTt[k.rr]
                k.rr ^= 1
                for c in range(8):
                    S.op("pe", lambda e: e.transpose(out=pT.t[:, c * 128:(c + 1) * 128], in_=mixt.t[:, c * 128:(c + 1) * 128], identity=ident.t[:]),
                         reads=[mb, ident.b], writes=[pT.bs[c // 4]], inc=(c % 4 == 3))
                S.op("act", lambda e: e.copy(out=xTt.t[:].rearrange("p c t -> p (c t)"), in_=pT.t[:]), reads=pT.bs, writes=[xTt.b])
                po2 = k.ps[0]
                for hb in range(2):
                    for c in range(8):
                        S.op("pe", lambda e: e.matmul(po2.t[:, hb * 512:(hb + 1) * 512], lhsT=xTt.t[:, c, :], rhs=w_out_b.t[:, c, hb * 512:(hb + 1) * 512],
                                                      start=(c == 0), stop=(c == 7)),
                             reads=[xTt.b, w_out_b.b], writes=[po2.bs[hb]], inc=(c == 7))
                epilogue_defer.append((t, po2))
                epilogue_now(k, t, work, lng, lnb, small, sm_b, po2)
        S.barrier()
    k.xcur = k.xres
    k.xcur_is_in = False


epilogue_defer = []


def epilogue_now(k, t, work, lng, lnb, small, sm_b, po2):
    epilogue(k, t, work, lng, lnb, small, sm_b, False, src=po2.t[:], src_bufs=po2.bs)


CONSTS = None
LAST_INPUTS = set()


def layer_inputs(inputs, b):
    m = {"x": np.ascontiguousarray(inputs["x"][b]), "ident": np.eye(128, dtype=np.float32)}
    inv_freq = (np.float32(10000.0) ** (-np.arange(0, 64, 2, dtype=np.float32) / np.float32(64))).astype(np.float32)
    ang = (np.arange(SEQ, dtype=np.float32)[:, None] * inv_freq[None, :]).astype(np.float32)
    cos2T = np.zeros((128, SEQ), np.float32)
    sin2T = np.zeros((128, SEQ), np.float32)
    cos2T[0:32] = np.cos(ang).T
    cos2T[32:64] = np.cos(ang).T
    sin2T[0:32] = np.sin(ang).T
    sin2T[32:64] = np.sin(ang).T
    m["cos2T"] = cos2T
    m["sin2T"] = sin2T
    m["tri"] = np.triu(np.ones((128, 128), np.float32))
    m["maskL"] = np.tril(np.ones((128, 128), np.float32), -1)
    for i in range(2):
        m["even_w_in_%d" % i] = inputs["even_w_in"][i]
        m["even_w_out_%d" % i] = inputs["even_w_out"][i]
        m["even_sgu_wT_%d" % i] = np.ascontiguousarray(np.transpose(inputs["even_sgu_w"][i], (2, 0, 1)))
        m["even_sgu_bT_%d" % i] = np.ascontiguousarray(inputs["even_sgu_b"][i].T)
        m["even_conv_wT_%d" % i] = np.ascontiguousarray(inputs["even_conv_w"][i].T.reshape(12, 128, 4).transpose(1, 0, 2))
        for nm in ("even_sgu_ln_g", "even_sgu_ln_b", "even_a_log", "even_dt_bias", "even_gdn_norm"):
            m["%s_%d" % (nm, i)] = inputs[nm][i][None, :]
    for i in range(2):
        for nm in ("odd_w_in", "odd_w_uq", "odd_w_ukv", "odd_w_out"):
            m["%s_%d" % (nm, i)] = inputs[nm][i]
        for nm in ("odd_q_norm", "odd_kv_norm"):
            m["%s_%d" % (nm, i)] = inputs[nm][i][None, :]
    for l in range(DEPTH):
        for nm in ("moe_w_router", "moe_w_gate_up", "moe_b_gate_up", "moe_w_down", "moe_b_down"):
            m["%s_%d" % (nm, l)] = inputs[nm][l]
        for nm in ("moe_b_router", "ln_ffn_g", "ln_ffn_b", "ln_mix_g", "ln_mix_b"):
            m["%s_%d" % (nm, l)] = inputs[nm][l][None, :]
    return m


def make_consts():
    return {"ident": np.eye(128, dtype=np.float32)}


def kernel(**inputs):
    phases = [("prep",)]
    for l in range(DEPTH):
        phases.append(("even", l) if l % 2 == 0 else ("odd", l))
        phases.append(("moe", l, l == DEPTH - 1))
    nc = build(phases)
    names = set(LAST_INPUTS)
    inputs = {k_: np.asarray(v) for k_, v in inputs.items()}
    n_cores = inputs["x"].shape[0]
    in_maps = []
    for b_ in range(n_cores):
        m = layer_inputs(inputs, b_)
        in_maps.append({k_: np.ascontiguousarray(v, dtype=np.float32) for k_, v in m.items() if k_ in names})
    res = run_bass_kernel_spmd(nc, in_maps, core_ids=list(range(n_cores)))
    return np.stack([np.asarray(r["y"], dtype=np.float32) for r in res.results], axis=0)
```
